# Optimizing a Trainium2 kernel written in Bass

```python
import jax
import jax.numpy as jnp
from jax import lax
import numpy as np

D_MODEL = 1024
BATCH = 16
SEQ = 4096
DEPTH = 4

GRID_W = 64
CTX_LEN = 256
HEAD_DIM = 64
ROPE_THETA = 10000.0
LN_EPS = 1e-6
MASK_VALUE = -1e30

CONV_CH = 512
CONV_K = 31
SWA_HEADS = 8
SWA_KV_HEADS = 2
SWA_GROUP = SWA_HEADS // SWA_KV_HEADS
SWA_WINDOW = 128
SWA_BLOCK = 128
POOL_CH = 512
POOL_WINDOWS = (2, 4, 8, 16)
POOL_GROUP = POOL_CH // len(POOL_WINDOWS)
MLA_HEADS = 8
MLA_Q_RANK = 256
MLA_KV_RANK = 128
MLA_NOPE = 64
MLA_ROPE = 32
MLA_V = 64
MLA_BLOCK = 128
N_BRANCH = 4
BRANCH_W = 512
N_EXPERTS = 32
TOP_K = 4
D_EXPERT = 1024
SWIGLU_LIMIT = 7.0
SWIGLU_ALPHA = 1.702
EXPERT_BLOCK = 256
DN_ALPHA = (2 * DEPTH) ** 0.25
DN_BETA = (8 * DEPTH) ** -0.25

IN_SIZES = (2 * CONV_CH, SWA_HEADS * HEAD_DIM, SWA_KV_HEADS * HEAD_DIM, SWA_KV_HEADS * HEAD_DIM,
            POOL_CH, MLA_Q_RANK, MLA_KV_RANK, MLA_ROPE)
IN_SPLITS = tuple(int(s) for s in np.cumsum(IN_SIZES)[:-1])
IN_W = int(sum(IN_SIZES))

kernel_name = 'hybrid_gated_mixers_moe_dit_block'


def layer_norm(x, g=None, b=None):
    xf = x.astype(jnp.float32)
    mu = xf.mean(-1, keepdims=True)
    var = jnp.square(xf - mu).mean(-1, keepdims=True)
    y = (xf - mu) * lax.rsqrt(var + LN_EPS)
    if g is not None:
        y = y * g.astype(jnp.float32) + b.astype(jnp.float32)
    return y.astype(x.dtype)


def rms_norm(x, g):
    xf = x.astype(jnp.float32)
    y = xf * lax.rsqrt(jnp.mean(xf * xf, -1, keepdims=True) + LN_EPS)
    return (y * g.astype(jnp.float32)).astype(x.dtype)


def axial_rope(seq_len, rot_dim):
    rows = seq_len // GRID_W
    row = jnp.repeat(jnp.arange(rows, dtype=jnp.float32), GRID_W)
    col = jnp.tile(jnp.arange(GRID_W, dtype=jnp.float32), rows)
    n_freq = rot_dim // 4
    inv = ROPE_THETA ** (-jnp.arange(n_freq, dtype=jnp.float32) / n_freq)
    ang = jnp.concatenate([row[:, None] * inv, col[:, None] * inv], axis=-1)
    return jnp.cos(ang), jnp.sin(ang)


def apply_rope(x, cos, sin):
    half = x.shape[-1] // 2
    xf = x.astype(jnp.float32)
    x1, x2 = xf[..., :half], xf[..., half:]
    cs, sn = cos[:, None, :], sin[:, None, :]
    return jnp.concatenate([x1 * cs - x2 * sn, x1 * sn + x2 * cs], axis=-1).astype(x.dtype)


def ada_terms(cvec, w, b):
    m = jax.nn.silu(cvec) @ w + b
    m = m.reshape(cvec.shape[:-1] + (1, 6, D_MODEL))
    return tuple(m[..., i, :] for i in range(6))


def modulate(x, shift, scale):
    return layer_norm(x) * (1 + scale) + shift


def conv_module(a, dw_w, dw_b, ln_g, ln_b):
    val, gate = jnp.split(a, 2, axis=-1)
    v = val * jax.nn.sigmoid(gate)
    y = lax.conv_general_dilated(v, dw_w[:, None, :], (1,), [(CONV_K // 2, CONV_K // 2)],
                                 dimension_numbers=('NWC', 'WIO', 'NWC'),
                                 feature_group_count=CONV_CH) + dw_b
    return jax.nn.silu(layer_norm(y, ln_g, ln_b))


def pool_mixer(u, pool_w, pool_scale):
    bsz, L, _ = u.shape
    cs = jnp.concatenate([jnp.zeros((bsz, 1, POOL_CH), jnp.float32),
                          jnp.cumsum(u.astype(jnp.float32), axis=1)], axis=1)
    t = jnp.arange(L)
    means = []
    for g, w in enumerate(POOL_WINDOWS):
        lo = jnp.clip(t - w // 2, 0, L)
        hi = jnp.clip(t + w // 2, 0, L)
        seg = cs[:, :, g * POOL_GROUP:(g + 1) * POOL_GROUP]
        means.append((seg[:, hi] - seg[:, lo]) / (hi - lo).astype(jnp.float32)[:, None])
    pooled = jnp.concatenate(means, axis=-1).astype(u.dtype) - u
    y = jnp.einsum('blgc,gcd->blgd', pooled.reshape(bsz, L, len(POOL_WINDOWS), POOL_GROUP), pool_w)
    return y.reshape(bsz, L, POOL_CH) * pool_scale


def sink_softmax(scores, sink):
    sk = jnp.broadcast_to(sink.astype(jnp.float32).reshape(SWA_KV_HEADS, SWA_GROUP)[None, :, :, None, None],
                          scores.shape[:-1] + (1,))
    return jax.nn.softmax(jnp.concatenate([scores, sk], axis=-1), axis=-1)[..., :-1]


def swa_latent(q, k, v, kc, vc, sink):
    bsz, L = q.shape[:2]
    nb = L // SWA_BLOCK
    span = SWA_BLOCK + 2 * SWA_WINDOW
    pad = ((0, 0), (SWA_WINDOW, SWA_WINDOW), (0, 0), (0, 0))
    kp, vp = jnp.pad(k, pad), jnp.pad(v, pad)
    r = jnp.arange(SWA_BLOCK)[:, None]
    s = jnp.arange(span)[None, :]
    band = (s >= r) & (s - r <= 2 * SWA_WINDOW)
    scale = HEAD_DIM ** -0.5
    qb = q.reshape(bsz, nb, SWA_BLOCK, SWA_KV_HEADS, SWA_GROUP, HEAD_DIM).swapaxes(0, 1)

    def block(args):
        i, qi = args
        start = i * SWA_BLOCK
        ki = lax.dynamic_slice_in_dim(kp, start, span, axis=1)
        vi = lax.dynamic_slice_in_dim(vp, start, span, axis=1)
        key_pos = start - SWA_WINDOW + jnp.arange(span)
        mask = band & ((key_pos >= 0) & (key_pos < L))[None, :]
        s_lat = jnp.where(mask, jnp.einsum('bqkgd,bskd->bkgqs', qi, ki).astype(jnp.float32) * scale, MASK_VALUE)
        s_ctx = jnp.einsum('bqkgd,bckd->bkgqc', qi, kc).astype(jnp.float32) * scale
        p = sink_softmax(jnp.concatenate([s_lat, s_ctx], axis=-1), sink).astype(v.dtype)
        return (jnp.einsum('bkgqs,bskd->bqkgd', p[..., :span], vi)
                + jnp.einsum('bkgqc,bckd->bqkgd', p[..., span:], vc))

    out = lax.map(block, (jnp.arange(nb), qb))
    return out.swapaxes(0, 1).reshape(bsz, L, SWA_HEADS * HEAD_DIM)


def swa_context(qc, kc, vc, sink):
    bsz, C = qc.shape[:2]
    s = jnp.einsum('bqkgd,bckd->bkgqc', qc, kc).astype(jnp.float32) * HEAD_DIM ** -0.5
    p = sink_softmax(s, sink).astype(vc.dtype)
    return jnp.einsum('bkgqc,bckd->bqkgd', p, vc).reshape(bsz, C, SWA_HEADS * HEAD_DIM)


def mla_queries(cq, q_g, w_uq, w_uk, rope):
    q = (rms_norm(cq, q_g) @ w_uq).reshape(cq.shape[:-1] + (MLA_HEADS, MLA_NOPE + MLA_ROPE))
    q_nope, q_rope = q[..., :MLA_NOPE], q[..., MLA_NOPE:]
    if rope is not None:
        q_rope = apply_rope(q_rope, *rope)
    return jnp.einsum('blhn,rhn->blhr', q_nope, w_uk), q_rope


def mla_attend(q_abs, q_rope, ckv, kr, w_uv):
    s = (jnp.einsum('bqhr,bsr->bhqs', q_abs, ckv)
         + jnp.einsum('bqhe,bse->bhqs', q_rope, kr)).astype(jnp.float32) * (MLA_NOPE + MLA_ROPE) ** -0.5
    p = jax.nn.softmax(s, axis=-1).astype(ckv.dtype)
    o = jnp.einsum('bqhr,rhv->bqhv', jnp.einsum('bhqs,bsr->bqhr', p, ckv), w_uv)
    return o.reshape(o.shape[:2] + (MLA_HEADS * MLA_V,))


def mla_latent(q_abs, q_rope, ckv, kr, w_uv):
    bsz, L = q_abs.shape[:2]
    nb = L // MLA_BLOCK
    qa = q_abs.reshape(bsz, nb, MLA_BLOCK, MLA_HEADS, MLA_KV_RANK).swapaxes(0, 1)
    qr = q_rope.reshape(bsz, nb, MLA_BLOCK, MLA_HEADS, MLA_ROPE).swapaxes(0, 1)
    out = lax.map(lambda a: mla_attend(a[0], a[1], ckv, kr, w_uv), (qa, qr))
    return out.swapaxes(0, 1).reshape(bsz, L, MLA_HEADS * MLA_V)


def merge_branches(h, ys, w_branch, w_gate, b_gate, w_out):
    merged = None
    for i, y in enumerate(ys):
        term = jax.nn.sigmoid(h @ w_gate[i] + b_gate[i]) * (y @ w_branch[i])
        merged = term if merged is None else merged + term
    return merged @ w_out


def token_mixer(h_lat, h_ctx, rope_swa, rope_mla, w_in, conv_w, conv_b, conv_ln_g, conv_ln_b, swa_sink,
                pool_w, pool_scale, mla_q_g, mla_w_uq, mla_kv_g, mla_w_uk, mla_w_uv,
                w_branch, w_gate, b_gate, w_out, ctx_out):
    bsz, L, _ = h_lat.shape
    C = h_ctx.shape[1]
    a_l, q_l, k_l, v_l, p_l, cq_l, ckv_l, kr_l = jnp.split(h_lat @ w_in, IN_SPLITS, axis=-1)
    a_c, q_c, k_c, v_c, p_c, cq_c, ckv_c, kr_c = jnp.split(h_ctx @ w_in, IN_SPLITS, axis=-1)
    kc = k_c.reshape(bsz, C, SWA_KV_HEADS, HEAD_DIM)
    vc = v_c.reshape(bsz, C, SWA_KV_HEADS, HEAD_DIM)
    ckv_ctx = rms_norm(ckv_c, mla_kv_g)

    y_a = conv_module(a_l, conv_w, conv_b, conv_ln_g, conv_ln_b)
    q = apply_rope(q_l.reshape(bsz, L, SWA_HEADS, HEAD_DIM), *rope_swa)
    q = q.reshape(bsz, L, SWA_KV_HEADS, SWA_GROUP, HEAD_DIM)
    k = apply_rope(k_l.reshape(bsz, L, SWA_KV_HEADS, HEAD_DIM), *rope_swa)
    v = v_l.reshape(bsz, L, SWA_KV_HEADS, HEAD_DIM)
    y_b = swa_latent(q, k, v, kc, vc, swa_sink)
    y_c = pool_mixer(p_l, pool_w, pool_scale)
    qa, qr = mla_queries(cq_l, mla_q_g, mla_w_uq, mla_w_uk, rope_mla)
    kr_lat = apply_rope(kr_l[:, :, None, :], *rope_mla)[:, :, 0]
    ckv_keys = jnp.concatenate([rms_norm(ckv_l, mla_kv_g), ckv_ctx], axis=1)
    kr_keys = jnp.concatenate([kr_lat, kr_c], axis=1)
    y_d = mla_latent(qa, qr, ckv_keys, kr_keys, mla_w_uv)
    y_lat = merge_branches(h_lat, (y_a, y_b, y_c, y_d), w_branch, w_gate, b_gate, w_out)
    if not ctx_out:
        return y_lat, None

    y_a_c = conv_module(a_c, conv_w, conv_b, conv_ln_g, conv_ln_b)
    y_b_c = swa_context(q_c.reshape(bsz, C, SWA_KV_HEADS, SWA_GROUP, HEAD_DIM), kc, vc, swa_sink)
    y_c_c = pool_mixer(p_c, pool_w, pool_scale)
    qa_c, qr_c = mla_queries(cq_c, mla_q_g, mla_w_uq, mla_w_uk, None)
    y_d_c = mla_attend(qa_c, qr_c, ckv_ctx, kr_c, mla_w_uv)
    y_ctx = merge_branches(h_ctx, (y_a_c, y_b_c, y_c_c, y_d_c), w_branch, w_gate, b_gate, w_out)
    return y_lat, y_ctx


def moe(h, router_w, router_b, w_gu, b_gu, w_down, b_down):
    n_tok = h.shape[0]
    logits = (h @ router_w + router_b).astype(jnp.float32)
    top_v, top_e = lax.top_k(logits, TOP_K)
    top_w = jax.nn.softmax(top_v, axis=-1)
    n_assign = n_tok * TOP_K
    flat_e = top_e.reshape(-1)
    order = jnp.argsort(flat_e)
    sorted_e = flat_e[order]
    sorted_tok = (order // TOP_K).astype(jnp.int32)
    sorted_w = top_w.reshape(-1)[order]
    counts = jnp.bincount(flat_e, length=N_EXPERTS)
    padded = (counts + EXPERT_BLOCK - 1) // EXPERT_BLOCK * EXPERT_BLOCK
    pad_end = jnp.cumsum(padded)
    pad_start = pad_end - padded
    grp_start = jnp.cumsum(counts) - counts
    dest = pad_start[sorted_e] + jnp.arange(n_assign, dtype=jnp.int32) - grp_start[sorted_e]
    n_blocks = -(-(n_assign + N_EXPERTS * (EXPERT_BLOCK - 1)) // EXPERT_BLOCK)
    n_rows = n_blocks * EXPERT_BLOCK
    row_tok = jnp.full((n_rows,), n_tok, jnp.int32).at[dest].set(sorted_tok)
    row_w = jnp.zeros((n_rows,), jnp.float32).at[dest].set(sorted_w)
    blk_e = jnp.minimum(jnp.searchsorted(pad_end, jnp.arange(n_blocks, dtype=jnp.int32) * EXPERT_BLOCK,
                                         side='right'), N_EXPERTS - 1)
    h_pad = jnp.concatenate([h, jnp.zeros((1, h.shape[1]), h.dtype)], axis=0)

    def step(acc, blk):
        tok, wgt, e = blk
        z = h_pad[tok] @ w_gu[e] + b_gu[e]
        gate = jnp.minimum(z[:, :D_EXPERT], SWIGLU_LIMIT)
        up = jnp.clip(z[:, D_EXPERT:], -SWIGLU_LIMIT, SWIGLU_LIMIT)
        y = ((up + 1) * gate * jax.nn.sigmoid(SWIGLU_ALPHA * gate)) @ w_down[e] + b_down[e]
        return acc.at[tok].add(wgt[:, None].astype(y.dtype) * y), None

    acc, _ = lax.scan(step, jnp.zeros((n_tok + 1, h.shape[1]), h.dtype),
                      (row_tok.reshape(n_blocks, EXPERT_BLOCK), row_w.reshape(n_blocks, EXPERT_BLOCK), blk_e))
    return acc[:n_tok]


def setup_inputs(seed: int = 0) -> dict:
    key = jax.random.key(seed)
    ks = iter(jax.random.split(key, 40))

    def nrm(shape, scale):
        return jax.random.normal(next(ks), shape, jnp.float32) * scale

    def gain(shape):
        return 1.0 + nrm(shape, 0.02)

    L, D = DEPTH, D_MODEL
    return {
        'x': nrm((BATCH, SEQ, D), 1.0),
        'c': nrm((BATCH, D), 1.0),
        'ctx': nrm((BATCH, CTX_LEN, D), 1.0),
        'c_ctx': nrm((D,), 1.0),
        'w_ada': nrm((L, D, 6 * D), D ** -0.5),
        'b_ada': nrm((L, 6 * D), 0.02),
        'w_in': nrm((L, D, IN_W), D ** -0.5),
        'conv_w': nrm((L, CONV_K, CONV_CH), CONV_K ** -0.5),
        'conv_b': nrm((L, CONV_CH), 0.02),
        'conv_ln_g': gain((L, CONV_CH)),
        'conv_ln_b': nrm((L, CONV_CH), 0.02),
        'swa_sink': nrm((L, SWA_HEADS), 1.0),
        'pool_w': nrm((L, len(POOL_WINDOWS), POOL_GROUP, POOL_GROUP), POOL_GROUP ** -0.5),
        'pool_scale': gain((L, POOL_CH)),
        'mla_q_g': gain((L, MLA_Q_RANK)),
        'mla_w_uq': nrm((L, MLA_Q_RANK, MLA_HEADS * (MLA_NOPE + MLA_ROPE)), MLA_Q_RANK ** -0.5),
        'mla_kv_g': gain((L, MLA_KV_RANK)),
        'mla_w_uk': nrm((L, MLA_KV_RANK, MLA_HEADS, MLA_NOPE), MLA_KV_RANK ** -0.5),
        'mla_w_uv': nrm((L, MLA_KV_RANK, MLA_HEADS, MLA_V), MLA_KV_RANK ** -0.5),
        'w_branch': nrm((L, N_BRANCH, BRANCH_W, D), BRANCH_W ** -0.5),
        'w_gate': nrm((L, N_BRANCH, D, D), D ** -0.5),
        'b_gate': nrm((L, N_BRANCH, D), 0.02),
        'w_out': nrm((L, D, D), D ** -0.5 * DN_BETA),
        'ln1_g': gain((L, D)),
        'ln1_b': nrm((L, D), 0.02),
        'router_w': nrm((L, D, N_EXPERTS), D ** -0.5),
        'router_b': nrm((L, N_EXPERTS), 0.01),
        'w_gu': nrm((L, N_EXPERTS, D, 2 * D_EXPERT), D ** -0.5),
        'b_gu': nrm((L, N_EXPERTS, 2 * D_EXPERT), 0.02),
        'w_down': nrm((L, N_EXPERTS, D_EXPERT, D), D_EXPERT ** -0.5 * DN_BETA),
        'b_down': nrm((L, N_EXPERTS, D), 0.02),
        'ln2_g': gain((L, D)),
        'ln2_b': nrm((L, D), 0.02),
    }


def reference(x, c, ctx, c_ctx, w_ada, b_ada, w_in, conv_w, conv_b, conv_ln_g, conv_ln_b, swa_sink,
              pool_w, pool_scale, mla_q_g, mla_w_uq, mla_kv_g, mla_w_uk, mla_w_uv, w_branch, w_gate, b_gate,
              w_out, ln1_g, ln1_b, router_w, router_b, w_gu, b_gu, w_down, b_down, ln2_g, ln2_b):
    seq_len = x.shape[1]
    rope_swa = axial_rope(seq_len, HEAD_DIM)
    rope_mla = axial_rope(seq_len, MLA_ROPE)
    for l in range(DEPTH):
        ctx_out = l < DEPTH - 1
        sh1, sc1, g1, sh2, sc2, g2 = ada_terms(c, w_ada[l], b_ada[l])
        csh1, csc1, cg1, csh2, csc2, cg2 = ada_terms(c_ctx, w_ada[l], b_ada[l])
        y_lat, y_ctx = token_mixer(modulate(x, sh1, sc1), modulate(ctx, csh1, csc1), rope_swa, rope_mla,
                                   w_in[l], conv_w[l], conv_b[l], conv_ln_g[l], conv_ln_b[l], swa_sink[l],
                                   pool_w[l], pool_scale[l], mla_q_g[l], mla_w_uq[l], mla_kv_g[l],
                                   mla_w_uk[l], mla_w_uv[l], w_branch[l], w_gate[l], b_gate[l], w_out[l],
                                   ctx_out)
        x = layer_norm(DN_ALPHA * x + g1 * y_lat, ln1_g[l], ln1_b[l])
        h_lat = modulate(x, sh2, sc2).reshape(-1, D_MODEL)
        n_lat = h_lat.shape[0]
        if ctx_out:
            ctx = layer_norm(DN_ALPHA * ctx + cg1 * y_ctx, ln1_g[l], ln1_b[l])
            h_ctx = modulate(ctx, csh2, csc2).reshape(-1, D_MODEL)
            m = moe(jnp.concatenate([h_lat, h_ctx], axis=0), router_w[l], router_b[l],
                    w_gu[l], b_gu[l], w_down[l], b_down[l])
            m_lat = m[:n_lat]
            ctx = layer_norm(DN_ALPHA * ctx + cg2 * m[n_lat:].reshape(ctx.shape), ln2_g[l], ln2_b[l])
        else:
            m_lat = moe(h_lat, router_w[l], router_b[l], w_gu[l], b_gu[l], w_down[l], b_down[l])
        x = layer_norm(DN_ALPHA * x + g2 * m_lat.reshape(x.shape), ln2_g[l], ln2_b[l])
    return x
```

```python
import numpy as np
import concourse.bass as bass
import concourse.mybir as mybir
from concourse.bass_utils import run_bass_kernel_spmd

F32 = mybir.dt.float32
BF16 = mybir.dt.bfloat16
I32 = mybir.dt.int32
U32 = mybir.dt.uint32
AF = mybir.ActivationFunctionType
ALU = mybir.AluOpType
AX = mybir.AxisListType


class Buf:
    __slots__ = ("name", "w", "rs")

    def __init__(self, name=""):
        self.name = name
        self.w = None
        self.rs = []


class Eng:
    def __init__(self, fw, eng, name):
        self.fw = fw
        self.e = eng
        self.name = name
        self.sem = fw.nc.alloc_semaphore("s_" + name)
        self.cnt = 0
        self.inorder = (name == "pe")
        self.waited = {}

    def wait_tok(self, tok):
        if tok is None:
            return
        sem, val = tok
        if self.inorder and sem is self.sem:
            return
        k = id(sem)
        if self.waited.get(k, 0) < val:
            self.e.wait_ge(sem, val)
            self.waited[k] = val

    def deps(self, reads, writes):
        for b in reads:
            self.wait_tok(b.w)
        for b in writes:
            self.wait_tok(b.w)
            for t in b.rs:
                self.wait_tok(t)

    def mark(self, tok, reads, writes):
        for b in reads:
            b.rs.append(tok)
        for b in writes:
            b.w = tok
            b.rs = []

    def op(self, ins, reads=(), writes=()):
        self.deps(reads, writes)
        i = ins()
        self.cnt += 1
        i.then_inc(self.sem, 1)
        tok = (self.sem, self.cnt)
        self.mark(tok, reads, writes)
        return tok


class DmaQ:
    def __init__(self, fw, engw, npool, name):
        self.fw = fw
        self.engw = engw
        self.pool = [[fw.nc.alloc_semaphore(f"d_{name}_{i}"), 0] for i in range(npool)]
        self.nxt = 0

    def dma(self, out, in_, reads=(), writes=(), fn=None, **kw):
        ew = self.engw
        ew.deps(reads, writes)
        slot = self.pool[self.nxt]
        self.nxt = (self.nxt + 1) % len(self.pool)
        sem, val = slot
        if val:
            ew.wait_tok((sem, val))
        if fn is None:
            i = ew.e.dma_start(out=out, in_=in_, **kw)
        else:
            i = fn(ew.e)
        slot[1] = val + 16
        i.then_inc(sem, 16)
        tok = (sem, val + 16)
        ew.mark(tok, reads, writes)
        return tok


class FW:
    def __init__(self, nc):
        self.nc = nc
        self.pe = Eng(self, nc.tensor, "pe")
        self.act = Eng(self, nc.scalar, "act")
        self.dve = Eng(self, nc.vector, "dve")
        self.pool = Eng(self, nc.gpsimd, "pool")
        self.sp = Eng(self, nc.sync, "sp")
        self.engs = [self.pe, self.act, self.dve, self.pool, self.sp]
        self.q_sp = DmaQ(self, self.sp, 24, "sp")
        self.q_pool = DmaQ(self, self.pool, 16, "pool")
        self.q_act = DmaQ(self, self.act, 8, "act")
        self.qs = [self.q_sp, self.q_pool, self.q_act]

    def barrier(self, engs=None):
        toks = []
        for e in self.engs:
            if e.cnt:
                toks.append((e.sem, e.cnt))
        for q in self.qs:
            for sem, val in q.pool:
                if val:
                    toks.append((sem, val))
        for e in (engs or self.engs):
            for t in toks:
                e.wait_tok(t)
from contextlib import ExitStack

D = 1024
NB = 2
L = 4096
C = 256
S = L + C
T = NB * S
DEPTH = 4
INW = 2720
EXTW = INW + 512 + 128 + 32
Q0, K0, V0, P0_, CQ0, CKV0, KR0 = 1024, 1536, 1664, 1792, 2304, 2560, 2688
QR0, KRT0, KRR0 = 2720, 3232, 3360
EPS = 1e-6
ALPHA = 8 ** 0.25
NE = 32
BLK = 256
NBLK = -(-(T * 4 + NE * (BLK - 1)) // BLK)
NROWS = NBLK * BLK


def tiles_all():
    out = []
    for b in range(NB):
        for i in range(L // 512):
            out.append((b * S + i * 512, 512, b, False, i * 512))
        out.append((b * S + L, 256, b, True, 0))
    return out


class KB:
    def __init__(self, nc, dbg=()):
        self.nc = nc
        self.f = FW(nc)
        self.dbg = set(dbg)
        self.dram = {}
        self.psf = []
        self.psb = []
        for i in range(6):
            self.psf.append((nc.alloc_psum_tensor(f"psf{i}", [128, 512], F32).ap(), Buf(f"psf{i}")))
        for i in range(2):
            self.psb.append((nc.alloc_psum_tensor(f"psb{i}", [128, 1024], BF16).ap(), Buf(f"psb{i}")))
        self.psf_i = 0
        self.pinned = set()
        self.bregs = {}
        self.psb_i = 0

    def ps(self):
        while True:
            i = self.psf_i % len(self.psf)
            self.psf_i += 1
            if i not in self.pinned:
                return self.psf[i]

    def breg(self, v):
        if v not in self.bregs:
            self.bregs[v] = self.nc.gpsimd.to_reg(v)
        return self.bregs[v]

    def pin(self):
        while True:
            i = self.psf_i % len(self.psf)
            self.psf_i += 1
            if i not in self.pinned:
                self.pinned.add(i)
                return self.psf[i] + (i,)

    def unpin(self, i):
        self.pinned.discard(i)

    def pb(self):
        r = self.psb[self.psb_i % len(self.psb)]
        self.psb_i += 1
        return r

    def din(self, name, shape, dt=F32):
        t = self.nc.dram_tensor(name, list(shape), dt, kind="ExternalInput").ap()
        self.dram[name] = (t, Buf(name))
        return t

    def dscr(self, name, shape, dt):
        kind = "ExternalOutput" if name in self.dbg else "Internal"
        t = self.nc.dram_tensor(name, list(shape), dt, kind=kind).ap()
        self.dram[name] = (t, Buf(name))
        return t

    def sb(self, es, name, shape, dt):
        self.uid = getattr(self, "uid", 0) + 1
        h = es.enter_context(self.nc.sbuf_tensor(f"{name}_u{self.uid}", list(shape), dt))
        return h.ap(), Buf(name)

    def ring(self, es, name, shape, dt, n=2):
        return [self.sb(es, f"{name}{i}", shape, dt) for i in range(n)]


def build_consts(kb, es):
    nc, f = kb.nc, kb.f
    c = {}
    ident, identB = kb.sb(es, "ident", [128, 128], BF16)
    f.pool.op(lambda: nc.gpsimd.memset(ident, 1.0), writes=[identB])
    f.pool.op(lambda: nc.gpsimd.affine_select(out=ident, in_=ident, pattern=[[-1, 128]], compare_op=ALU.is_equal,
                                              fill=0.0, base=0, channel_multiplier=1), reads=[identB], writes=[identB])
    c["ident"] = (ident, identB)
    onesb, onesbB = kb.sb(es, "onesb", [128, 128], BF16)
    f.pool.op(lambda: nc.gpsimd.memset(onesb, 1.0), writes=[onesbB])
    c["onesb"] = (onesb, onesbB)
    onesf, onesfB = kb.sb(es, "onesf", [128, 128], F32)
    f.pool.op(lambda: nc.gpsimd.memset(onesf, 1.0), writes=[onesfB])
    c["onesf"] = (onesf, onesfB)
    nh, nhB = kb.sb(es, "neghalf", [128, 512], F32)
    f.pool.op(lambda: nc.gpsimd.memset(nh, -0.5), writes=[nhB])
    c["neghalf"] = (nh, nhB)
    orow, orowB = kb.sb(es, "onesrow", [1, 512], BF16)
    f.pool.op(lambda: nc.gpsimd.memset(orow, 1.0), writes=[orowB])
    c["onesrow"] = (orow, orowB)
    return c


def phase_ada(kb, cs, c_in, cctx_in, w_ada, b_ada, ADA):
    nc, f = kb.nc, kb.f
    ADAb = kb.dram["ADA"][1]
    with ExitStack() as es:
        crow, crowB = kb.sb(es, "crow", [3, D], F32)
        f.q_sp.dma(crow[0:2, :], c_in, writes=[crowB])
        f.q_sp.dma(crow[2:3, :], cctx_in.rearrange("(o d) -> o d", o=1), writes=[crowB])
        sg, sgB = kb.sb(es, "csg", [3, D], F32)
        f.act.op(lambda: nc.scalar.activation(out=sg, in_=crow, func=AF.Sigmoid), reads=[crowB], writes=[sgB])
        f.dve.op(lambda: nc.vector.tensor_tensor(out=sg, in0=sg, in1=crow, op=ALU.mult), reads=[sgB, crowB], writes=[sgB])
        id3, id3B = kb.sb(es, "id3", [3, 3], F32)
        f.pool.op(lambda: nc.gpsimd.memset(id3, 1.0), writes=[id3B])
        f.pool.op(lambda: nc.gpsimd.affine_select(out=id3, in_=id3, pattern=[[-1, 3]], compare_op=ALU.is_equal,
                                                  fill=0.0, base=0, channel_multiplier=1), reads=[id3B], writes=[id3B])
        sT, sTB = kb.sb(es, "sT", [128, 8, 3], F32)
        pt, ptB = kb.ps()
        for k in range(8):
            f.pe.op(lambda: nc.tensor.matmul(pt[:, k * 3:(k + 1) * 3], lhsT=sg[:, k * 128:(k + 1) * 128], rhs=id3,
                                             start=True, stop=True), reads=[sgB, id3B], writes=[ptB])
        f.dve.op(lambda: nc.vector.tensor_copy(out=sT.rearrange("p k r -> p (k r)"), in_=pt[:, 0:24]), reads=[ptB], writes=[sTB])
        ones3, ones3B = kb.sb(es, "ones3", [1, 3], F32)
        f.pool.op(lambda: nc.gpsimd.memset(ones3, 1.0), writes=[ones3B])
        wr = kb.ring(es, "wada", [128, 8, 512], F32, 2)
        br = kb.ring(es, "bada", [1, 512], F32, 2)
        orr = kb.ring(es, "oada", [3, 512], F32, 2)
        it = 0
        for l in range(DEPTH):
            wv = w_ada[l].rearrange("(k p) c -> p k c", p=128)
            for cc in range(12):
                w, wB = wr[it % 2]
                bb, bbB = br[it % 2]
                o, oB = orr[it % 2]
                f.q_sp.dma(w, wv[:, :, cc * 512:(cc + 1) * 512], writes=[wB])
                f.q_sp.dma(bb, b_ada[l:l + 1, cc * 512:(cc + 1) * 512], writes=[bbB])
                p, pB = kb.ps()
                for k in range(8):
                    f.pe.op(lambda: nc.tensor.matmul(p[0:3, :], lhsT=sT[:, k, :], rhs=w[:, k, :], start=(k == 0), stop=False),
                            reads=[sTB, wB], writes=[pB])
                f.pe.op(lambda: nc.tensor.matmul(p[0:3, :], lhsT=ones3, rhs=bb, start=False, stop=True),
                        reads=[ones3B, bbB], writes=[pB])
                f.act.op(lambda: nc.scalar.copy(out=o, in_=p[0:3, :]), reads=[pB], writes=[oB])
                f.q_sp.dma(ADA[l, :, cc * 512:(cc + 1) * 512], o, reads=[oB], writes=[ADAb])
                it += 1
    f.barrier()


def load_bcast(kb, dst, dstB, src_row_ap, n=128):
    kb.f.q_sp.dma(dst, src_row_ap.partition_broadcast(n), writes=[dstB])


def ln_tile(kb, xs, xsB, st, stB, eps_sqrt=True):
    nc, f = kb.nc, kb.f
    f.dve.op(lambda: nc.vector.bn_stats(out=st[:, 0:6], in_=xs[:, 0:512]), reads=[xsB], writes=[stB])
    f.dve.op(lambda: nc.vector.bn_stats(out=st[:, 6:12], in_=xs[:, 512:1024]), reads=[xsB], writes=[stB])
    f.dve.op(lambda: nc.vector.bn_aggr(out=st[:, 12:14], in_=st[:, 0:12]), reads=[stB], writes=[stB])
    f.dve.op(lambda: nc.vector.tensor_scalar(out=st[:, 14:15], in0=st[:, 13:14], scalar1=EPS, scalar2=None, op0=ALU.add),
             reads=[stB], writes=[stB])
    nh, nhB = kb.c["neghalf"]
    f.pool.op(lambda: nc.gpsimd.tensor_tensor(out=st[:, 15:16], in0=st[:, 14:15], in1=nh[:, 0:1], op=ALU.pow),
              reads=[stB, nhB], writes=[stB])
    return st[:, 12:13], st[:, 15:16]


def phase_proj(kb, l, xsrc, w_in, mla_q_g, mla_w_uq, mla_kv_g, mla_w_uk, mla_w_uv, tabs, ADA):
    nc, f = kb.nc, kb.f
    dr = kb.dram
    with ExitStack() as es:
        wext, wextB = kb.sb(es, "wext", [128, 8, EXTW], BF16)
        wv = w_in[l].rearrange("(k p) c -> p k c", p=128)
        f.q_pool.dma(wext[:, :, 0:2048], wv[:, :, 0:2048], writes=[wextB])
        f.q_pool.dma(wext[:, :, 2048:INW], wv[:, :, 2048:INW], writes=[wextB])

        def rot(dst0, src0, nheads, half, eng_neg, eng_cp, w=wext, wB=wextB, nk=8):
            for k in range(nk):
                s = w[:, k, src0:src0 + nheads * 2 * half].rearrange("p (h two d) -> p h two d", two=2, d=half)
                d = w[:, k, dst0:dst0 + nheads * 2 * half].rearrange("p (h two d) -> p h two d", two=2, d=half)
                f.act.op(lambda: nc.scalar.mul(out=d[:, :, 0, :], in_=s[:, :, 1, :], mul=-1.0), reads=[wB], writes=[wB])
                f.dve.op(lambda: nc.vector.tensor_copy(out=d[:, :, 1, :], in_=s[:, :, 0, :]), reads=[wB], writes=[wB])
        rot(QR0, Q0, 8, 32, None, None)
        rot(KRT0, K0, 2, 32, None, None)
        rot(KRR0, KR0, 1, 16, None, None)
        wuq, wuqB = kb.sb(es, "wuq", [128, 2, 768], BF16)
        f.q_pool.dma(wuq, mla_w_uq[l].rearrange("(k p) c -> p k c", p=128), writes=[wuqB])
        wuqr, wuqrB = kb.sb(es, "wuqr", [128, 2, 768], BF16)
        f.pool.op(lambda: nc.gpsimd.memset(wuqr, 0.0), writes=[wuqrB])
        for k in range(2):
            s = wuq[:, k, :].rearrange("p (h c) -> p h c", c=96)[:, :, 64:96].rearrange("p h (two d) -> p h two d", two=2)
            d = wuqr[:, k, :].rearrange("p (h c) -> p h c", c=96)[:, :, 64:96].rearrange("p h (two d) -> p h two d", two=2)
            f.act.op(lambda: nc.scalar.mul(out=d[:, :, 0, :], in_=s[:, :, 1, :], mul=-1.0), reads=[wuqB, wuqrB], writes=[wuqrB])
            f.dve.op(lambda: nc.vector.tensor_copy(out=d[:, :, 1, :], in_=s[:, :, 0, :]), reads=[wuqB, wuqrB], writes=[wuqrB])
        wuk, wukB = kb.sb(es, "wuk", [128, 512], BF16)
        f.q_pool.dma(wuk, mla_w_uk[l].rearrange("r h n -> r (h n)"), writes=[wukB])
        wuv, wuvB = kb.sb(es, "wuv", [128, 512], BF16)
        f.q_pool.dma(wuv, mla_w_uv[l].rearrange("r h n -> r (h n)"), writes=[wuvB])
        gq, gqB = kb.sb(es, "gq", [128, 2], F32)
        f.q_sp.dma(gq, mla_q_g[l].rearrange("(k p) -> p k", p=128), writes=[gqB])
        gkv, gkvB = kb.sb(es, "gkv", [128, 1], F32)
        f.q_sp.dma(gkv, mla_kv_g[l].rearrange("(p o) -> p o", o=1), writes=[gkvB])
        mods = []
        for r in range(3):
            sc, scB = kb.sb(es, f"sc1_{r}", [128, D], F32)
            sh, shB = kb.sb(es, f"sh1_{r}", [128, D], F32)
            load_bcast(kb, sh, shB, ADA[l, r:r + 1, 0:D])
            load_bcast(kb, sc, scB, ADA[l, r:r + 1, D:2 * D])
            f.pool.op(lambda: nc.gpsimd.tensor_scalar(out=sc, in0=sc, scalar1=1.0, scalar2=None, op0=ALU.add),
                      reads=[scB], writes=[scB])
            mods.append((sc, scB, sh, shB))
        ident, identB = kb.c["ident"]
        onesb, onesbB = kb.c["onesb"]
        nh, nhB = kb.c["neghalf"]
        xs_r = kb.ring(es, "xs", [128, D], F32, 2)
        st_r = kb.ring(es, "st", [128, 16], F32, 2)
        xn_r = kb.ring(es, "xn", [128, D], F32, 2)
        hb_r = kb.ring(es, "hb", [128, D], BF16, 2)
        hT_r = kb.ring(es, "hT", [128, 8, 512], BF16, 2)
        tab_r = {k: kb.ring(es, "tab_" + k, [tabs[k].shape[0], 512], F32, 1) for k in tabs}
        sg_r = kb.ring(es, "sgt", [128, 512], F32, 2)
        vT_r = kb.ring(es, "vTt", [128, 4, 512], BF16, 1)
        qT_r = kb.ring(es, "qTt", [128, 4, 512], BF16, 1)
        kT_r = kb.ring(es, "kTt", [128, 512], BF16, 2)
        pT_r = kb.ring(es, "pTt", [128, 4, 512], BF16, 1)
        t1_r = kb.ring(es, "t1", [128, 512], F32, 3)
        t2_r = kb.ring(es, "t2", [128, 512], F32, 3)
        vtm_r = kb.ring(es, "vtm", [128, 4, 128], BF16, 2)
        sq_r = kb.ring(es, "sq", [128, 3, 512], BF16, 1)
        rq_r = kb.ring(es, "rq", [128, 2, 512], F32, 1)
        cqn_r = kb.ring(es, "cqn", [128, 2, 512], BF16, 2)
        ckvn_r = kb.ring(es, "ckvn", [128, 512], BF16, 2)
        krT_r = kb.ring(es, "krT", [32, 512], BF16, 2)
        qm_r = kb.ring(es, "qm", [96, 8, 512], BF16, 1)
        kn_r = kb.ring(es, "kn", [64, 8, 512], BF16, 1)
        vm_r = kb.ring(es, "vmt", [128, 4, 512], BF16, 1)
        HT, HTb = dr["HT"]
        VT, VTb = dr["VT"]
        QT, QTb = dr["QT"]
        KT, KTb = dr["KT"]
        Vd, Vdb = dr["V"]
        PT, PTb = dr["PT"]
        QM, QMb = dr["QM"]
        KN, KNb = dr["KN"]
        KR, KRb = dr["KR"]
        VM, VMb = dr["VM"]

        def proj(c0, m, hT, hTB, n, w=wext, wB=wextB, nk=8):
            p, pB = kb.ps()
            for k in range(nk):
                f.pe.op(lambda: nc.tensor.matmul(p[0:m, 0:n], lhsT=w[:, k, c0:c0 + m], rhs=hT[:, k, 0:n],
                                                 start=(k == 0), stop=(k == nk - 1)), reads=[wB, hTB], writes=[pB])
            return p, pB

        for ti, (t0, n, b, isctx, pos) in enumerate(tiles_all()):
            r = 2 if isctx else b
            sc, scB, sh, shB = mods[r]
            hT, hTB = hT_r[ti % 2]
            nsub = n // 128
            for j in range(nsub):
                it = ti * 4 + j
                xs, xsB = xs_r[it % len(xs_r)]
                st, stB = st_r[it % len(st_r)]
                xn, xnB = xn_r[it % 2]
                hb, hbB = hb_r[it % 2]
                f.q_sp.dma(xs, xsrc(t0 + j * 128, 128), writes=[xsB])
                mean, rstd = ln_tile(kb, xs, xsB, st, stB)
                f.dve.op(lambda: nc.vector.tensor_scalar(out=xn, in0=xs, scalar1=mean, scalar2=rstd, op0=ALU.subtract, op1=ALU.mult),
                         reads=[xsB, stB], writes=[xnB])
                f.pool.op(lambda: nc.gpsimd.tensor_tensor(out=xn, in0=xn, in1=sc, op=ALU.mult), reads=[xnB, scB], writes=[xnB])
                f.dve.op(lambda: nc.vector.tensor_tensor(out=hb, in0=xn, in1=sh, op=ALU.add), reads=[xnB, shB], writes=[hbB])
                pt, ptB = kb.pb()
                for k in range(8):
                    f.pe.op(lambda: nc.tensor.transpose(pt[:, k * 128:(k + 1) * 128], hb[:, k * 128:(k + 1) * 128], ident),
                            reads=[hbB, identB], writes=[ptB])
                f.act.op(lambda: nc.scalar.copy(out=hT[:, :, j * 128:(j + 1) * 128], in_=pt.rearrange("p (k t) -> p k t", k=8)),
                         reads=[ptB], writes=[hTB])
            f.q_sp.dma(HT.rearrange("(k p) t -> p k t", p=128)[:, :, t0:t0 + n], hT[:, :, 0:n], reads=[hTB], writes=[HTb])
            tb = {}
            if not isctx:
                for k in tabs:
                    ta, taB = tab_r[k][0]
                    f.q_sp.dma(ta[:, 0:n], tabs[k][:, pos:pos + n], writes=[taB])
                    tb[k] = (ta, taB)
            vT, vTB = vT_r[ti % len(vT_r)]
            for cc in range(4):
                pg, pgB = proj(512 + cc * 128, 128, hT, hTB, n)
                sg, sgB = sg_r[cc % 2]
                f.act.op(lambda: nc.scalar.activation(out=sg[:, 0:n], in_=pg[:, 0:n], func=AF.Sigmoid), reads=[pgB], writes=[sgB])
                pv, pvB = proj(cc * 128, 128, hT, hTB, n)
                f.dve.op(lambda: nc.vector.tensor_tensor(out=vT[:, cc, 0:n], in0=pv[:, 0:n], in1=sg[:, 0:n], op=ALU.mult),
                         reads=[pvB, sgB], writes=[vTB])
            f.q_sp.dma(VT.rearrange("(k p) t -> p k t", p=128)[:, :, t0:t0 + n], vT[:, :, 0:n], reads=[vTB], writes=[VTb])

            def roped(c0, cr0, m, dst, dstB, ck, sk, ii):
                p1, p1B = proj(c0, m, hT, hTB, n)
                if isctx:
                    f.act.op(lambda: nc.scalar.copy(out=dst, in_=p1[0:m, 0:n]), reads=[p1B], writes=[dstB])
                    return
                p2, p2B = proj(cr0, m, hT, hTB, n)
                t1, t1B = t1_r[ii % 3]
                t2, t2B = t2_r[ii % 3]
                co, coB = tb[ck]
                si, siB = tb[sk]
                f.dve.op(lambda: nc.vector.tensor_tensor(out=t1[0:m, 0:n], in0=p1[0:m, 0:n], in1=co[0:m, 0:n], op=ALU.mult),
                         reads=[p1B, coB], writes=[t1B])
                f.dve.op(lambda: nc.vector.tensor_tensor(out=t2[0:m, 0:n], in0=p2[0:m, 0:n], in1=si[0:m, 0:n], op=ALU.mult),
                         reads=[p2B, siB], writes=[t2B])
                f.pool.op(lambda: nc.gpsimd.tensor_tensor(out=dst, in0=t1[0:m, 0:n], in1=t2[0:m, 0:n], op=ALU.add),
                          reads=[t1B, t2B], writes=[dstB])
            qT, qTB = qT_r[ti % len(qT_r)]
            for cc in range(4):
                roped(Q0 + cc * 128, QR0 + cc * 128, 128, qT[:, cc, 0:n], qTB, "c64", "s64", cc)
            f.q_sp.dma(QT.rearrange("(k p) t -> p k t", p=128)[:, :, t0:t0 + n], qT[:, :, 0:n], reads=[qTB], writes=[QTb])
            kT, kTB = kT_r[ti % 2]
            roped(K0, KRT0, 128, kT[:, 0:n], kTB, "c64", "s64", 4)
            f.q_sp.dma(KT[:, t0:t0 + n], kT[:, 0:n], reads=[kTB], writes=[KTb])
            krT, krTB = krT_r[ti % 2]
            roped(KR0, KRR0, 32, krT[:, 0:n], krTB, "c32", "s32", 5)
            f.q_sp.dma(KR[:, t0:t0 + n], krT[:, 0:n], reads=[krTB], writes=[KRb])
            vtm, vtmB = vtm_r[ti % 2]
            for j in range(nsub):
                p, pB = kb.ps()
                for k in range(8):
                    f.pe.op(lambda: nc.tensor.matmul(p[:, 0:128], lhsT=hT[:, k, j * 128:(j + 1) * 128], rhs=wext[:, k, V0:V0 + 128],
                                                     start=(k == 0), stop=(k == 7)), reads=[hTB, wextB], writes=[pB])
                f.act.op(lambda: nc.scalar.copy(out=vtm[:, j, :], in_=p[:, 0:128]), reads=[pB], writes=[vtmB])
            f.q_sp.dma(Vd[t0:t0 + n, :].rearrange("(j p) c -> p j c", p=128), vtm[:, 0:nsub, :], reads=[vtmB], writes=[Vdb])
            pT, pTB = pT_r[ti % len(pT_r)]
            for cc in range(4):
                p, pB = proj(P0_ + cc * 128, 128, hT, hTB, n)
                f.act.op(lambda: nc.scalar.copy(out=pT[:, cc, 0:n], in_=p[:, 0:n]), reads=[pB], writes=[pTB])
            f.q_sp.dma(PT.rearrange("(k p) t -> p k t", p=128)[:, :, t0:t0 + n], pT[:, :, 0:n], reads=[pTB], writes=[PTb])
            pcs = [proj(CQ0, 128, hT, hTB, n), proj(CQ0 + 128, 128, hT, hTB, n), proj(CKV0, 128, hT, hTB, n)]
            sq, sqB = sq_r[ti % len(sq_r)]
            for i3 in range(3):
                f.act.op(lambda: nc.scalar.activation(out=sq[:, i3, 0:n], in_=pcs[i3][0][:, 0:n], func=AF.Square),
                         reads=[pcs[i3][1]], writes=[sqB])
            rq, rqB = rq_r[ti % len(rq_r)]
            for i2, (ks, div) in enumerate((((0, 1), 256.0), ((2,), 128.0))):
                pss, pssB = kb.ps()
                for ii, k in enumerate(ks):
                    f.pe.op(lambda: nc.tensor.matmul(pss[:, 0:n], lhsT=onesb, rhs=sq[:, k, 0:n], start=(ii == 0), stop=(ii == len(ks) - 1)),
                            reads=[onesbB, sqB], writes=[pssB])
                f.dve.op(lambda: nc.vector.tensor_scalar(out=rq[:, i2, 0:n], in0=pss[:, 0:n], scalar1=1.0 / div, scalar2=EPS,
                                                         op0=ALU.mult, op1=ALU.add), reads=[pssB], writes=[rqB])
                f.pool.op(lambda: nc.gpsimd.tensor_tensor(out=rq[:, i2, 0:n], in0=rq[:, i2, 0:n], in1=nh[:, 0:n], op=ALU.pow),
                          reads=[rqB, nhB], writes=[rqB])
            cqn, cqnB = cqn_r[ti % 2]
            for k in range(2):
                f.dve.op(lambda: nc.vector.scalar_tensor_tensor(out=cqn[:, k, 0:n], in0=pcs[k][0][:, 0:n], scalar=gq[:, k:k + 1],
                                                                in1=rq[:, 0, 0:n], op0=ALU.mult, op1=ALU.mult),
                         reads=[pcs[k][1], gqB, rqB], writes=[cqnB])
            ckvn, ckvnB = ckvn_r[ti % 2]
            f.dve.op(lambda: nc.vector.scalar_tensor_tensor(out=ckvn[:, 0:n], in0=pcs[2][0][:, 0:n], scalar=gkv[:, 0:1],
                                                            in1=rq[:, 1, 0:n], op0=ALU.mult, op1=ALU.mult),
                     reads=[pcs[2][1], gkvB, rqB], writes=[ckvnB])
            qm, qmB = qm_r[ti % len(qm_r)]
            for h in range(8):
                p1, p1B = proj(h * 96, 96, cqn, cqnB, n, w=wuq, wB=wuqB, nk=2)
                if isctx:
                    f.act.op(lambda: nc.scalar.copy(out=qm[:, h, 0:n], in_=p1[0:96, 0:n]), reads=[p1B], writes=[qmB])
                else:
                    p2, p2B = proj(h * 96, 96, cqn, cqnB, n, w=wuqr, wB=wuqrB, nk=2)
                    t1, t1B = t1_r[h % 3]
                    t2, t2B = t2_r[h % 3]
                    co, coB = tb["c96"]
                    si, siB = tb["s96"]
                    f.dve.op(lambda: nc.vector.tensor_tensor(out=t1[0:96, 0:n], in0=p1[0:96, 0:n], in1=co[0:96, 0:n], op=ALU.mult),
                             reads=[p1B, coB], writes=[t1B])
                    f.dve.op(lambda: nc.vector.tensor_tensor(out=t2[0:96, 0:n], in0=p2[0:96, 0:n], in1=si[0:96, 0:n], op=ALU.mult),
                             reads=[p2B, siB], writes=[t2B])
                    f.pool.op(lambda: nc.gpsimd.tensor_tensor(out=qm[:, h, 0:n], in0=t1[0:96, 0:n], in1=t2[0:96, 0:n], op=ALU.add),
                              reads=[t1B, t2B], writes=[qmB])
            f.q_sp.dma(QM.rearrange("h r t -> r h t")[:, :, t0:t0 + n], qm[:, :, 0:n], reads=[qmB], writes=[QMb])
            kn, knB = kn_r[ti % len(kn_r)]
            for h in range(8):
                p, pB = kb.ps()
                f.pe.op(lambda: nc.tensor.matmul(p[0:64, 0:n], lhsT=wuk[:, h * 64:(h + 1) * 64], rhs=ckvn[:, 0:n], start=True, stop=True),
                        reads=[wukB, ckvnB], writes=[pB])
                f.act.op(lambda: nc.scalar.copy(out=kn[:, h, 0:n], in_=p[0:64, 0:n]), reads=[pB], writes=[knB])
            f.q_sp.dma(KN.rearrange("h r t -> r h t")[:, :, t0:t0 + n], kn[:, :, 0:n], reads=[knB], writes=[KNb])
            vm, vmB = vm_r[ti % len(vm_r)]
            for j in range(nsub):
                p, pB = kb.ps()
                f.pe.op(lambda: nc.tensor.matmul(p[:, 0:512], lhsT=ckvn[:, j * 128:(j + 1) * 128], rhs=wuv, start=True, stop=True),
                        reads=[ckvnB, wuvB], writes=[pB])
                f.act.op(lambda: nc.scalar.copy(out=vm[:, j, :], in_=p[:, 0:512]), reads=[pB], writes=[vmB])
            f.q_sp.dma(VM[t0:t0 + n, :].rearrange("(j p) c -> p j c", p=128), vm[:, 0:nsub, :], reads=[vmB], writes=[VMb])
    f.barrier()


def segs():
    out = []
    for b in range(NB):
        out.append((b * S, L, b, False))
        out.append((b * S + L, C, b, True))
    return out


def seg_tiles():
    out = []
    for (s0, sl, b, isctx) in segs():
        n = 512 if not isctx else 256
        for i in range(sl // n):
            out.append((s0 + i * n, n, s0, s0 + sl, b, isctx))
    return out


def load_halo(kb, dst, dstB, src3, t0, n, halo, lo_lim, hi_lim):
    nc, f = kb.nc, kb.f
    lo = max(t0 - halo, lo_lim)
    hi = min(t0 + n + halo, hi_lim)
    off = lo - (t0 - halo)
    if off > 0:
        f.pool.op(lambda: nc.gpsimd.memset(dst[:, :, 0:off], 0.0), writes=[dstB])
    if off + (hi - lo) < n + 2 * halo:
        f.pool.op(lambda: nc.gpsimd.memset(dst[:, :, off + (hi - lo):n + 2 * halo], 0.0), writes=[dstB])
    f.q_sp.dma(dst[:, :, off:off + (hi - lo)], src3[:, :, lo:hi], writes=[dstB])


def phase_conv(kb, l, conv_w, conv_b, ln_g, ln_b):
    nc, f = kb.nc, kb.f
    VT, VTb = kb.dram["VT"]
    YA, YAb = kb.dram["YA"]
    onesf, onesfB = kb.c["onesf"]
    nh, nhB = kb.c["neghalf"]
    with ExitStack() as es:
        cw, cwB = kb.sb(es, "cw", [128, 4, 31], F32)
        for k in range(4):
            f.q_sp.dma(cw[:, k, :], conv_w[l][:, k * 128:(k + 1) * 128].rearrange("j p -> p j"), writes=[cwB])
        prm, prmB = kb.sb(es, "cprm", [128, 3, 4], F32)
        for i, a in enumerate((conv_b, ln_g, ln_b)):
            f.q_sp.dma(prm[:, i, :], a[l].rearrange("(k p) -> p k", p=128), writes=[prmB])
        ve_r = kb.ring(es, "vext", [128, 4, 512 + 30], BF16, 2)
        y_r = kb.ring(es, "cy", [128, 4, 512], F32, 2)
        sq_r = kb.ring(es, "csq", [128, 4, 512], F32, 1)
        mean_r = kb.ring(es, "cmean", [128, 512], F32, 2)
        rstd_r = kb.ring(es, "crstd", [128, 512], F32, 2)
        z_r = kb.ring(es, "cz", [128, 512], F32, 2)
        ya_r = kb.ring(es, "cya", [128, 4, 512], BF16, 2)
        VT3 = VT.rearrange("(k p) t -> p k t", p=128)
        YA3 = YA.rearrange("(k p) t -> p k t", p=128)
        for ti, (t0, n, slo, shi, b, isctx) in enumerate(seg_tiles()):
            ve, veB = ve_r[ti % 2]
            load_halo(kb, ve, veB, VT3, t0, n, 15, slo, shi)
            y, yB = y_r[ti % 2]
            for k in range(4):
                f.dve.op(lambda: nc.vector.tensor_scalar(out=y[:, k, 0:n], in0=ve[:, k, 0:n], scalar1=cw[:, k, 0:1], scalar2=prm[:, 0, k:k + 1],
                                                         op0=ALU.mult, op1=ALU.add), reads=[veB, cwB, prmB], writes=[yB])
                for j in range(1, 31):
                    f.dve.op(lambda: nc.vector.scalar_tensor_tensor(out=y[:, k, 0:n], in0=ve[:, k, j:j + n], scalar=cw[:, k, j:j + 1],
                                                                    in1=y[:, k, 0:n], op0=ALU.mult, op1=ALU.add),
                             reads=[veB, cwB, yB], writes=[yB])
            sq, sqB = sq_r[0]
            f.act.op(lambda: nc.scalar.activation(out=sq[:, :, 0:n], in_=y[:, :, 0:n], func=AF.Square), reads=[yB], writes=[sqB])
            p1, p1B = kb.ps()
            p2, p2B = kb.ps()
            for k in range(4):
                f.pe.op(lambda: nc.tensor.matmul(p1[:, 0:n], lhsT=onesf, rhs=y[:, k, 0:n], start=(k == 0), stop=(k == 3)),
                        reads=[onesfB, yB], writes=[p1B])
            for k in range(4):
                f.pe.op(lambda: nc.tensor.matmul(p2[:, 0:n], lhsT=onesf, rhs=sq[:, k, 0:n], start=(k == 0), stop=(k == 3)),
                        reads=[onesfB, sqB], writes=[p2B])
            mean, meanB = mean_r[ti % 2]
            rstd, rstdB = rstd_r[ti % 2]
            f.act.op(lambda: nc.scalar.mul(out=mean[:, 0:n], in_=p1[:, 0:n], mul=1.0 / 512), reads=[p1B], writes=[meanB])
            f.dve.op(lambda: nc.vector.tensor_tensor(out=rstd[:, 0:n], in0=mean[:, 0:n], in1=mean[:, 0:n], op=ALU.mult),
                     reads=[meanB], writes=[rstdB])
            f.dve.op(lambda: nc.vector.scalar_tensor_tensor(out=rstd[:, 0:n], in0=p2[:, 0:n], scalar=1.0 / 512, in1=rstd[:, 0:n],
                                                            op0=ALU.mult, op1=ALU.subtract), reads=[p2B, rstdB], writes=[rstdB])
            f.dve.op(lambda: nc.vector.tensor_scalar(out=rstd[:, 0:n], in0=rstd[:, 0:n], scalar1=EPS, scalar2=None, op0=ALU.add),
                     reads=[rstdB], writes=[rstdB])
            f.pool.op(lambda: nc.gpsimd.tensor_tensor(out=rstd[:, 0:n], in0=rstd[:, 0:n], in1=nh[:, 0:n], op=ALU.pow),
                      reads=[rstdB, nhB], writes=[rstdB])
            ya, yaB = ya_r[ti % 2]
            for k in range(4):
                z, zB = z_r[k % 2]
                f.dve.op(lambda: nc.vector.tensor_tensor(out=z[:, 0:n], in0=y[:, k, 0:n], in1=mean[:, 0:n], op=ALU.subtract),
                         reads=[yB, meanB], writes=[zB])
                f.pool.op(lambda: nc.gpsimd.tensor_tensor(out=z[:, 0:n], in0=z[:, 0:n], in1=rstd[:, 0:n], op=ALU.mult),
                          reads=[zB, rstdB], writes=[zB])
                f.act.op(lambda: nc.scalar.activation(out=ya[:, k, 0:n], in_=z[:, 0:n], func=AF.Silu, scale=prm[:, 1, k:k + 1],
                                                      bias=prm[:, 2, k:k + 1]), reads=[zB, prmB], writes=[yaB])
            f.q_sp.dma(YA3[:, :, t0:t0 + n], ya[:, :, 0:n], reads=[yaB], writes=[YAb])
    f.barrier()


def phase_pool(kb, l, pool_w, pool_scale):
    nc, f = kb.nc, kb.f
    PT, PTb = kb.dram["PT"]
    YC, YCb = kb.dram["YC"]
    rc_d = kb.dram["tab_rc"][0]
    with ExitStack() as es:
        pw, pwB = kb.sb(es, "pw", [128, 4, 128], BF16)
        f.q_pool.dma(pw, pool_w[l].rearrange("g c d -> c g d"), writes=[pwB])
        psc, pscB = kb.sb(es, "psc", [128, 4], F32)
        f.q_sp.dma(psc, pool_scale[l].rearrange("(k p) -> p k", p=128), writes=[pscB])
        rc, rcB = kb.sb(es, "rc", [128, 2, 4, 16], F32)
        f.q_sp.dma(rc.rearrange("p a g e -> p (a g e)"), rc_d, writes=[rcB])
        ue_r = kb.ring(es, "uext", [128, 4, 512 + 16], BF16, 2)
        A_r = kb.ring(es, "pA", [128, 512 + 16], F32, 2)
        B_r = kb.ring(es, "pB", [128, 512 + 16], F32, 2)
        pm_r = kb.ring(es, "ppm", [128, 512], BF16, 2)
        pmf_r = kb.ring(es, "ppmf", [128, 16], F32, 2)
        yc_r = kb.ring(es, "pyc", [128, 4, 512], BF16, 2)
        PT3 = PT.rearrange("(k p) t -> p k t", p=128)
        YC3 = YC.rearrange("(k p) t -> p k t", p=128)
        it = 0
        for ti, (t0, n, slo, shi, b, isctx) in enumerate(seg_tiles()):
            ue, ueB = ue_r[ti % 2]
            load_halo(kb, ue, ueB, PT3, t0, n, 8, slo, shi)
            yc, ycB = yc_r[ti % 2]
            E = n + 16
            ai = 1 if isctx else 0
            for g in range(4):
                w = 2 << g
                A, AB = A_r[it % 2]
                Bt, BB = B_r[it % 2]
                it += 1
                f.dve.op(lambda: nc.vector.tensor_tensor(out=A[:, 1:E], in0=ue[:, g, 0:E - 1], in1=ue[:, g, 1:E], op=ALU.add),
                         reads=[ueB], writes=[AB])
                s, sB = A, AB
                if g >= 1:
                    f.pool.op(lambda: nc.gpsimd.tensor_tensor(out=Bt[:, 2:E - 1], in0=A[:, 1:E - 2], in1=A[:, 3:E], op=ALU.add),
                              reads=[AB], writes=[BB])
                    s, sB = Bt, BB
                if g >= 2:
                    f.dve.op(lambda: nc.vector.tensor_tensor(out=A[:, 4:E - 3], in0=Bt[:, 2:E - 5], in1=Bt[:, 6:E - 1], op=ALU.add),
                             reads=[BB], writes=[AB])
                    s, sB = A, AB
                if g >= 3:
                    f.pool.op(lambda: nc.gpsimd.tensor_tensor(out=Bt[:, 8:E - 7], in0=A[:, 4:E - 11], in1=A[:, 12:E - 3], op=ALU.add),
                              reads=[AB], writes=[BB])
                    s, sB = Bt, BB
                pm, pmB = pm_r[it % 2]
                f.dve.op(lambda: nc.vector.scalar_tensor_tensor(out=pm[:, 0:n], in0=s[:, 8:8 + n], scalar=1.0 / w, in1=ue[:, g, 8:8 + n],
                                                                op0=ALU.mult, op1=ALU.subtract), reads=[sB, ueB], writes=[pmB])
                pmf, pmfB = pmf_r[it % 2]
                if t0 == slo:
                    f.dve.op(lambda: nc.vector.tensor_tensor(out=pmf[:, 0:8], in0=s[:, 8:16], in1=rc[:, ai, g, 0:8], op=ALU.mult),
                             reads=[sB, rcB], writes=[pmfB])
                    f.dve.op(lambda: nc.vector.tensor_tensor(out=pm[:, 0:8], in0=pmf[:, 0:8], in1=ue[:, g, 8:16], op=ALU.subtract),
                             reads=[pmfB, ueB], writes=[pmB])
                if t0 + n == shi:
                    f.dve.op(lambda: nc.vector.tensor_tensor(out=pmf[:, 8:16], in0=s[:, n:n + 8], in1=rc[:, ai, g, 8:16], op=ALU.mult),
                             reads=[sB, rcB], writes=[pmfB])
                    f.dve.op(lambda: nc.vector.tensor_tensor(out=pm[:, n - 8:n], in0=pmf[:, 8:16], in1=ue[:, g, n:n + 8], op=ALU.subtract),
                             reads=[pmfB, ueB], writes=[pmB])
                p, pB = kb.ps()
                f.pe.op(lambda: nc.tensor.matmul(p[:, 0:n], lhsT=pw[:, g, :], rhs=pm[:, 0:n], start=True, stop=True),
                        reads=[pwB, pmB], writes=[pB])
                f.dve.op(lambda: nc.vector.tensor_scalar(out=yc[:, g, 0:n], in0=p[:, 0:n], scalar1=psc[:, g:g + 1], scalar2=None, op0=ALU.mult),
                         reads=[pB, pscB], writes=[ycB])
            f.q_sp.dma(YC3[:, :, t0:t0 + n], yc[:, :, 0:n], reads=[ycB], writes=[YCb])
    f.barrier()


def phase_swa(kb, l, swa_sink):
    nc, f = kb.nc, kb.f
    QT, QTb = kb.dram["QT"]
    KT, KTb = kb.dram["KT"]
    Vd, Vdb = kb.dram["V"]
    YB, YBb = kb.dram["YB"]
    ident, identB = kb.c["ident"]
    scale = 64 ** -0.5
    with ExitStack() as es:
        mL, mLB = kb.sb(es, "mL", [128, 4, 128], BF16)
        mR, mRB = kb.sb(es, "mR", [128, 4, 128], BF16)
        for m, mB, cm, st in ((mL, mLB, 1, -1), (mR, mRB, -1, 1)):
            f.pool.op(lambda: nc.gpsimd.memset(m, 1.0), writes=[mB])
            f.pool.op(lambda: nc.gpsimd.affine_select(out=m, in_=m, pattern=[[0, 4], [st, 128]], compare_op=ALU.is_ge, fill=0.0,
                                                      base=0, channel_multiplier=cm), reads=[mB], writes=[mB])
        esk, eskB = kb.sb(es, "esk", [128, 8], F32)
        f.q_sp.dma(esk, swa_sink[l:l + 1, :].partition_broadcast(128), writes=[eskB])
        f.act.op(lambda: nc.scalar.activation(out=esk, in_=esk, func=AF.Exp), reads=[eskB], writes=[eskB])
        qg_r = kb.ring(es, "qg", [64, 4, S], BF16, 2)
        kj_r = kb.ring(es, "kj", [64, S], BF16, 2)
        vj_r = kb.ring(es, "vj", [128, 34, 65], BF16, 2)
        pT_r = kb.ring(es, "spT", [128, 512], BF16, 6)
        den_r = kb.ring(es, "sden", [128, 8], F32, 2)
        yb_r = kb.ring(es, "syb", [128, 256], BF16, 2)
        ybT_r = kb.ring(es, "sybT", [128, 2, 512], BF16, 2)
        for vj, vjB in vj_r:
            f.pool.op(lambda: nc.gpsimd.memset(vj[:, :, 64:65], 1.0), writes=[vjB])
        ip = 0
        ig = 0
        for b in range(NB):
            for j in range(2):
                it = b * 2 + j
                qg, qgB = qg_r[it % 2]
                kj, kjB = kj_r[it % 2]
                vj, vjB = vj_r[it % 2]
                f.q_sp.dma(qg, QT[j * 256:(j + 1) * 256, b * S:(b + 1) * S].rearrange("(h d) t -> d h t", d=64), writes=[qgB])
                f.q_sp.dma(kj, KT[j * 64:(j + 1) * 64, b * S:(b + 1) * S], writes=[kjB])
                f.q_sp.dma(vj[:, :, 0:64], Vd[b * S:(b + 1) * S, j * 64:(j + 1) * 64].rearrange("(c p) d -> p c d", p=128), writes=[vjB])
                for i in range(34):
                    if i < 32:
                        chunks = [c for c in (i - 1, i, i + 1) if 0 <= c < 32] + [32, 33]
                    else:
                        chunks = [32, 33]
                    po, poB, pidx = kb.pin()
                    pts = []
                    for c in chunks:
                        ps, psB = kb.ps()
                        f.pe.op(lambda: nc.tensor.matmul(ps[:, 0:512], lhsT=kj[:, c * 128:(c + 1) * 128], rhs=qg[:, :, i * 128:(i + 1) * 128],
                                                         start=True, stop=True), reads=[kjB, qgB], writes=[psB])
                        pT, pTB = pT_r[ip % 6]
                        ip += 1
                        f.act.op(lambda: nc.scalar.activation(out=pT, in_=ps[:, 0:512], func=AF.Exp, scale=scale), reads=[psB], writes=[pTB])
                        if i < 32 and c == i - 1:
                            f.pool.op(lambda: nc.gpsimd.tensor_tensor(out=pT, in0=pT, in1=mL.rearrange("p h q -> p (h q)"), op=ALU.mult),
                                      reads=[pTB, mLB], writes=[pTB])
                        if i < 31 and c == i + 1:
                            f.pool.op(lambda: nc.gpsimd.tensor_tensor(out=pT, in0=pT, in1=mR.rearrange("p h q -> p (h q)"), op=ALU.mult),
                                      reads=[pTB, mRB], writes=[pTB])
                        pts.append((pT, pTB, c))
                    for hh in range(4):
                        for ci, (pT, pTB, c) in enumerate(pts):
                            f.pe.op(lambda: nc.tensor.matmul(po[:, hh * 65:(hh + 1) * 65], lhsT=pT[:, hh * 128:(hh + 1) * 128], rhs=vj[:, c, :],
                                                             start=(ci == 0), stop=(ci == len(pts) - 1)), reads=[pTB, vjB], writes=[poB])
                    den, denB = den_r[ig % 2]
                    po3 = po[:, 0:260].rearrange("p (h c) -> p h c", c=65)
                    f.dve.op(lambda: nc.vector.tensor_tensor(out=den[:, 0:4], in0=po3[:, :, 64], in1=esk[:, 4 * j:4 * j + 4], op=ALU.add),
                             reads=[poB, eskB], writes=[denB])
                    f.dve.op(lambda: nc.vector.reciprocal(out=den[:, 4:8], in_=den[:, 0:4]), reads=[denB], writes=[denB])
                    yb, ybB = yb_r[ig % 2]
                    for hh in range(4):
                        f.dve.op(lambda: nc.vector.tensor_scalar(out=yb[:, hh * 64:(hh + 1) * 64], in0=po[:, hh * 65:hh * 65 + 64],
                                                                 scalar1=den[:, 4 + hh:5 + hh], scalar2=None, op0=ALU.mult),
                                 reads=[poB, denB], writes=[ybB])
                    kb.unpin(pidx)
                    grp0 = (i // 4) * 4 if i < 32 else 32
                    gsz = 4 if i < 32 else 2
                    ybT, ybTB = ybT_r[(it * 9 + (i // 4)) % 2]
                    pt, ptB = kb.pb()
                    for k in range(2):
                        f.pe.op(lambda: nc.tensor.transpose(pt[:, k * 128:(k + 1) * 128], yb[:, k * 128:(k + 1) * 128], ident),
                                reads=[ybB, identB], writes=[ptB])
                    off = (i - grp0) * 128
                    f.act.op(lambda: nc.scalar.copy(out=ybT[:, :, off:off + 128], in_=pt[:, 0:256].rearrange("p (k t) -> p k t", k=2)),
                             reads=[ptB], writes=[ybTB])
                    if i - grp0 == gsz - 1:
                        tq0 = b * S + grp0 * 128
                        f.q_sp.dma(YB[j * 256:(j + 1) * 256, tq0:tq0 + gsz * 128].rearrange("(k p) t -> p k t", p=128),
                                   ybT[:, :, 0:gsz * 128], reads=[ybTB], writes=[YBb])
                    ig += 1
    f.barrier()


def phase_mla(kb, l):
    nc, f = kb.nc, kb.f
    QM, QMb = kb.dram["QM"]
    KN, KNb = kb.dram["KN"]
    KR, KRb = kb.dram["KR"]
    VM, VMb = kb.dram["VM"]
    YD, YDb = kb.dram["YD"]
    onesf, onesfB = kb.c["onesf"]
    scale = 96 ** -0.5
    with ExitStack() as es:
        kh_r = kb.ring(es, "kh", [96, S], BF16, 2)
        qh_r = kb.ring(es, "qh", [96, S], BF16, 2)
        vh_r = kb.ring(es, "vh", [128, 34, 65], BF16, 2)
        pT_r = kb.ring(es, "mpT", [128, 512], BF16, 4)
        rd_r = kb.ring(es, "mrd", [65, 512], F32, 2)
        on_r = kb.ring(es, "mon", [64, 512], F32, 2)
        yd_r = kb.ring(es, "myd", [64, 512], BF16, 2)
        for vh, vhB in vh_r:
            f.pool.op(lambda: nc.gpsimd.memset(vh[:, :, 64:65], 1.0), writes=[vhB])
        ip = 0
        iq = 0
        for b in range(NB):
            for h in range(8):
                it = b * 8 + h
                kh, khB = kh_r[it % 2]
                qh, qhB = qh_r[it % 2]
                vh, vhB = vh_r[it % 2]
                f.q_sp.dma(kh[0:64, :], KN[h, :, b * S:(b + 1) * S], writes=[khB])
                f.q_sp.dma(kh[64:96, :], KR[:, b * S:(b + 1) * S], writes=[khB])
                f.q_sp.dma(qh, QM[h, :, b * S:(b + 1) * S], writes=[qhB])
                f.q_sp.dma(vh[:, :, 0:64], VM[b * S:(b + 1) * S, h * 64:(h + 1) * 64].rearrange("(c p) d -> p c d", p=128), writes=[vhB])
                for qi in range(9):
                    q0 = qi * 512
                    n = 512 if qi < 8 else 256
                    chunks = list(range(34)) if qi < 8 else [32, 33]
                    po, poB, pidx = kb.pin()
                    pend = []
                    LAG = 2
                    for ci in range(len(chunks)):
                        c = chunks[ci]
                        ps, psB = kb.ps()
                        f.pe.op(lambda: nc.tensor.matmul(ps[:, 0:n], lhsT=kh[:, c * 128:(c + 1) * 128], rhs=qh[:, q0:q0 + n], start=True, stop=True),
                                reads=[khB, qhB], writes=[psB])
                        pT, pTB = pT_r[ip % 4]
                        ip += 1
                        f.act.op(lambda: nc.scalar.activation(out=pT[:, 0:n], in_=ps[:, 0:n], func=AF.Exp, scale=scale), reads=[psB], writes=[pTB])
                        pend.append((pT, pTB, c, ci))
                        if len(pend) > LAG:
                            pT2, pT2B, c2, ci2 = pend.pop(0)
                            f.pe.op(lambda: nc.tensor.matmul(po[0:65, 0:n], lhsT=vh[:, c2, :], rhs=pT2[:, 0:n], start=(ci2 == 0), stop=(ci2 == len(chunks) - 1)),
                                    reads=[vhB, pT2B], writes=[poB])
                    while pend:
                        pT2, pT2B, c2, ci2 = pend.pop(0)
                        f.pe.op(lambda: nc.tensor.matmul(po[0:65, 0:n], lhsT=vh[:, c2, :], rhs=pT2[:, 0:n], start=(ci2 == 0), stop=(ci2 == len(chunks) - 1)),
                                reads=[vhB, pT2B], writes=[poB])
                    rd, rdB = rd_r[iq % 2]
                    on, onB = on_r[iq % 2]
                    yd, ydB = yd_r[iq % 2]
                    iq += 1
                    f.dve.op(lambda: nc.vector.reciprocal(out=rd[64:65, 0:n], in_=po[64:65, 0:n]), reads=[poB], writes=[rdB])
                    f.act.op(lambda: nc.scalar.copy(out=on[:, 0:n], in_=po[0:64, 0:n]), reads=[poB], writes=[onB])
                    kb.unpin(pidx)
                    pbc, pbcB = kb.ps()
                    f.pe.op(lambda: nc.tensor.matmul(pbc[0:64, 0:n], lhsT=onesf[64:65, 0:64], rhs=rd[64:65, 0:n], start=True, stop=True),
                            reads=[onesfB, rdB], writes=[pbcB])
                    f.dve.op(lambda: nc.vector.tensor_tensor(out=yd[:, 0:n], in0=on[:, 0:n], in1=pbc[0:64, 0:n], op=ALU.mult),
                             reads=[onB, pbcB], writes=[ydB])
                    f.q_sp.dma(YD[h * 64:(h + 1) * 64, b * S + q0:b * S + q0 + n], yd[:, 0:n], reads=[ydB], writes=[YDb])
    f.barrier()


def resid_ln(kb, es_bufs, src_y, src_yB, ysrc_is_psum_halves, xs, xsB, gt, gtB, lg, lgB, lb, lbB, dst_ap, dstB, idx):
    nc, f = kb.nc, kb.f
    t_r, st_r, o_r = es_bufs
    t, tB = t_r[idx % len(t_r)]
    st, stB = st_r[idx % len(st_r)]
    o, oB = o_r[idx % len(o_r)]
    for hf in range(2):
        ya, yaB = src_y[hf]
        f.dve.op(lambda: nc.vector.tensor_tensor(out=t[:, hf * 512:(hf + 1) * 512], in0=ya, in1=gt[:, hf * 512:(hf + 1) * 512], op=ALU.mult),
                 reads=[yaB, gtB], writes=[tB])
    f.dve.op(lambda: nc.vector.scalar_tensor_tensor(out=t, in0=xs, scalar=ALPHA, in1=t, op0=ALU.mult, op1=ALU.add),
             reads=[xsB, tB], writes=[tB])
    mean, rstd = ln_tile(kb, t, tB, st, stB)
    f.dve.op(lambda: nc.vector.tensor_scalar(out=o, in0=t, scalar1=mean, scalar2=rstd, op0=ALU.subtract, op1=ALU.mult),
             reads=[tB, stB], writes=[oB])
    f.pool.op(lambda: nc.gpsimd.tensor_tensor(out=o, in0=o, in1=lg, op=ALU.mult), reads=[oB, lgB], writes=[oB])
    f.pool.op(lambda: nc.gpsimd.tensor_tensor(out=o, in0=o, in1=lb, op=ALU.add), reads=[oB, lbB], writes=[oB])
    f.q_sp.dma(dst_ap, o, reads=[oB], writes=[dstB])


def phase_merge(kb, l, xsrc, I, ADA, XA, skip_ctx=False):
    nc, f = kb.nc, kb.f
    HT, HTb = kb.dram["HT"]
    XAb = kb.dram["XA"][1]
    with ExitStack() as es:
        wg, wgB = kb.sb(es, "wg", [128, 4, 8, D], BF16)
        for i in range(4):
            f.q_pool.dma(wg[:, i], I["w_gate"][l, i].rearrange("(k p) c -> p k c", p=128), writes=[wgB])
        wb, wbB = kb.sb(es, "wbr", [128, 4, 4, D], BF16)
        for i in range(4):
            f.q_pool.dma(wb[:, i], I["w_branch"][l, i].rearrange("(k p) c -> p k c", p=128), writes=[wbB])
        wo, woB = kb.sb(es, "wo", [128, 8, D], BF16)
        f.q_pool.dma(wo, I["w_out"][l].rearrange("(k p) c -> p k c", p=128), writes=[woB])
        bg, bgB = kb.sb(es, "bg", [128, 4, 8], F32)
        for i in range(4):
            f.q_sp.dma(bg[:, i, :], I["b_gate"][l, i].rearrange("(k p) -> p k", p=128), writes=[bgB])
        g1 = []
        for r in range(3):
            g, gB = kb.sb(es, f"g1_{r}", [128, D], F32)
            load_bcast(kb, g, gB, ADA[l, r:r + 1, 2 * D:3 * D])
            g1.append((g, gB))
        lg, lgB = kb.sb(es, "ln1g", [128, D], F32)
        lb, lbB = kb.sb(es, "ln1b", [128, D], F32)
        load_bcast(kb, lg, lgB, I["ln1_g"][l:l + 1, :])
        load_bcast(kb, lb, lbB, I["ln1_b"][l:l + 1, :])
        hT_r = kb.ring(es, "mhT", [128, 8, 512], BF16, 1)
        ys_r = kb.ring(es, "mys", [128, 16, 512], BF16, 1)
        mT_r = kb.ring(es, "mmT", [128, 8, 512], BF16, 1)
        sg_r = kb.ring(es, "msg", [128, 512], F32, 2)
        tmp_r = kb.ring(es, "mtmp", [128, 512], F32, 2)
        acc_r = kb.ring(es, "macc", [128, 512], F32, 2)
        xs_r = kb.ring(es, "mxs", [128, D], F32, 2)
        rl = (kb.ring(es, "mt", [128, D], F32, 1), kb.ring(es, "mst", [128, 16], F32, 2), kb.ring(es, "mo", [128, D], F32, 2))
        HT3 = HT.rearrange("(k p) t -> p k t", p=128)
        idx = 0
        for ti, (t0, n, b, isctx, pos) in enumerate(tiles_all()):
            if isctx and skip_ctx:
                continue
            r = 2 if isctx else b
            hT, hTB = hT_r[0]
            ys, ysB = ys_r[0]
            mT, mTB = mT_r[0]
            f.q_sp.dma(hT[:, :, 0:n], HT3[:, :, t0:t0 + n], writes=[hTB])
            for i, nm in enumerate(("YA", "YB", "YC", "YD")):
                f.q_sp.dma(ys[:, i * 4:(i + 1) * 4, 0:n], kb.dram[nm][0].rearrange("(k p) t -> p k t", p=128)[:, :, t0:t0 + n], writes=[ysB])
            for oc in range(8):
                acc, accB = acc_r[oc % 2]
                for i in range(4):
                    pg, pgB = kb.ps()
                    for k in range(8):
                        f.pe.op(lambda: nc.tensor.matmul(pg[:, 0:n], lhsT=wg[:, i, k, oc * 128:(oc + 1) * 128], rhs=hT[:, k, 0:n],
                                                         start=(k == 0), stop=(k == 7)), reads=[wgB, hTB], writes=[pgB])
                    sg, sgB = sg_r[i % 2]
                    f.act.op(lambda: nc.scalar.activation(out=sg[:, 0:n], in_=pg[:, 0:n], func=AF.Sigmoid, bias=bg[:, i, oc:oc + 1]),
                             reads=[pgB, bgB], writes=[sgB])
                    pbr, pbrB = kb.ps()
                    for k in range(4):
                        f.pe.op(lambda: nc.tensor.matmul(pbr[:, 0:n], lhsT=wb[:, i, k, oc * 128:(oc + 1) * 128], rhs=ys[:, i * 4 + k, 0:n],
                                                         start=(k == 0), stop=(k == 3)), reads=[wbB, ysB], writes=[pbrB])
                    if i == 0:
                        f.dve.op(lambda: nc.vector.tensor_tensor(out=acc[:, 0:n], in0=sg[:, 0:n], in1=pbr[:, 0:n], op=ALU.mult),
                                 reads=[sgB, pbrB], writes=[accB])
                    else:
                        tmp, tmpB = tmp_r[i % 2]
                        f.dve.op(lambda: nc.vector.tensor_tensor(out=tmp[:, 0:n], in0=sg[:, 0:n], in1=pbr[:, 0:n], op=ALU.mult),
                                 reads=[sgB, pbrB], writes=[tmpB])
                        if i < 3:
                            f.pool.op(lambda: nc.gpsimd.tensor_tensor(out=acc[:, 0:n], in0=acc[:, 0:n], in1=tmp[:, 0:n], op=ALU.add),
                                      reads=[accB, tmpB], writes=[accB])
                        else:
                            f.pool.op(lambda: nc.gpsimd.tensor_tensor(out=mT[:, oc, 0:n], in0=acc[:, 0:n], in1=tmp[:, 0:n], op=ALU.add),
                                      reads=[accB, tmpB], writes=[mTB])
            g, gB = g1[r]
            for j in range(n // 128):
                xs, xsB = xs_r[idx % 2]
                f.q_sp.dma(xs, xsrc(t0 + j * 128, 128), writes=[xsB])
                halves = []
                for hf in range(2):
                    po, poB = kb.ps()
                    for k in range(8):
                        f.pe.op(lambda: nc.tensor.matmul(po[:, 0:512], lhsT=mT[:, k, j * 128:(j + 1) * 128], rhs=wo[:, k, hf * 512:(hf + 1) * 512],
                                                         start=(k == 0), stop=(k == 7)), reads=[mTB, woB], writes=[poB])
                    halves.append((po[:, 0:512], poB))
                resid_ln(kb, rl, halves, None, True, xs, xsB, g, gB, lg, lgB, lb, lbB, XA[t0 + j * 128:t0 + (j + 1) * 128, :], XAb, idx)
                idx += 1
    f.barrier()


NT128 = T // 128
BIGIDX = 4.0e6


def phase_moe(kb, l, I, ADA, XA, XB, out, last):
    nc, f = kb.nc, kb.f
    dr = kb.dram
    H2, H2b = dr["H2"]
    XG, XGb = dr["XG"]
    YG, YGb = dr["YG"]
    XAb = dr["XA"][1]
    ident, identB = kb.c["ident"]
    onesb, onesbB = kb.c["onesb"]
    onesf, onesfB = kb.c["onesf"]
    with ExitStack() as es0:
        TK, TKB = kb.sb(es0, "TK", [128, NT128, 12], F32)
        WK, WKB = kb.sb(es0, "WK", [128, NT128, 4], F32)
        DSTI, DSTIB = kb.sb(es0, "DSTI", [128, NT128, 4], I32)
        carry, carryB = kb.sb(es0, "carry", [128, 32], F32)
        eio, eioB = kb.sb(es0, "eio", [128, 32], F32)
        eioi, eioiB = kb.sb(es0, "eioi", [128, 32], I32)
        f.pool.op(lambda: nc.gpsimd.iota(eioi, pattern=[[1, 32]], base=0, channel_multiplier=0), writes=[eioiB])
        f.dve.op(lambda: nc.vector.tensor_copy(out=eio, in_=eioi), reads=[eioiB], writes=[eioB])
        f.pool.op(lambda: nc.gpsimd.memset(carry, 0.0), writes=[carryB])
        IDXG, IDXGB = kb.sb(es0, "IDXG", [128, NBLK], I32)
        BEI, BEIB = kb.sb(es0, "BEI", [128, NBLK], I32)
        IDX2, IDX2B = kb.sb(es0, "IDX2", [128, 2, NBLK], I32)
        BE, BEB = kb.sb(es0, "BE", [128, NBLK], F32)
        with ExitStack() as es:
            WBG, WBGb = dr["WBG"]
            WBD, WBDb = dr["WBD"]
            tg_r = kb.ring(es, "tg", [128, 8, 2048], BF16, 2)
            td_r = kb.ring(es, "td", [128, 8, 1024], BF16, 2)
            for e in range(NE):
                tg, tgB = tg_r[e % 2]
                f.q_pool.dma(tg, I["w_gu"][l, e].rearrange("(p k) c -> p k c", k=8), writes=[tgB])
                f.q_sp.dma(WBG[e * 128:(e + 1) * 128, :], tg.rearrange("p k c -> p (k c)"), reads=[tgB], writes=[WBGb])
                td, tdB = td_r[e % 2]
                f.q_pool.dma(td, I["w_down"][l, e].rearrange("(p k) c -> p k c", k=8), writes=[tdB])
                f.q_sp.dma(WBD[e * 128:(e + 1) * 128, :], td.rearrange("p k c -> p (k c)"), reads=[tdB], writes=[WBDb])
            U, UB = kb.sb(es, "U", [128, 128], BF16)
            f.pool.op(lambda: nc.gpsimd.memset(U, 1.0), writes=[UB])
            f.pool.op(lambda: nc.gpsimd.affine_select(out=U, in_=U, pattern=[[1, 128]], compare_op=ALU.is_gt, fill=0.0, base=0,
                                                      channel_multiplier=-1), reads=[UB], writes=[UB])
            wr, wrB = kb.sb(es, "wr", [128, 8, 32], BF16)
            f.q_pool.dma(wr, I["router_w"][l].rearrange("(k p) e -> p k e", p=128), writes=[wrB])
            rb, rbB = kb.sb(es, "rb", [1, 32], BF16)
            f.q_pool.dma(rb, I["router_b"][l:l + 1, :], writes=[rbB])
            mods = []
            for r in range(3):
                sc, scB = kb.sb(es, f"sc2_{r}", [128, D], F32)
                sh, shB = kb.sb(es, f"sh2_{r}", [128, D], F32)
                load_bcast(kb, sh, shB, ADA[l, r:r + 1, 3 * D:4 * D])
                load_bcast(kb, sc, scB, ADA[l, r:r + 1, 4 * D:5 * D])
                f.pool.op(lambda: nc.gpsimd.tensor_scalar(out=sc, in0=sc, scalar1=1.0, scalar2=None, op0=ALU.add), reads=[scB], writes=[scB])
                mods.append((sc, scB, sh, shB))
            xs_r = kb.ring(es, "rxs", [128, D], F32, 2)
            st_r = kb.ring(es, "rst", [128, 16], F32, 2)
            xn_r = kb.ring(es, "rxn", [128, D], F32, 2)
            hb_r = kb.ring(es, "rhb", [128, D], BF16, 2)
            hT_r = kb.ring(es, "rhT", [128, 8, 128], BF16, 2)
            lg_r = kb.ring(es, "rlg", [128, 32], F32, 2)
            t8_r = kb.ring(es, "rt8", [128, 16], F32, 2)
            M_r = kb.ring(es, "rM", [128, 32], BF16, 2)
            rk_r = kb.ring(es, "rrk", [128, 32], F32, 2)
            jk_r = kb.ring(es, "rjk", [128, 32], F32, 2)
            for it in range(NT128):
                t0 = it * 128
                b, o = divmod(t0, S)
                r = 2 if o >= L else b
                sc, scB, sh, shB = mods[r]
                xs, xsB = xs_r[it % 2]
                st, stB = st_r[it % 2]
                xn, xnB = xn_r[it % 2]
                hb, hbB = hb_r[it % 2]
                hT, hTB = hT_r[it % 2]
                f.q_sp.dma(xs, XA[t0:t0 + 128, :], writes=[xsB])
                mean, rstd = ln_tile(kb, xs, xsB, st, stB)
                f.dve.op(lambda: nc.vector.tensor_scalar(out=xn, in0=xs, scalar1=mean, scalar2=rstd, op0=ALU.subtract, op1=ALU.mult),
                         reads=[xsB, stB], writes=[xnB])
                f.pool.op(lambda: nc.gpsimd.tensor_tensor(out=xn, in0=xn, in1=sc, op=ALU.mult), reads=[xnB, scB], writes=[xnB])
                f.dve.op(lambda: nc.vector.tensor_tensor(out=hb, in0=xn, in1=sh, op=ALU.add), reads=[xnB, shB], writes=[hbB])
                f.q_sp.dma(H2[t0:t0 + 128, :], hb, reads=[hbB], writes=[H2b])
                pt, ptB = kb.pb()
                for k in range(8):
                    f.pe.op(lambda: nc.tensor.transpose(pt[:, k * 128:(k + 1) * 128], hb[:, k * 128:(k + 1) * 128], ident),
                            reads=[hbB, identB], writes=[ptB])
                f.act.op(lambda: nc.scalar.copy(out=hT, in_=pt.rearrange("p (k t) -> p k t", k=8)), reads=[ptB], writes=[hTB])
                pl, plB = kb.ps()
                for k in range(8):
                    f.pe.op(lambda: nc.tensor.matmul(pl[:, 0:32], lhsT=hT[:, k, :], rhs=wr[:, k, :], start=(k == 0), stop=False),
                            reads=[hTB, wrB], writes=[plB])
                f.pe.op(lambda: nc.tensor.matmul(pl[:, 0:32], lhsT=onesb[0:1, :], rhs=rb, start=False, stop=True),
                        reads=[onesbB, rbB], writes=[plB])
                lg, lgB = lg_r[it % 2]
                f.act.op(lambda: nc.scalar.copy(out=lg, in_=pl[:, 0:32]), reads=[plB], writes=[lgB])
                t8, t8B = t8_r[it % 2]
                f.dve.op(lambda: nc.vector.max(out=t8[:, 0:8], in_=lg), reads=[lgB], writes=[t8B])
                M, MB = M_r[it % 2]
                f.dve.op(lambda: nc.vector.tensor_scalar(out=M, in0=lg, scalar1=t8[:, 3:4], scalar2=None, op0=ALU.is_ge),
                         reads=[lgB, t8B], writes=[MB])
                pr, prB = kb.ps()
                f.pe.op(lambda: nc.tensor.matmul(pr[:, 0:32], lhsT=U, rhs=M, start=True, stop=True), reads=[UB, MB], writes=[prB])
                f.pe.op(lambda: nc.tensor.matmul(pr[:, 32:64], lhsT=onesb, rhs=M, start=True, stop=True), reads=[onesbB, MB], writes=[prB])
                rk, rkB = rk_r[it % 2]
                f.dve.op(lambda: nc.vector.tensor_tensor(out=rk, in0=pr[:, 0:32], in1=carry, op=ALU.add), reads=[prB, carryB], writes=[rkB])
                f.dve.op(lambda: nc.vector.tensor_tensor(out=carry, in0=pr[:, 32:64], in1=carry, op=ALU.add), reads=[prB, carryB], writes=[carryB])
                jk, jkB = jk_r[it % 2]
                for k in range(4):
                    f.dve.op(lambda: nc.vector.scalar_tensor_tensor(out=jk, in0=lg, scalar=t8[:, k:k + 1], in1=rk, op0=ALU.is_equal, op1=ALU.mult,
                                                                    accum_out=TK[:, it, 4 + k:5 + k]), reads=[lgB, t8B, rkB], writes=[jkB, TKB])
                    f.dve.op(lambda: nc.vector.scalar_tensor_tensor(out=jk, in0=lg, scalar=t8[:, k:k + 1], in1=eio, op0=ALU.is_equal, op1=ALU.mult,
                                                                    accum_out=TK[:, it, 8 + k:9 + k]), reads=[lgB, t8B, eioB], writes=[jkB, TKB])
                f.dve.op(lambda: nc.vector.tensor_scalar(out=t8[:, 8:9], in0=t8[:, 0:1], scalar1=-1.0, scalar2=None, op0=ALU.mult),
                         reads=[t8B], writes=[t8B])
                f.act.op(lambda: nc.scalar.activation(out=TK[:, it, 0:4], in_=t8[:, 0:4], func=AF.Exp, bias=t8[:, 8:9], accum_out=t8[:, 9:10]),
                         reads=[t8B], writes=[TKB, t8B])
                f.dve.op(lambda: nc.vector.reciprocal(out=t8[:, 10:11], in_=t8[:, 9:10]), reads=[t8B], writes=[t8B])
                f.dve.op(lambda: nc.vector.tensor_scalar(out=WK[:, it, :], in0=TK[:, it, 0:4], scalar1=t8[:, 10:11], scalar2=None, op0=ALU.mult),
                         reads=[TKB, t8B], writes=[WKB])
        f.barrier()
        with ExitStack() as es:
            MAXB = T // BLK + 1
            i256i, i256iB = kb.sb(es, "i256i", [128, NBLK], I32)
            i256, i256B = kb.sb(es, "i256", [128, NBLK], F32)
            f.pool.op(lambda: nc.gpsimd.iota(i256i, pattern=[[BLK, NBLK]], base=0, channel_multiplier=0), writes=[i256iB])
            f.dve.op(lambda: nc.vector.tensor_copy(out=i256, in_=i256i), reads=[i256iB], writes=[i256B])
            cmp, cmpB = kb.sb(es, "cmpA", [128, 32, MAXB], F32)
            f.dve.op(lambda: nc.vector.tensor_tensor(out=cmp, in0=carry.unsqueeze(2).to_broadcast([128, 32, MAXB]),
                                                     in1=i256[:, 0:MAXB].unsqueeze(1).to_broadcast([128, 32, MAXB]), op=ALU.is_gt),
                     reads=[carryB, i256B], writes=[cmpB])
            pe_a, pe_aB = kb.sb(es, "pend_a", [128, 32], F32)
            pe_b, pe_bB = kb.sb(es, "pend_b", [128, 32], F32)
            pst, pstB = kb.sb(es, "pstart", [128, 32], F32)
            pad, padB = kb.sb(es, "padded", [128, 32], F32)
            f.dve.op(lambda: nc.vector.reduce_sum(out=pad, in_=cmp, axis=AX.X), reads=[cmpB], writes=[padB])
            f.dve.op(lambda: nc.vector.tensor_scalar(out=pad, in0=pad, scalar1=float(BLK), scalar2=None, op0=ALU.mult), reads=[padB], writes=[padB])
            f.dve.op(lambda: nc.vector.tensor_copy(out=pe_a, in_=pad), reads=[padB], writes=[pe_aB])
            cur, curB, nxt, nxtB = pe_a, pe_aB, pe_b, pe_bB
            for s in (1, 2, 4, 8, 16):
                f.dve.op(lambda: nc.vector.tensor_copy(out=nxt[:, 0:s], in_=cur[:, 0:s]), reads=[curB], writes=[nxtB])
                f.dve.op(lambda: nc.vector.tensor_tensor(out=nxt[:, s:32], in0=cur[:, s:32], in1=cur[:, 0:32 - s], op=ALU.add),
                         reads=[curB], writes=[nxtB])
                cur, curB, nxt, nxtB = nxt, nxtB, cur, curB
            pend, pendB = cur, curB
            f.dve.op(lambda: nc.vector.tensor_tensor(out=pst, in0=pend, in1=pad, op=ALU.subtract), reads=[pendB, padB], writes=[pstB])
            cmp2, cmp2B = kb.sb(es, "cmpB", [128, NBLK, 32], F32)
            f.dve.op(lambda: nc.vector.tensor_tensor(out=cmp2, in0=pend.unsqueeze(1).to_broadcast([128, NBLK, 32]),
                                                     in1=i256.unsqueeze(2).to_broadcast([128, NBLK, 32]), op=ALU.is_le),
                     reads=[pendB, i256B], writes=[cmp2B])
            f.dve.op(lambda: nc.vector.reduce_sum(out=BE, in_=cmp2, axis=AX.X), reads=[cmp2B], writes=[BEB])
            f.dve.op(lambda: nc.vector.tensor_scalar(out=BE, in0=BE, scalar1=31.0, scalar2=None, op0=ALU.min), reads=[BEB], writes=[BEB])
            chg, chgB = kb.sb(es, "chg", [128, NBLK], F32)
            f.pool.op(lambda: nc.gpsimd.memset(chg[:, 0:1], 1.0), writes=[chgB])
            f.dve.op(lambda: nc.vector.tensor_tensor(out=chg[:, 1:NBLK], in0=BE[:, 1:NBLK], in1=BE[:, 0:NBLK - 1], op=ALU.not_equal),
                     reads=[BEB], writes=[chgB])
            base, baseB = kb.sb(es, "ibase", [128, NBLK], F32)
            f.dve.op(lambda: nc.vector.tensor_scalar(out=base, in0=BE, scalar1=128.0, scalar2=-BIGIDX, op0=ALU.mult, op1=ALU.add),
                     reads=[BEB], writes=[baseB])
            f.dve.op(lambda: nc.vector.tensor_tensor(out=base, in0=base, in1=chg, op=ALU.mult), reads=[baseB, chgB], writes=[baseB])
            pio_i, pio_iB = kb.sb(es, "pio_i", [128, 8], I32)
            pio, pioB = kb.sb(es, "pio", [128, 8], F32)
            f.pool.op(lambda: nc.gpsimd.iota(pio_i, pattern=[[128, 8]], base=0, channel_multiplier=1), writes=[pio_iB])
            f.dve.op(lambda: nc.vector.tensor_copy(out=pio, in_=pio_i), reads=[pio_iB], writes=[pioB])
            f.dve.op(lambda: nc.vector.tensor_scalar(out=base, in0=base, scalar1=BIGIDX, scalar2=None, op0=ALU.add), reads=[baseB], writes=[baseB])
            idxf, idxfB = kb.sb(es, "idxf", [128, NBLK], F32)
            f.dve.op(lambda: nc.vector.tensor_scalar(out=idxf, in0=base, scalar1=pio[:, 0:1], scalar2=None, op0=ALU.add),
                     reads=[baseB, pioB], writes=[idxfB])
            f.dve.op(lambda: nc.vector.tensor_copy(out=IDXG, in_=idxf), reads=[idxfB], writes=[IDXGB])
            idx2, idx2B = kb.sb(es, "idx2", [128, NBLK], F32)
            f.dve.op(lambda: nc.vector.tensor_scalar(out=idx2, in0=idxf, scalar1=2.0, scalar2=None, op0=ALU.mult), reads=[idxfB], writes=[idx2B])
            f.dve.op(lambda: nc.vector.tensor_copy(out=IDX2[:, 0, :], in_=idx2), reads=[idx2B], writes=[IDX2B])
            f.dve.op(lambda: nc.vector.tensor_scalar(out=idx2, in0=idx2, scalar1=1.0, scalar2=None, op0=ALU.add), reads=[idx2B], writes=[idx2B])
            f.dve.op(lambda: nc.vector.tensor_copy(out=IDX2[:, 1, :], in_=idx2), reads=[idx2B], writes=[IDX2B])
            f.dve.op(lambda: nc.vector.tensor_scalar(out=idxf, in0=base, scalar1=1.0 / 128.0, scalar2=float(l * NE), op0=ALU.mult, op1=ALU.add),
                     reads=[baseB], writes=[idxfB])
            f.dve.op(lambda: nc.vector.tensor_copy(out=BEI, in_=idxf), reads=[idxfB], writes=[BEIB])
            zt, ztB = kb.sb(es, "zt", [128, 8192], BF16)
            f.pool.op(lambda: nc.gpsimd.memset(zt, 0.0), writes=[ztB])
            XGf = XG.rearrange("(a p r) c -> a p (r c)", p=128, r=8)
            for a in range(NROWS // 1024):
                f.q_sp.dma(XGf[a], zt, reads=[ztB], writes=[XGb])
            dst, dstB = kb.sb(es, "dstf", [128, NT128, 4], F32)
            jk_r = kb.ring(es, "cjk", [128, 32], F32, 2)
            for it in range(NT128):
                jk, jkB = jk_r[it % 2]
                for k in range(4):
                    f.dve.op(lambda: nc.vector.scalar_tensor_tensor(out=jk, in0=eio, scalar=TK[:, it, 8 + k:9 + k], in1=pst, op0=ALU.is_equal,
                                                                    op1=ALU.mult, accum_out=dst[:, it, k:k + 1]),
                             reads=[eioB, TKB, pstB], writes=[jkB, dstB])
            f.dve.op(lambda: nc.vector.tensor_tensor(out=dst, in0=dst, in1=TK[:, :, 4:8], op=ALU.add), reads=[dstB, TKB], writes=[dstB])
            f.dve.op(lambda: nc.vector.tensor_copy(out=DSTI, in_=dst), reads=[dstB], writes=[DSTIB])
            f.barrier()
            hb_r = kb.ring(es, "shb", [128, D], BF16, 3)
            for it in range(NT128):
                hb, hbB = hb_r[it % 3]
                f.q_sp.dma(hb, H2[it * 128:(it + 1) * 128, :], writes=[hbB])
                for k in range(4):
                    f.q_pool.dma(None, None, reads=[hbB, DSTIB], writes=[XGb],
                                 fn=lambda g: g.indirect_dma_start(out=XG, out_offset=bass.IndirectOffsetOnAxis(ap=DSTI[:, it, k:k + 1], axis=0),
                                                                   in_=hb, in_offset=None, bounds_check=kb.breg(NROWS - 1), oob_is_err=False))
        f.barrier()
        with ExitStack() as es:
            wgu2, wguB = kb.sb(es, "wgu", [128, 8 * 2048], BF16)
            wdn2, wdnB = kb.sb(es, "wdn", [128, 8 * D], BF16)
            wgu = wgu2.rearrange("p (k c) -> p k c", k=8)
            wdn = wdn2.rearrange("p (k c) -> p k c", k=8)
            WBG = dr["WBG"][0]
            WBD = dr["WBD"][0]
            bgu, bguB = kb.sb(es, "bgu", [128, 2048], BF16)
            bdn, bdnB = kb.sb(es, "bdn", [128, D], BF16)
            WGU2 = I["w_gu"].rearrange("l e (q k) c -> (l e q) k c", k=4)
            WDN2 = I["w_down"].rearrange("l e (q k) c -> (l e q) k c", k=4)
            BGU2 = I["b_gu"].rearrange("l e c -> (l e) c")
            BDN2 = I["b_down"].rearrange("l e c -> (l e) c")
            xr_r = kb.ring(es, "bxr", [128, 2, D], BF16, 2)
            xT_r = kb.ring(es, "bxT", [128, 8, BLK], BF16, 2)
            aT_r = kb.ring(es, "baT", [128, 8, BLK], BF16, 2)
            g_r = kb.ring(es, "bg", [128, BLK], F32, 2)
            sg_r = kb.ring(es, "bsg", [128, BLK], F32, 2)
            u_r = kb.ring(es, "bu", [128, BLK], F32, 2)
            yo_r = kb.ring(es, "byo", [128, 2, D], F32, 2)
            for bi in range(NBLK):
                f.q_pool.dma(None, None, reads=[IDXGB], writes=[wguB],
                             fn=lambda g: g.indirect_dma_start(out=wgu2, out_offset=None, in_=WBG,
                                                               in_offset=bass.IndirectOffsetOnAxis(ap=IDXG[:, bi:bi + 1], axis=0),
                                                               bounds_check=kb.breg(NE * 128 - 1), oob_is_err=False))
                f.q_pool.dma(None, None, reads=[BEIB], writes=[bguB],
                             fn=lambda g: g.indirect_dma_start(out=bgu, out_offset=None, in_=BGU2,
                                                               in_offset=bass.IndirectOffsetOnAxis(ap=BEI[:, bi:bi + 1], axis=0),
                                                               bounds_check=kb.breg((l + 1) * NE - 1), oob_is_err=False))
                f.q_pool.dma(None, None, reads=[IDXGB], writes=[wdnB],
                             fn=lambda g: g.indirect_dma_start(out=wdn2, out_offset=None, in_=WBD,
                                                               in_offset=bass.IndirectOffsetOnAxis(ap=IDXG[:, bi:bi + 1], axis=0),
                                                               bounds_check=kb.breg(NE * 128 - 1), oob_is_err=False))
                f.q_pool.dma(None, None, reads=[BEIB], writes=[bdnB],
                             fn=lambda g: g.indirect_dma_start(out=bdn, out_offset=None, in_=BDN2,
                                                               in_offset=bass.IndirectOffsetOnAxis(ap=BEI[:, bi:bi + 1], axis=0),
                                                               bounds_check=kb.breg((l + 1) * NE - 1), oob_is_err=False))
                xr, xrB = xr_r[bi % 2]
                f.q_sp.dma(xr, XG[bi * BLK:(bi + 1) * BLK, :].rearrange("(j p) c -> p j c", p=128), writes=[xrB])
                xT, xTB = xT_r[bi % 2]
                for j in range(2):
                    pt, ptB = kb.pb()
                    for k in range(8):
                        f.pe.op(lambda: nc.tensor.transpose(pt[:, k * 128:(k + 1) * 128], xr[:, j, :].rearrange("r (p k) -> r k p", k=8)[:, k, :], ident),
                                reads=[xrB, identB], writes=[ptB])
                    f.act.op(lambda: nc.scalar.copy(out=xT[:, :, j * 128:(j + 1) * 128], in_=pt.rearrange("p (k t) -> p k t", k=8)),
                             reads=[ptB], writes=[xTB])
                aT, aTB = aT_r[bi % 2]
                for oc in range(8):
                    pz = []
                    for half in range(2):
                        p, pB = kb.ps()
                        for k in range(8):
                            wsl = wgu[:, k, half * 1024:(half + 1) * 1024].rearrange("p (m j) -> p j m", j=8)[:, oc, :]
                            f.pe.op(lambda: nc.tensor.matmul(p[:, 0:BLK], lhsT=wsl, rhs=xT[:, k, :], start=(k == 0), stop=False),
                                    reads=[wguB, xTB], writes=[pB])
                        bsl = bgu[0:1, half * 1024:(half + 1) * 1024].rearrange("p (m j) -> p j m", j=8)[:, oc, :]
                        f.pe.op(lambda: nc.tensor.matmul(p[:, 0:BLK], lhsT=bsl, rhs=kb.c["onesrow"][0][0:1, 0:BLK],
                                                         start=False, stop=True), reads=[bguB, kb.c["onesrow"][1]], writes=[pB])
                        pz.append((p, pB))
                    (pg, pgB), (pu, puB) = pz
                    g, gB = g_r[oc % 2]
                    sg, sgB = sg_r[oc % 2]
                    u, uB = u_r[oc % 2]
                    f.dve.op(lambda: nc.vector.tensor_scalar(out=g, in0=pg[:, 0:BLK], scalar1=7.0, scalar2=None, op0=ALU.min), reads=[pgB], writes=[gB])
                    f.act.op(lambda: nc.scalar.activation(out=sg, in_=g, func=AF.Sigmoid, scale=1.702), reads=[gB], writes=[sgB])
                    f.dve.op(lambda: nc.vector.tensor_scalar(out=u, in0=pu[:, 0:BLK], scalar1=-7.0, scalar2=7.0, op0=ALU.max, op1=ALU.min),
                             reads=[puB], writes=[uB])
                    f.dve.op(lambda: nc.vector.scalar_tensor_tensor(out=u, in0=u, scalar=1.0, in1=g, op0=ALU.add, op1=ALU.mult),
                             reads=[uB, gB], writes=[uB])
                    f.pool.op(lambda: nc.gpsimd.tensor_tensor(out=aT[:, oc, :], in0=u, in1=sg, op=ALU.mult), reads=[uB, sgB], writes=[aTB])
                yo, yoB = yo_r[bi % 2]
                for j in range(2):
                    for hf in range(2):
                        p, pB = kb.ps()
                        for k in range(8):
                            f.pe.op(lambda: nc.tensor.matmul(p[:, 0:512], lhsT=aT[:, k, j * 128:(j + 1) * 128], rhs=wdn[:, k, hf * 512:(hf + 1) * 512],
                                                             start=(k == 0), stop=False), reads=[aTB, wdnB], writes=[pB])
                        f.pe.op(lambda: nc.tensor.matmul(p[:, 0:512], lhsT=onesb[0:1, :], rhs=bdn[0:1, hf * 512:(hf + 1) * 512], start=False, stop=True),
                                reads=[onesbB, bdnB], writes=[pB])
                        f.act.op(lambda: nc.scalar.copy(out=yo[:, j, hf * 512:(hf + 1) * 512], in_=p[:, 0:512]), reads=[pB], writes=[yoB])
                f.q_sp.dma(YG[bi * BLK:(bi + 1) * BLK, :].rearrange("(j p) c -> p j c", p=128), yo, reads=[yoB], writes=[YGb])
        f.barrier()
        with ExitStack() as es:
            g2 = []
            for r in range(3):
                g, gB = kb.sb(es, f"g2_{r}", [128, D], F32)
                load_bcast(kb, g, gB, ADA[l, r:r + 1, 5 * D:6 * D])
                g2.append((g, gB))
            lg, lgB = kb.sb(es, "ln2g", [128, D], F32)
            lb, lbB = kb.sb(es, "ln2b", [128, D], F32)
            load_bcast(kb, lg, lgB, I["ln2_g"][l:l + 1, :])
            load_bcast(kb, lb, lbB, I["ln2_b"][l:l + 1, :])
            yk_r = kb.ring(es, "eyk", [128, D], F32, 8)
            m_r = kb.ring(es, "em", [128, D], F32, 2)
            xs_r = kb.ring(es, "exs", [128, D], F32, 2)
            rl = (kb.ring(es, "et", [128, D], F32, 2), kb.ring(es, "est", [128, 16], F32, 2), kb.ring(es, "eo", [128, D], F32, 2))
            outB = dr["out"][1]
            idx = 0
            for it in range(NT128):
                t0 = it * 128
                b, o = divmod(t0, S)
                isctx = o >= L
                if isctx and last:
                    continue
                r = 2 if isctx else b
                yks = []
                for k in range(4):
                    yk, ykB = yk_r[(idx * 4 + k) % 8]
                    f.q_pool.dma(None, None, reads=[DSTIB, YGb], writes=[ykB],
                                 fn=lambda g: g.indirect_dma_start(out=yk, out_offset=None, in_=YG,
                                                                   in_offset=bass.IndirectOffsetOnAxis(ap=DSTI[:, it, k:k + 1], axis=0),
                                                                   bounds_check=kb.breg(NROWS - 1), oob_is_err=False))
                    yks.append((yk, ykB))
                m, mB = m_r[idx % 2]
                f.dve.op(lambda: nc.vector.tensor_scalar(out=m, in0=yks[0][0], scalar1=WK[:, it, 0:1], scalar2=None, op0=ALU.mult),
                         reads=[yks[0][1], WKB], writes=[mB])
                for k in range(1, 4):
                    f.dve.op(lambda: nc.vector.scalar_tensor_tensor(out=m, in0=yks[k][0], scalar=WK[:, it, k:k + 1], in1=m, op0=ALU.mult, op1=ALU.add),
                             reads=[yks[k][1], WKB, mB], writes=[mB])
                xs, xsB = xs_r[idx % 2]
                f.q_sp.dma(xs, XA[t0:t0 + 128, :], writes=[xsB])
                if last:
                    dst_ap, dB = out[b * L + o:b * L + o + 128, :], outB
                else:
                    dst_ap, dB = XB[t0:t0 + 128, :], dr["XB"][1]
                g, gB = g2[r]
                resid_ln(kb, rl, [(m[:, 0:512], mB), (m[:, 512:1024], mB)], None, False, xs, xsB, g, gB, lg, lgB, lb, lbB, dst_ap, dB, idx)
                idx += 1
    f.barrier()
import numpy as np


def make_tables():
    t = np.arange(L)
    row = (t // 64).astype(np.float32)
    col = (t % 64).astype(np.float32)

    def tab(rot_dim):
        nf = rot_dim // 4
        inv = (10000.0 ** (-np.arange(nf, dtype=np.float32) / nf)).astype(np.float32)
        ang = np.concatenate([row[:, None] * inv, col[:, None] * inv], axis=-1).astype(np.float32)
        return np.cos(ang).astype(np.float32), np.sin(ang).astype(np.float32)
    c64, s64 = tab(64)
    c32, s32 = tab(32)
    out = {}
    idx = (np.arange(128) % 64) % 32
    out["c64"] = np.ascontiguousarray(c64[:, idx].T)
    out["s64"] = np.ascontiguousarray(s64[:, idx].T)
    idx = np.arange(32) % 16
    out["c32"] = np.ascontiguousarray(c32[:, idx].T)
    out["s32"] = np.ascontiguousarray(s32[:, idx].T)
    c96 = np.ones((96, L), np.float32)
    s96 = np.zeros((96, L), np.float32)
    c96[64:] = out["c32"]
    s96[64:] = out["s32"]
    out["c96"] = c96
    out["s96"] = s96
    rc = np.zeros((2, 4, 16), np.float32)
    for a, Ls in enumerate((L, C)):
        for g in range(4):
            w = 2 << g
            for e in range(8):
                tau = e
                rc[a, g, e] = 1.0 / (min(tau + w // 2, Ls) - max(tau - w // 2, 0))
                tau = Ls - 8 + e
                rc[a, g, 8 + e] = 1.0 / (min(tau + w // 2, Ls) - max(tau - w // 2, 0))
    out["rc"] = np.ascontiguousarray(np.broadcast_to(rc.reshape(1, -1), (128, 128)))
    return out


PARAMS = [("w_ada", (4, 1024, 6144)), ("b_ada", (4, 6144)), ("w_in", (4, 1024, 2720)), ("conv_w", (4, 31, 512)),
          ("conv_b", (4, 512)), ("conv_ln_g", (4, 512)), ("conv_ln_b", (4, 512)), ("swa_sink", (4, 8)),
          ("pool_w", (4, 4, 128, 128)), ("pool_scale", (4, 512)), ("mla_q_g", (4, 256)), ("mla_w_uq", (4, 256, 768)),
          ("mla_kv_g", (4, 128)), ("mla_w_uk", (4, 128, 8, 64)), ("mla_w_uv", (4, 128, 8, 64)),
          ("w_branch", (4, 4, 512, 1024)), ("w_gate", (4, 4, 1024, 1024)), ("b_gate", (4, 4, 1024)),
          ("w_out", (4, 1024, 1024)), ("ln1_g", (4, 1024)), ("ln1_b", (4, 1024)), ("router_w", (4, 1024, 32)),
          ("router_b", (4, 32)), ("w_gu", (4, 32, 1024, 2048)), ("b_gu", (4, 32, 2048)), ("w_down", (4, 32, 1024, 1024)),
          ("b_down", (4, 32, 1024)), ("ln2_g", (4, 1024)), ("ln2_b", (4, 1024))]


def build(nlayers=DEPTH, upto=99, dbg=()):
    nc = bass.Bass("TRN2", target_bir_lowering=False)
    kb = KB(nc, dbg)
    I = {}
    I["x"] = kb.din("x", (NB * L, D))
    I["c"] = kb.din("c", (NB, D))
    I["ctx"] = kb.din("ctx", (NB * C, D))
    I["c_ctx"] = kb.din("c_ctx", (D,))
    for name, shp in PARAMS:
        if upto < 7 and name in ("w_gu", "w_down"):
            continue
        I[name] = kb.din(name, shp)
    tabs = {}
    for k, rows in (("c64", 128), ("s64", 128), ("c32", 32), ("s32", 32), ("c96", 96), ("s96", 96)):
        tabs[k] = kb.din("tab_" + k, (rows, L))
    kb.din("tab_rc", (128, 2 * 4 * 16))
    out = nc.dram_tensor("out", [NB * L, D], F32, kind="ExternalOutput").ap()
    kb.dram["out"] = (out, Buf("out"))
    ADA = kb.dscr("ADA", (DEPTH, 3, 6 * D), F32)
    kb.dscr("HT", (D, T), BF16)
    kb.dscr("VT", (512, T), BF16)
    kb.dscr("QT", (512, T), BF16)
    kb.dscr("KT", (128, T), BF16)
    kb.dscr("V", (T, 128), BF16)
    kb.dscr("PT", (512, T), BF16)
    kb.dscr("QM", (8, 96, T), BF16)
    kb.dscr("KN", (8, 64, T), BF16)
    kb.dscr("KR", (32, T), BF16)
    kb.dscr("VM", (T, 512), BF16)
    for nm in ("YA", "YB", "YC", "YD"):
        kb.dscr(nm, (512, T), BF16)
    kb.dscr("H2", (T, D), BF16)
    kb.dscr("WBG", (NE * 128, 8 * 2048), BF16)
    kb.dscr("WBD", (NE * 128, 8 * D), BF16)
    kb.dscr("XG", (NROWS, D), BF16)
    kb.dscr("YG", (NROWS, D), F32)
    XA = kb.dscr("XA", (T, D), F32)
    XB = kb.dscr("XB", (T, D), F32)
    with ExitStack() as ces:
        ces.enter_context(nc.allow_non_contiguous_dma(reason="small strided parameter loads"))
        ces.enter_context(nc.allow_low_precision(reason="bf16 matmul operands"))
        kb.c = build_consts(kb, ces)
        phase_ada(kb, kb.c, I["c"], I["c_ctx"], I["w_ada"], I["b_ada"], ADA)
        for l in range(nlayers):
            if l == 0:
                def xsrc(t0, n):
                    b, o = divmod(t0, S)
                    if o < L:
                        return I["x"][b * L + o:b * L + o + n, :]
                    return I["ctx"][b * C + (o - L):b * C + (o - L) + n, :]
            else:
                def xsrc(t0, n):
                    return XB[t0:t0 + n, :]
            if upto >= 1:
                phase_proj(kb, l, xsrc, I["w_in"], I["mla_q_g"], I["mla_w_uq"], I["mla_kv_g"], I["mla_w_uk"], I["mla_w_uv"], tabs, ADA)
            if upto >= 2:
                phase_conv(kb, l, I["conv_w"], I["conv_b"], I["conv_ln_g"], I["conv_ln_b"])
            if upto >= 3:
                phase_pool(kb, l, I["pool_w"], I["pool_scale"])
            if upto >= 4:
                phase_swa(kb, l, I["swa_sink"])
            if upto >= 5:
                phase_mla(kb, l)
            if upto >= 6:
                phase_merge(kb, l, xsrc, I, ADA, XA, skip_ctx=(l == DEPTH - 1))
            if upto >= 7:
                phase_moe(kb, l, I, ADA, XA, XB, out, l == nlayers - 1)
        kb.f.barrier()
    return nc


def core_inputs(inputs, core):
    m = {}
    b0 = core * NB
    m["x"] = np.ascontiguousarray(inputs["x"][b0:b0 + NB].reshape(NB * L, D))
    m["c"] = np.ascontiguousarray(inputs["c"][b0:b0 + NB])
    m["ctx"] = np.ascontiguousarray(inputs["ctx"][b0:b0 + NB].reshape(NB * C, D))
    m["c_ctx"] = np.ascontiguousarray(inputs["c_ctx"])
    for name, shp in PARAMS:
        m[name] = np.ascontiguousarray(inputs[name], dtype=np.float32)
    return m


_NC_CACHE = {}


def kernel(**inputs):
    n = 8
    if "nc" not in _NC_CACHE:
        _NC_CACHE["nc"] = build(nlayers=DEPTH, upto=99, dbg=())
    nc = _NC_CACHE["nc"]
    tabs = make_tables()
    shared = {}
    for name, shp in PARAMS:
        shared[name] = np.ascontiguousarray(np.asarray(inputs[name], dtype=np.float32))
    for k, v in tabs.items():
        shared["tab_" + k] = v
    x = np.asarray(inputs["x"], dtype=np.float32)
    c = np.asarray(inputs["c"], dtype=np.float32)
    ctx = np.asarray(inputs["ctx"], dtype=np.float32)
    c_ctx = np.ascontiguousarray(np.asarray(inputs["c_ctx"], dtype=np.float32))
    in_maps = []
    for core in range(n):
        b0 = core * NB
        m = dict(shared)
        m["x"] = np.ascontiguousarray(x[b0:b0 + NB].reshape(NB * L, D))
        m["c"] = np.ascontiguousarray(c[b0:b0 + NB])
        m["ctx"] = np.ascontiguousarray(ctx[b0:b0 + NB].reshape(NB * C, D))
        m["c_ctx"] = c_ctx
        in_maps.append(m)
    res = run_bass_kernel_spmd(nc, in_maps, core_ids=list(range(n)))
    outs = [np.asarray(r["out"], dtype=np.float32).reshape(NB, L, D) for r in res.results]
    return np.concatenate(outs, axis=0)
```

```python
import numpy as np
import concourse.bass as bass
import concourse.mybir as mybir
from concourse.bass_utils import run_bass_kernel_spmd

F32 = mybir.dt.float32
BF16 = mybir.dt.bfloat16
I32 = mybir.dt.int32
U32 = mybir.dt.uint32
AF = mybir.ActivationFunctionType
ALU = mybir.AluOpType
AX = mybir.AxisListType


class Buf:
    __slots__ = ("name", "w", "rs")

    def __init__(self, name=""):
        self.name = name
        self.w = None
        self.rs = []


class Eng:
    def __init__(self, fw, eng, name):
        self.fw = fw
        self.e = eng
        self.name = name
        self.sem = fw.nc.alloc_semaphore("s_" + name)
        self.cnt = 0
        self.inorder = (name == "pe")
        self.waited = {}

    def wait_tok(self, tok):
        if tok is None:
            return
        sem, val = tok
        if self.inorder and sem is self.sem:
            return
        k = id(sem)
        if self.waited.get(k, 0) < val:
            self.e.wait_ge(sem, val)
            self.waited[k] = val

    def deps(self, reads, writes):
        for b in reads:
            self.wait_tok(b.w)
        for b in writes:
            self.wait_tok(b.w)
            for t in b.rs:
                self.wait_tok(t)

    def mark(self, tok, reads, writes):
        for b in reads:
            b.rs.append(tok)
        for b in writes:
            b.w = tok
            b.rs = []

    def op(self, ins, reads=(), writes=()):
        self.deps(reads, writes)
        i = ins()
        self.cnt += 1
        i.then_inc(self.sem, 1)
        tok = (self.sem, self.cnt)
        self.mark(tok, reads, writes)
        return tok


class DmaQ:
    def __init__(self, fw, engw, npool, name):
        self.fw = fw
        self.engw = engw
        self.pool = [[fw.nc.alloc_semaphore(f"d_{name}_{i}"), 0] for i in range(npool)]
        self.nxt = 0

    def dma(self, out, in_, reads=(), writes=(), fn=None, **kw):
        ew = self.engw
        ew.deps(reads, writes)
        slot = self.pool[self.nxt]
        self.nxt = (self.nxt + 1) % len(self.pool)
        sem, val = slot
        if val:
            ew.wait_tok((sem, val))
        if fn is None:
            i = ew.e.dma_start(out=out, in_=in_, **kw)
        else:
            i = fn(ew.e)
        slot[1] = val + 16
        i.then_inc(sem, 16)
        tok = (sem, val + 16)
        ew.mark(tok, reads, writes)
        return tok


class FW:
    def __init__(self, nc):
        self.nc = nc
        self.pe = Eng(self, nc.tensor, "pe")
        self.act = Eng(self, nc.scalar, "act")
        self.dve = Eng(self, nc.vector, "dve")
        self.pool = Eng(self, nc.gpsimd, "pool")
        self.sp = Eng(self, nc.sync, "sp")
        self.engs = [self.pe, self.act, self.dve, self.pool, self.sp]
        self.q_sp = DmaQ(self, self.sp, 24, "sp")
        self.q_pool = DmaQ(self, self.pool, 16, "pool")
        self.q_act = DmaQ(self, self.act, 8, "act")
        self.qs = [self.q_sp, self.q_pool, self.q_act]

    def barrier(self, engs=None):
        toks = []
        for e in self.engs:
            if e.cnt:
                toks.append((e.sem, e.cnt))
        for q in self.qs:
            for sem, val in q.pool:
                if val:
                    toks.append((sem, val))
        for e in (engs or self.engs):
            for t in toks:
                e.wait_tok(t)
from contextlib import ExitStack

D = 1024
NB = 2
L = 4096
C = 256
S = L + C
T = NB * S
DEPTH = 4
INW = 2720
EXTW = INW + 512 + 128 + 32
Q0, K0, V0, P0_, CQ0, CKV0, KR0 = 1024, 1536, 1664, 1792, 2304, 2560, 2688
QR0, KRT0, KRR0 = 2720, 3232, 3360
EPS = 1e-6
ALPHA = 8 ** 0.25
NE = 32
BLK = 256
NBLK = -(-(T * 4 + NE * (BLK - 1)) // BLK)
NROWS = NBLK * BLK


def tiles_all():
    out = []
    for b in range(NB):
        for i in range(L // 512):
            out.append((b * S + i * 512, 512, b, False, i * 512))
        out.append((b * S + L, 256, b, True, 0))
    return out


class KB:
    def __init__(self, nc, dbg=()):
        self.nc = nc
        self.f = FW(nc)
        self.dbg = set(dbg)
        self.dram = {}
        self.psf = []
        self.psb = []
        for i in range(6):
            self.psf.append((nc.alloc_psum_tensor(f"psf{i}", [128, 512], F32).ap(), Buf(f"psf{i}")))
        for i in range(2):
            self.psb.append((nc.alloc_psum_tensor(f"psb{i}", [128, 1024], BF16).ap(), Buf(f"psb{i}")))
        self.psf_i = 0
        self.pinned = set()
        self.bregs = {}
        self.psb_i = 0

    def ps(self):
        while True:
            i = self.psf_i % len(self.psf)
            self.psf_i += 1
            if i not in self.pinned:
                return self.psf[i]

    def breg(self, v):
        if v not in self.bregs:
            self.bregs[v] = self.nc.gpsimd.to_reg(v)
        return self.bregs[v]

    def pin(self):
        while True:
            i = self.psf_i % len(self.psf)
            self.psf_i += 1
            if i not in self.pinned:
                self.pinned.add(i)
                return self.psf[i] + (i,)

    def unpin(self, i):
        self.pinned.discard(i)

    def pb(self):
        r = self.psb[self.psb_i % len(self.psb)]
        self.psb_i += 1
        return r

    def din(self, name, shape, dt=F32):
        t = self.nc.dram_tensor(name, list(shape), dt, kind="ExternalInput").ap()
        self.dram[name] = (t, Buf(name))
        return t

    def dscr(self, name, shape, dt):
        kind = "ExternalOutput" if name in self.dbg else "Internal"
        t = self.nc.dram_tensor(name, list(shape), dt, kind=kind).ap()
        self.dram[name] = (t, Buf(name))
        return t

    def sb(self, es, name, shape, dt):
        self.uid = getattr(self, "uid", 0) + 1
        h = es.enter_context(self.nc.sbuf_tensor(f"{name}_u{self.uid}", list(shape), dt))
        return h.ap(), Buf(name)

    def ring(self, es, name, shape, dt, n=2):
        return [self.sb(es, f"{name}{i}", shape, dt) for i in range(n)]


def build_consts(kb, es):
    nc, f = kb.nc, kb.f
    c = {}
    ident, identB = kb.sb(es, "ident", [128, 128], BF16)
    f.pool.op(lambda: nc.gpsimd.memset(ident, 1.0), writes=[identB])
    f.pool.op(lambda: nc.gpsimd.affine_select(out=ident, in_=ident, pattern=[[-1, 128]], compare_op=ALU.is_equal,
                                              fill=0.0, base=0, channel_multiplier=1), reads=[identB], writes=[identB])
    c["ident"] = (ident, identB)
    onesb, onesbB = kb.sb(es, "onesb", [128, 128], BF16)
    f.pool.op(lambda: nc.gpsimd.memset(onesb, 1.0), writes=[onesbB])
    c["onesb"] = (onesb, onesbB)
    onesf, onesfB = kb.sb(es, "onesf", [128, 128], F32)
    f.pool.op(lambda: nc.gpsimd.memset(onesf, 1.0), writes=[onesfB])
    c["onesf"] = (onesf, onesfB)
    nh, nhB = kb.sb(es, "neghalf", [128, 512], F32)
    f.pool.op(lambda: nc.gpsimd.memset(nh, -0.5), writes=[nhB])
    c["neghalf"] = (nh, nhB)
    orow, orowB = kb.sb(es, "onesrow", [1, 512], BF16)
    f.pool.op(lambda: nc.gpsimd.memset(orow, 1.0), writes=[orowB])
    c["onesrow"] = (orow, orowB)
    epsc, epscB = kb.sb(es, "epsc", [128, 1], F32)
    f.pool.op(lambda: nc.gpsimd.memset(epsc, EPS), writes=[epscB])
    c["epsc"] = (epsc, epscB)
    return c


def phase_ada(kb, cs, c_in, cctx_in, w_ada, b_ada, ADA):
    nc, f = kb.nc, kb.f
    ADAb = kb.dram["ADA"][1]
    with ExitStack() as es:
        crow, crowB = kb.sb(es, "crow", [3, D], F32)
        f.q_sp.dma(crow[0:2, :], c_in, writes=[crowB])
        f.q_sp.dma(crow[2:3, :], cctx_in.rearrange("(o d) -> o d", o=1), writes=[crowB])
        sg, sgB = kb.sb(es, "csg", [3, D], F32)
        f.act.op(lambda: nc.scalar.activation(out=sg, in_=crow, func=AF.Sigmoid), reads=[crowB], writes=[sgB])
        f.dve.op(lambda: nc.vector.tensor_tensor(out=sg, in0=sg, in1=crow, op=ALU.mult), reads=[sgB, crowB], writes=[sgB])
        id3, id3B = kb.sb(es, "id3", [3, 3], F32)
        f.pool.op(lambda: nc.gpsimd.memset(id3, 1.0), writes=[id3B])
        f.pool.op(lambda: nc.gpsimd.affine_select(out=id3, in_=id3, pattern=[[-1, 3]], compare_op=ALU.is_equal,
                                                  fill=0.0, base=0, channel_multiplier=1), reads=[id3B], writes=[id3B])
        sT, sTB = kb.sb(es, "sT", [128, 8, 3], F32)
        pt, ptB = kb.ps()
        for k in range(8):
            f.pe.op(lambda: nc.tensor.matmul(pt[:, k * 3:(k + 1) * 3], lhsT=sg[:, k * 128:(k + 1) * 128], rhs=id3,
                                             start=True, stop=True), reads=[sgB, id3B], writes=[ptB])
        f.dve.op(lambda: nc.vector.tensor_copy(out=sT.rearrange("p k r -> p (k r)"), in_=pt[:, 0:24]), reads=[ptB], writes=[sTB])
        ones3, ones3B = kb.sb(es, "ones3", [1, 3], F32)
        f.pool.op(lambda: nc.gpsimd.memset(ones3, 1.0), writes=[ones3B])
        wr = kb.ring(es, "wada", [128, 8, 512], F32, 2)
        br = kb.ring(es, "bada", [1, 512], F32, 2)
        orr = kb.ring(es, "oada", [3, 512], F32, 2)
        it = 0
        for l in range(DEPTH):
            wv = w_ada[l].rearrange("(k p) c -> p k c", p=128)
            for cc in range(12):
                w, wB = wr[it % 2]
                bb, bbB = br[it % 2]
                o, oB = orr[it % 2]
                f.q_sp.dma(w, wv[:, :, cc * 512:(cc + 1) * 512], writes=[wB])
                f.q_sp.dma(bb, b_ada[l:l + 1, cc * 512:(cc + 1) * 512], writes=[bbB])
                p, pB = kb.ps()
                for k in range(8):
                    f.pe.op(lambda: nc.tensor.matmul(p[0:3, :], lhsT=sT[:, k, :], rhs=w[:, k, :], start=(k == 0), stop=False),
                            reads=[sTB, wB], writes=[pB])
                f.pe.op(lambda: nc.tensor.matmul(p[0:3, :], lhsT=ones3, rhs=bb, start=False, stop=True),
                        reads=[ones3B, bbB], writes=[pB])
                f.act.op(lambda: nc.scalar.copy(out=o, in_=p[0:3, :]), reads=[pB], writes=[oB])
                f.q_sp.dma(ADA[l, :, cc * 512:(cc + 1) * 512], o, reads=[oB], writes=[ADAb])
                it += 1
    f.barrier()


def load_bcast(kb, dst, dstB, src_row_ap, n=128):
    kb.f.q_sp.dma(dst, src_row_ap.partition_broadcast(n), writes=[dstB])


def ln_tile(kb, xs, xsB, st, stB, eps_sqrt=True):
    nc, f = kb.nc, kb.f
    f.dve.op(lambda: nc.vector.bn_stats(out=st[:, 0:6], in_=xs[:, 0:512]), reads=[xsB], writes=[stB])
    f.dve.op(lambda: nc.vector.bn_stats(out=st[:, 6:12], in_=xs[:, 512:1024]), reads=[xsB], writes=[stB])
    f.dve.op(lambda: nc.vector.bn_aggr(out=st[:, 12:14], in_=st[:, 0:12]), reads=[stB], writes=[stB])
    f.dve.op(lambda: nc.vector.tensor_scalar(out=st[:, 14:15], in0=st[:, 13:14], scalar1=EPS, scalar2=None, op0=ALU.add),
             reads=[stB], writes=[stB])
    nh, nhB = kb.c["neghalf"]
    f.pool.op(lambda: nc.gpsimd.tensor_tensor(out=st[:, 15:16], in0=st[:, 14:15], in1=nh[:, 0:1], op=ALU.pow),
              reads=[stB, nhB], writes=[stB])
    return st[:, 12:13], st[:, 15:16]


def phase_proj(kb, l, xsrc, w_in, mla_q_g, mla_w_uq, mla_kv_g, mla_w_uk, mla_w_uv, tabs, ADA):
    nc, f = kb.nc, kb.f
    dr = kb.dram
    with ExitStack() as es:
        wext, wextB = kb.sb(es, "wext", [128, 8, EXTW], BF16)
        wv = w_in[l].rearrange("(k p) c -> p k c", p=128)
        f.q_pool.dma(wext[:, :, 0:2048], wv[:, :, 0:2048], writes=[wextB])
        f.q_pool.dma(wext[:, :, 2048:INW], wv[:, :, 2048:INW], writes=[wextB])

        def rot(dst0, src0, nheads, half, eng_neg, eng_cp, w=wext, wB=wextB, nk=8):
            for k in range(nk):
                s = w[:, k, src0:src0 + nheads * 2 * half].rearrange("p (h two d) -> p h two d", two=2, d=half)
                d = w[:, k, dst0:dst0 + nheads * 2 * half].rearrange("p (h two d) -> p h two d", two=2, d=half)
                f.act.op(lambda: nc.scalar.mul(out=d[:, :, 0, :], in_=s[:, :, 1, :], mul=-1.0), reads=[wB], writes=[wB])
                f.dve.op(lambda: nc.vector.tensor_copy(out=d[:, :, 1, :], in_=s[:, :, 0, :]), reads=[wB], writes=[wB])
        rot(QR0, Q0, 8, 32, None, None)
        rot(KRT0, K0, 2, 32, None, None)
        rot(KRR0, KR0, 1, 16, None, None)
        wuq, wuqB = kb.sb(es, "wuq", [128, 2, 768], BF16)
        f.q_pool.dma(wuq, mla_w_uq[l].rearrange("(k p) c -> p k c", p=128), writes=[wuqB])
        wuqr, wuqrB = kb.sb(es, "wuqr", [128, 2, 768], BF16)
        f.pool.op(lambda: nc.gpsimd.memset(wuqr, 0.0), writes=[wuqrB])
        for k in range(2):
            s = wuq[:, k, :].rearrange("p (h c) -> p h c", c=96)[:, :, 64:96].rearrange("p h (two d) -> p h two d", two=2)
            d = wuqr[:, k, :].rearrange("p (h c) -> p h c", c=96)[:, :, 64:96].rearrange("p h (two d) -> p h two d", two=2)
            f.act.op(lambda: nc.scalar.mul(out=d[:, :, 0, :], in_=s[:, :, 1, :], mul=-1.0), reads=[wuqB, wuqrB], writes=[wuqrB])
            f.dve.op(lambda: nc.vector.tensor_copy(out=d[:, :, 1, :], in_=s[:, :, 0, :]), reads=[wuqB, wuqrB], writes=[wuqrB])
        wuk, wukB = kb.sb(es, "wuk", [128, 512], BF16)
        f.q_pool.dma(wuk, mla_w_uk[l].rearrange("r h n -> r (h n)"), writes=[wukB])
        wuv, wuvB = kb.sb(es, "wuv", [128, 512], BF16)
        f.q_pool.dma(wuv, mla_w_uv[l].rearrange("r h n -> r (h n)"), writes=[wuvB])
        gq, gqB = kb.sb(es, "gq", [128, 2], F32)
        f.q_sp.dma(gq, mla_q_g[l].rearrange("(k p) -> p k", p=128), writes=[gqB])
        gkv, gkvB = kb.sb(es, "gkv", [128, 1], F32)
        f.q_sp.dma(gkv, mla_kv_g[l].rearrange("(p o) -> p o", o=1), writes=[gkvB])
        mods = []
        for r in range(3):
            sc, scB = kb.sb(es, f"sc1_{r}", [128, D], F32)
            sh, shB = kb.sb(es, f"sh1_{r}", [128, D], F32)
            load_bcast(kb, sh, shB, ADA[l, r:r + 1, 0:D])
            load_bcast(kb, sc, scB, ADA[l, r:r + 1, D:2 * D])
            f.pool.op(lambda: nc.gpsimd.tensor_scalar(out=sc, in0=sc, scalar1=1.0, scalar2=None, op0=ALU.add),
                      reads=[scB], writes=[scB])
            mods.append((sc, scB, sh, shB))
        ident, identB = kb.c["ident"]
        onesb, onesbB = kb.c["onesb"]
        nh, nhB = kb.c["neghalf"]
        xs_r = kb.ring(es, "xs", [128, D], F32, 2)
        st_r = kb.ring(es, "st", [128, 16], F32, 2)
        xn_r = kb.ring(es, "xn", [128, D], F32, 2)
        hb_r = kb.ring(es, "hb", [128, D], BF16, 2)
        hT_r = kb.ring(es, "hT", [128, 8, 512], BF16, 2)
        tab_r = {k: kb.ring(es, "tab_" + k, [tabs[k].shape[0], 512], F32, 1) for k in tabs}
        sg_r = kb.ring(es, "sgt", [128, 512], F32, 2)
        vT_r = kb.ring(es, "vTt", [128, 4, 512], BF16, 1)
        qT_r = kb.ring(es, "qTt", [128, 4, 512], BF16, 1)
        kT_r = kb.ring(es, "kTt", [128, 512], BF16, 2)
        pT_r = kb.ring(es, "pTt", [128, 4, 512], BF16, 1)
        t1_r = kb.ring(es, "t1", [128, 512], F32, 3)
        t2_r = kb.ring(es, "t2", [128, 512], F32, 3)
        vtm_r = kb.ring(es, "vtm", [128, 4, 128], BF16, 2)
        sq_r = kb.ring(es, "sq", [128, 3, 512], BF16, 1)
        rq_r = kb.ring(es, "rq", [128, 2, 512], F32, 1)
        cqn_r = kb.ring(es, "cqn", [128, 2, 512], BF16, 2)
        ckvn_r = kb.ring(es, "ckvn", [128, 512], BF16, 2)
        krT_r = kb.ring(es, "krT", [32, 512], BF16, 2)
        qm_r = kb.ring(es, "qm", [96, 8, 512], BF16, 1)
        kn_r = kb.ring(es, "kn", [64, 8, 512], BF16, 1)
        vm_r = kb.ring(es, "vmt", [128, 4, 512], BF16, 1)
        HT, HTb = dr["HT"]
        VT, VTb = dr["VT"]
        QT, QTb = dr["QT"]
        KT, KTb = dr["KT"]
        Vd, Vdb = dr["V"]
        PT, PTb = dr["PT"]
        QM, QMb = dr["QM"]
        KN, KNb = dr["KN"]
        KR, KRb = dr["KR"]
        VM, VMb = dr["VM"]

        def proj(c0, m, hT, hTB, n, w=wext, wB=wextB, nk=8):
            p, pB = kb.ps()
            for k in range(nk):
                f.pe.op(lambda: nc.tensor.matmul(p[0:m, 0:n], lhsT=w[:, k, c0:c0 + m], rhs=hT[:, k, 0:n],
                                                 start=(k == 0), stop=(k == nk - 1)), reads=[wB, hTB], writes=[pB])
            return p, pB

        for ti, (t0, n, b, isctx, pos) in enumerate(tiles_all()):
            r = 2 if isctx else b
            sc, scB, sh, shB = mods[r]
            hT, hTB = hT_r[ti % 2]
            nsub = n // 128
            for j in range(nsub):
                it = ti * 4 + j
                xs, xsB = xs_r[it % len(xs_r)]
                st, stB = st_r[it % len(st_r)]
                xn, xnB = xn_r[it % 2]
                hb, hbB = hb_r[it % 2]
                f.q_sp.dma(xs, xsrc(t0 + j * 128, 128), writes=[xsB])
                mean, rstd = ln_tile(kb, xs, xsB, st, stB)
                f.dve.op(lambda: nc.vector.tensor_scalar(out=xn, in0=xs, scalar1=mean, scalar2=rstd, op0=ALU.subtract, op1=ALU.mult),
                         reads=[xsB, stB], writes=[xnB])
                f.pool.op(lambda: nc.gpsimd.tensor_tensor(out=xn, in0=xn, in1=sc, op=ALU.mult), reads=[xnB, scB], writes=[xnB])
                f.dve.op(lambda: nc.vector.tensor_tensor(out=hb, in0=xn, in1=sh, op=ALU.add), reads=[xnB, shB], writes=[hbB])
                pt, ptB = kb.pb()
                for k in range(8):
                    f.pe.op(lambda: nc.tensor.transpose(pt[:, k * 128:(k + 1) * 128], hb[:, k * 128:(k + 1) * 128], ident),
                            reads=[hbB, identB], writes=[ptB])
                f.act.op(lambda: nc.scalar.copy(out=hT[:, :, j * 128:(j + 1) * 128], in_=pt.rearrange("p (k t) -> p k t", k=8)),
                         reads=[ptB], writes=[hTB])
            f.q_sp.dma(HT.rearrange("(k p) t -> p k t", p=128)[:, :, t0:t0 + n], hT[:, :, 0:n], reads=[hTB], writes=[HTb])
            tb = {}
            if not isctx:
                for k in tabs:
                    ta, taB = tab_r[k][0]
                    f.q_sp.dma(ta[:, 0:n], tabs[k][:, pos:pos + n], writes=[taB])
                    tb[k] = (ta, taB)
            vT, vTB = vT_r[ti % len(vT_r)]
            for cc in range(4):
                pg, pgB = proj(512 + cc * 128, 128, hT, hTB, n)
                sg, sgB = sg_r[cc % 2]
                f.act.op(lambda: nc.scalar.activation(out=sg[:, 0:n], in_=pg[:, 0:n], func=AF.Sigmoid), reads=[pgB], writes=[sgB])
                pv, pvB = proj(cc * 128, 128, hT, hTB, n)
                f.dve.op(lambda: nc.vector.tensor_tensor(out=vT[:, cc, 0:n], in0=pv[:, 0:n], in1=sg[:, 0:n], op=ALU.mult),
                         reads=[pvB, sgB], writes=[vTB])
            f.q_sp.dma(VT.rearrange("(k p) t -> p k t", p=128)[:, :, t0:t0 + n], vT[:, :, 0:n], reads=[vTB], writes=[VTb])

            def roped(c0, cr0, m, dst, dstB, ck, sk, ii):
                p1, p1B = proj(c0, m, hT, hTB, n)
                if isctx:
                    f.act.op(lambda: nc.scalar.copy(out=dst, in_=p1[0:m, 0:n]), reads=[p1B], writes=[dstB])
                    return
                p2, p2B = proj(cr0, m, hT, hTB, n)
                t1, t1B = t1_r[ii % 3]
                t2, t2B = t2_r[ii % 3]
                co, coB = tb[ck]
                si, siB = tb[sk]
                f.dve.op(lambda: nc.vector.tensor_tensor(out=t1[0:m, 0:n], in0=p1[0:m, 0:n], in1=co[0:m, 0:n], op=ALU.mult),
                         reads=[p1B, coB], writes=[t1B])
                f.dve.op(lambda: nc.vector.tensor_tensor(out=t2[0:m, 0:n], in0=p2[0:m, 0:n], in1=si[0:m, 0:n], op=ALU.mult),
                         reads=[p2B, siB], writes=[t2B])
                f.pool.op(lambda: nc.gpsimd.tensor_tensor(out=dst, in0=t1[0:m, 0:n], in1=t2[0:m, 0:n], op=ALU.add),
                          reads=[t1B, t2B], writes=[dstB])
            qT, qTB = qT_r[ti % len(qT_r)]
            for cc in range(4):
                roped(Q0 + cc * 128, QR0 + cc * 128, 128, qT[:, cc, 0:n], qTB, "c64", "s64", cc)
            f.q_sp.dma(QT.rearrange("(k p) t -> p k t", p=128)[:, :, t0:t0 + n], qT[:, :, 0:n], reads=[qTB], writes=[QTb])
            kT, kTB = kT_r[ti % 2]
            roped(K0, KRT0, 128, kT[:, 0:n], kTB, "c64", "s64", 4)
            f.q_sp.dma(KT[:, t0:t0 + n], kT[:, 0:n], reads=[kTB], writes=[KTb])
            krT, krTB = krT_r[ti % 2]
            roped(KR0, KRR0, 32, krT[:, 0:n], krTB, "c32", "s32", 5)
            f.q_sp.dma(KR[:, t0:t0 + n], krT[:, 0:n], reads=[krTB], writes=[KRb])
            vtm, vtmB = vtm_r[ti % 2]
            for j in range(nsub):
                p, pB = kb.ps()
                for k in range(8):
                    f.pe.op(lambda: nc.tensor.matmul(p[:, 0:128], lhsT=hT[:, k, j * 128:(j + 1) * 128], rhs=wext[:, k, V0:V0 + 128],
                                                     start=(k == 0), stop=(k == 7)), reads=[hTB, wextB], writes=[pB])
                f.act.op(lambda: nc.scalar.copy(out=vtm[:, j, :], in_=p[:, 0:128]), reads=[pB], writes=[vtmB])
            f.q_sp.dma(Vd[t0:t0 + n, :].rearrange("(j p) c -> p j c", p=128), vtm[:, 0:nsub, :], reads=[vtmB], writes=[Vdb])
            pT, pTB = pT_r[ti % len(pT_r)]
            for cc in range(4):
                p, pB = proj(P0_ + cc * 128, 128, hT, hTB, n)
                f.act.op(lambda: nc.scalar.copy(out=pT[:, cc, 0:n], in_=p[:, 0:n]), reads=[pB], writes=[pTB])
            f.q_sp.dma(PT.rearrange("(k p) t -> p k t", p=128)[:, :, t0:t0 + n], pT[:, :, 0:n], reads=[pTB], writes=[PTb])
            pcs = [proj(CQ0, 128, hT, hTB, n), proj(CQ0 + 128, 128, hT, hTB, n), proj(CKV0, 128, hT, hTB, n)]
            sq, sqB = sq_r[ti % len(sq_r)]
            for i3 in range(3):
                f.act.op(lambda: nc.scalar.activation(out=sq[:, i3, 0:n], in_=pcs[i3][0][:, 0:n], func=AF.Square),
                         reads=[pcs[i3][1]], writes=[sqB])
            rq, rqB = rq_r[ti % len(rq_r)]
            for i2, (ks, div) in enumerate((((0, 1), 256.0), ((2,), 128.0))):
                pss, pssB = kb.ps()
                for ii, k in enumerate(ks):
                    f.pe.op(lambda: nc.tensor.matmul(pss[:, 0:n], lhsT=onesb, rhs=sq[:, k, 0:n], start=(ii == 0), stop=(ii == len(ks) - 1)),
                            reads=[onesbB, sqB], writes=[pssB])
                f.act.op(lambda: nc.scalar.activation(out=rq[:, i2, 0:n], in_=pss[:, 0:n], func=AF.Sqrt, scale=1.0 / div,
                                                      bias=kb.c["epsc"][0][:, 0:1]), reads=[pssB, kb.c["epsc"][1]], writes=[rqB])
                f.dve.op(lambda: nc.vector.reciprocal(out=rq[:, i2, 0:n], in_=rq[:, i2, 0:n]), reads=[rqB], writes=[rqB])
            cqn, cqnB = cqn_r[ti % 2]
            for k in range(2):
                f.dve.op(lambda: nc.vector.scalar_tensor_tensor(out=cqn[:, k, 0:n], in0=pcs[k][0][:, 0:n], scalar=gq[:, k:k + 1],
                                                                in1=rq[:, 0, 0:n], op0=ALU.mult, op1=ALU.mult),
                         reads=[pcs[k][1], gqB, rqB], writes=[cqnB])
            ckvn, ckvnB = ckvn_r[ti % 2]
            f.dve.op(lambda: nc.vector.scalar_tensor_tensor(out=ckvn[:, 0:n], in0=pcs[2][0][:, 0:n], scalar=gkv[:, 0:1],
                                                            in1=rq[:, 1, 0:n], op0=ALU.mult, op1=ALU.mult),
                     reads=[pcs[2][1], gkvB, rqB], writes=[ckvnB])
            qm, qmB = qm_r[ti % len(qm_r)]
            for h in range(8):
                p1, p1B = proj(h * 96, 96, cqn, cqnB, n, w=wuq, wB=wuqB, nk=2)
                if isctx:
                    f.act.op(lambda: nc.scalar.copy(out=qm[:, h, 0:n], in_=p1[0:96, 0:n]), reads=[p1B], writes=[qmB])
                else:
                    p2, p2B = proj(h * 96, 96, cqn, cqnB, n, w=wuqr, wB=wuqrB, nk=2)
                    t1, t1B = t1_r[h % 3]
                    t2, t2B = t2_r[h % 3]
                    co, coB = tb["c96"]
                    si, siB = tb["s96"]
                    f.dve.op(lambda: nc.vector.tensor_tensor(out=t1[0:96, 0:n], in0=p1[0:96, 0:n], in1=co[0:96, 0:n], op=ALU.mult),
                             reads=[p1B, coB], writes=[t1B])
                    f.dve.op(lambda: nc.vector.tensor_tensor(out=t2[0:96, 0:n], in0=p2[0:96, 0:n], in1=si[0:96, 0:n], op=ALU.mult),
                             reads=[p2B, siB], writes=[t2B])
                    f.pool.op(lambda: nc.gpsimd.tensor_tensor(out=qm[:, h, 0:n], in0=t1[0:96, 0:n], in1=t2[0:96, 0:n], op=ALU.add),
                              reads=[t1B, t2B], writes=[qmB])
            f.q_sp.dma(QM.rearrange("h r t -> r h t")[:, :, t0:t0 + n], qm[:, :, 0:n], reads=[qmB], writes=[QMb])
            kn, knB = kn_r[ti % len(kn_r)]
            for h in range(8):
                p, pB = kb.ps()
                f.pe.op(lambda: nc.tensor.matmul(p[0:64, 0:n], lhsT=wuk[:, h * 64:(h + 1) * 64], rhs=ckvn[:, 0:n], start=True, stop=True),
                        reads=[wukB, ckvnB], writes=[pB])
                f.act.op(lambda: nc.scalar.copy(out=kn[:, h, 0:n], in_=p[0:64, 0:n]), reads=[pB], writes=[knB])
            f.q_sp.dma(KN.rearrange("h r t -> r h t")[:, :, t0:t0 + n], kn[:, :, 0:n], reads=[knB], writes=[KNb])
            vm, vmB = vm_r[ti % len(vm_r)]
            for j in range(nsub):
                p, pB = kb.ps()
                f.pe.op(lambda: nc.tensor.matmul(p[:, 0:512], lhsT=ckvn[:, j * 128:(j + 1) * 128], rhs=wuv, start=True, stop=True),
                        reads=[ckvnB, wuvB], writes=[pB])
                f.act.op(lambda: nc.scalar.copy(out=vm[:, j, :], in_=p[:, 0:512]), reads=[pB], writes=[vmB])
            f.q_sp.dma(VM[t0:t0 + n, :].rearrange("(j p) c -> p j c", p=128), vm[:, 0:nsub, :], reads=[vmB], writes=[VMb])
    f.barrier()


def segs():
    out = []
    for b in range(NB):
        out.append((b * S, L, b, False))
        out.append((b * S + L, C, b, True))
    return out


def seg_tiles():
    out = []
    for (s0, sl, b, isctx) in segs():
        n = 512 if not isctx else 256
        for i in range(sl // n):
            out.append((s0 + i * n, n, s0, s0 + sl, b, isctx))
    return out


def load_halo(kb, dst, dstB, src3, t0, n, halo, lo_lim, hi_lim):
    nc, f = kb.nc, kb.f
    lo = max(t0 - halo, lo_lim)
    hi = min(t0 + n + halo, hi_lim)
    off = lo - (t0 - halo)
    if off > 0:
        f.pool.op(lambda: nc.gpsimd.memset(dst[:, :, 0:off], 0.0), writes=[dstB])
    if off + (hi - lo) < n + 2 * halo:
        f.pool.op(lambda: nc.gpsimd.memset(dst[:, :, off + (hi - lo):n + 2 * halo], 0.0), writes=[dstB])
    f.q_sp.dma(dst[:, :, off:off + (hi - lo)], src3[:, :, lo:hi], writes=[dstB])


def phase_conv(kb, l, conv_w, conv_b, ln_g, ln_b):
    nc, f = kb.nc, kb.f
    VT, VTb = kb.dram["VT"]
    YA, YAb = kb.dram["YA"]
    onesf, onesfB = kb.c["onesf"]
    nh, nhB = kb.c["neghalf"]
    with ExitStack() as es:
        cw, cwB = kb.sb(es, "cw", [128, 4, 31], F32)
        for k in range(4):
            f.q_sp.dma(cw[:, k, :], conv_w[l][:, k * 128:(k + 1) * 128].rearrange("j p -> p j"), writes=[cwB])
        prm, prmB = kb.sb(es, "cprm", [128, 3, 4], F32)
        for i, a in enumerate((conv_b, ln_g, ln_b)):
            f.q_sp.dma(prm[:, i, :], a[l].rearrange("(k p) -> p k", p=128), writes=[prmB])
        ve_r = kb.ring(es, "vext", [128, 4, 512 + 30], BF16, 2)
        y_r = kb.ring(es, "cy", [128, 4, 512], F32, 2)
        sq_r = kb.ring(es, "csq", [128, 4, 512], F32, 1)
        mean_r = kb.ring(es, "cmean", [128, 512], F32, 2)
        rstd_r = kb.ring(es, "crstd", [128, 512], F32, 2)
        z_r = kb.ring(es, "cz", [128, 512], F32, 2)
        ya_r = kb.ring(es, "cya", [128, 4, 512], BF16, 2)
        VT3 = VT.rearrange("(k p) t -> p k t", p=128)
        YA3 = YA.rearrange("(k p) t -> p k t", p=128)
        for ti, (t0, n, slo, shi, b, isctx) in enumerate(seg_tiles()):
            ve, veB = ve_r[ti % 2]
            load_halo(kb, ve, veB, VT3, t0, n, 15, slo, shi)
            y, yB = y_r[ti % 2]
            for k in range(4):
                f.dve.op(lambda: nc.vector.tensor_scalar(out=y[:, k, 0:n], in0=ve[:, k, 0:n], scalar1=cw[:, k, 0:1], scalar2=prm[:, 0, k:k + 1],
                                                         op0=ALU.mult, op1=ALU.add), reads=[veB, cwB, prmB], writes=[yB])
                for j in range(1, 31):
                    f.dve.op(lambda: nc.vector.scalar_tensor_tensor(out=y[:, k, 0:n], in0=ve[:, k, j:j + n], scalar=cw[:, k, j:j + 1],
                                                                    in1=y[:, k, 0:n], op0=ALU.mult, op1=ALU.add),
                             reads=[veB, cwB, yB], writes=[yB])
            sq, sqB = sq_r[0]
            f.act.op(lambda: nc.scalar.activation(out=sq[:, :, 0:n], in_=y[:, :, 0:n], func=AF.Square), reads=[yB], writes=[sqB])
            p1, p1B = kb.ps()
            p2, p2B = kb.ps()
            for k in range(4):
                f.pe.op(lambda: nc.tensor.matmul(p1[:, 0:n], lhsT=onesf, rhs=y[:, k, 0:n], start=(k == 0), stop=(k == 3)),
                        reads=[onesfB, yB], writes=[p1B])
            for k in range(4):
                f.pe.op(lambda: nc.tensor.matmul(p2[:, 0:n], lhsT=onesf, rhs=sq[:, k, 0:n], start=(k == 0), stop=(k == 3)),
                        reads=[onesfB, sqB], writes=[p2B])
            mean, meanB = mean_r[ti % 2]
            rstd, rstdB = rstd_r[ti % 2]
            f.act.op(lambda: nc.scalar.mul(out=mean[:, 0:n], in_=p1[:, 0:n], mul=1.0 / 512), reads=[p1B], writes=[meanB])
            f.dve.op(lambda: nc.vector.tensor_tensor(out=rstd[:, 0:n], in0=mean[:, 0:n], in1=mean[:, 0:n], op=ALU.mult),
                     reads=[meanB], writes=[rstdB])
            f.dve.op(lambda: nc.vector.scalar_tensor_tensor(out=rstd[:, 0:n], in0=p2[:, 0:n], scalar=1.0 / 512, in1=rstd[:, 0:n],
                                                            op0=ALU.mult, op1=ALU.subtract), reads=[p2B, rstdB], writes=[rstdB])
            f.act.op(lambda: nc.scalar.activation(out=rstd[:, 0:n], in_=rstd[:, 0:n], func=AF.Sqrt, bias=kb.c["epsc"][0][:, 0:1]),
                     reads=[rstdB, kb.c["epsc"][1]], writes=[rstdB])
            f.dve.op(lambda: nc.vector.reciprocal(out=rstd[:, 0:n], in_=rstd[:, 0:n]), reads=[rstdB], writes=[rstdB])
            ya, yaB = ya_r[ti % 2]
            for k in range(4):
                z, zB = z_r[k % 2]
                f.dve.op(lambda: nc.vector.tensor_tensor(out=z[:, 0:n], in0=y[:, k, 0:n], in1=mean[:, 0:n], op=ALU.subtract),
                         reads=[yB, meanB], writes=[zB])
                f.pool.op(lambda: nc.gpsimd.tensor_tensor(out=z[:, 0:n], in0=z[:, 0:n], in1=rstd[:, 0:n], op=ALU.mult),
                          reads=[zB, rstdB], writes=[zB])
                f.act.op(lambda: nc.scalar.activation(out=ya[:, k, 0:n], in_=z[:, 0:n], func=AF.Silu, scale=prm[:, 1, k:k + 1],
                                                      bias=prm[:, 2, k:k + 1]), reads=[zB, prmB], writes=[yaB])
            f.q_sp.dma(YA3[:, :, t0:t0 + n], ya[:, :, 0:n], reads=[yaB], writes=[YAb])
    f.barrier()


def phase_pool(kb, l, pool_w, pool_scale):
    nc, f = kb.nc, kb.f
    PT, PTb = kb.dram["PT"]
    YC, YCb = kb.dram["YC"]
    rc_d = kb.dram["tab_rc"][0]
    with ExitStack() as es:
        pw, pwB = kb.sb(es, "pw", [128, 4, 128], BF16)
        f.q_pool.dma(pw, pool_w[l].rearrange("g c d -> c g d"), writes=[pwB])
        psc, pscB = kb.sb(es, "psc", [128, 4], F32)
        f.q_sp.dma(psc, pool_scale[l].rearrange("(k p) -> p k", p=128), writes=[pscB])
        rc, rcB = kb.sb(es, "rc", [128, 2, 4, 16], F32)
        f.q_sp.dma(rc.rearrange("p a g e -> p (a g e)"), rc_d, writes=[rcB])
        ue_r = kb.ring(es, "uext", [128, 4, 512 + 16], BF16, 2)
        A_r = kb.ring(es, "pA", [128, 512 + 16], F32, 2)
        B_r = kb.ring(es, "pB", [128, 512 + 16], F32, 2)
        pm_r = kb.ring(es, "ppm", [128, 512], BF16, 2)
        pmf_r = kb.ring(es, "ppmf", [128, 16], F32, 2)
        yc_r = kb.ring(es, "pyc", [128, 4, 512], BF16, 2)
        PT3 = PT.rearrange("(k p) t -> p k t", p=128)
        YC3 = YC.rearrange("(k p) t -> p k t", p=128)
        it = 0
        for ti, (t0, n, slo, shi, b, isctx) in enumerate(seg_tiles()):
            ue, ueB = ue_r[ti % 2]
            load_halo(kb, ue, ueB, PT3, t0, n, 8, slo, shi)
            yc, ycB = yc_r[ti % 2]
            E = n + 16
            ai = 1 if isctx else 0
            for g in range(4):
                w = 2 << g
                A, AB = A_r[it % 2]
                Bt, BB = B_r[it % 2]
                it += 1
                f.dve.op(lambda: nc.vector.tensor_tensor(out=A[:, 1:E], in0=ue[:, g, 0:E - 1], in1=ue[:, g, 1:E], op=ALU.add),
                         reads=[ueB], writes=[AB])
                s, sB = A, AB
                if g >= 1:
                    f.pool.op(lambda: nc.gpsimd.tensor_tensor(out=Bt[:, 2:E - 1], in0=A[:, 1:E - 2], in1=A[:, 3:E], op=ALU.add),
                              reads=[AB], writes=[BB])
                    s, sB = Bt, BB
                if g >= 2:
                    f.dve.op(lambda: nc.vector.tensor_tensor(out=A[:, 4:E - 3], in0=Bt[:, 2:E - 5], in1=Bt[:, 6:E - 1], op=ALU.add),
                             reads=[BB], writes=[AB])
                    s, sB = A, AB
                if g >= 3:
                    f.pool.op(lambda: nc.gpsimd.tensor_tensor(out=Bt[:, 8:E - 7], in0=A[:, 4:E - 11], in1=A[:, 12:E - 3], op=ALU.add),
                              reads=[AB], writes=[BB])
                    s, sB = Bt, BB
                pm, pmB = pm_r[it % 2]
                f.dve.op(lambda: nc.vector.scalar_tensor_tensor(out=pm[:, 0:n], in0=s[:, 8:8 + n], scalar=1.0 / w, in1=ue[:, g, 8:8 + n],
                                                                op0=ALU.mult, op1=ALU.subtract), reads=[sB, ueB], writes=[pmB])
                pmf, pmfB = pmf_r[it % 2]
                if t0 == slo:
                    f.dve.op(lambda: nc.vector.tensor_tensor(out=pmf[:, 0:8], in0=s[:, 8:16], in1=rc[:, ai, g, 0:8], op=ALU.mult),
                             reads=[sB, rcB], writes=[pmfB])
                    f.dve.op(lambda: nc.vector.tensor_tensor(out=pm[:, 0:8], in0=pmf[:, 0:8], in1=ue[:, g, 8:16], op=ALU.subtract),
                             reads=[pmfB, ueB], writes=[pmB])
                if t0 + n == shi:
                    f.dve.op(lambda: nc.vector.tensor_tensor(out=pmf[:, 8:16], in0=s[:, n:n + 8], in1=rc[:, ai, g, 8:16], op=ALU.mult),
                             reads=[sB, rcB], writes=[pmfB])
                    f.dve.op(lambda: nc.vector.tensor_tensor(out=pm[:, n - 8:n], in0=pmf[:, 8:16], in1=ue[:, g, n:n + 8], op=ALU.subtract),
                             reads=[pmfB, ueB], writes=[pmB])
                p, pB = kb.ps()
                f.pe.op(lambda: nc.tensor.matmul(p[:, 0:n], lhsT=pw[:, g, :], rhs=pm[:, 0:n], start=True, stop=True),
                        reads=[pwB, pmB], writes=[pB])
                f.dve.op(lambda: nc.vector.tensor_scalar(out=yc[:, g, 0:n], in0=p[:, 0:n], scalar1=psc[:, g:g + 1], scalar2=None, op0=ALU.mult),
                         reads=[pB, pscB], writes=[ycB])
            f.q_sp.dma(YC3[:, :, t0:t0 + n], yc[:, :, 0:n], reads=[ycB], writes=[YCb])
    f.barrier()


def phase_swa(kb, l, swa_sink):
    nc, f = kb.nc, kb.f
    QT, QTb = kb.dram["QT"]
    KT, KTb = kb.dram["KT"]
    Vd, Vdb = kb.dram["V"]
    YB, YBb = kb.dram["YB"]
    ident, identB = kb.c["ident"]
    scale = 64 ** -0.5
    with ExitStack() as es:
        mL, mLB = kb.sb(es, "mL", [128, 4, 128], BF16)
        mR, mRB = kb.sb(es, "mR", [128, 4, 128], BF16)
        for m, mB, cm, st in ((mL, mLB, 1, -1), (mR, mRB, -1, 1)):
            f.pool.op(lambda: nc.gpsimd.memset(m, 1.0), writes=[mB])
            f.pool.op(lambda: nc.gpsimd.affine_select(out=m, in_=m, pattern=[[0, 4], [st, 128]], compare_op=ALU.is_ge, fill=0.0,
                                                      base=0, channel_multiplier=cm), reads=[mB], writes=[mB])
        esk, eskB = kb.sb(es, "esk", [128, 8], F32)
        f.q_sp.dma(esk, swa_sink[l:l + 1, :].partition_broadcast(128), writes=[eskB])
        f.act.op(lambda: nc.scalar.activation(out=esk, in_=esk, func=AF.Exp), reads=[eskB], writes=[eskB])
        qg_r = kb.ring(es, "qg", [64, 4, S], BF16, 2)
        kj_r = kb.ring(es, "kj", [64, S], BF16, 2)
        vj_r = kb.ring(es, "vj", [128, 34, 65], BF16, 2)
        pT_r = kb.ring(es, "spT", [128, 512], BF16, 6)
        den_r = kb.ring(es, "sden", [128, 8], F32, 2)
        yb_r = kb.ring(es, "syb", [128, 256], BF16, 2)
        ybT_r = kb.ring(es, "sybT", [128, 2, 512], BF16, 2)
        for vj, vjB in vj_r:
            f.pool.op(lambda: nc.gpsimd.memset(vj[:, :, 64:65], 1.0), writes=[vjB])
        ip = 0
        ig = 0
        for b in range(NB):
            for j in range(2):
                it = b * 2 + j
                qg, qgB = qg_r[it % 2]
                kj, kjB = kj_r[it % 2]
                vj, vjB = vj_r[it % 2]
                f.q_sp.dma(qg, QT[j * 256:(j + 1) * 256, b * S:(b + 1) * S].rearrange("(h d) t -> d h t", d=64), writes=[qgB])
                f.q_sp.dma(kj, KT[j * 64:(j + 1) * 64, b * S:(b + 1) * S], writes=[kjB])
                f.q_sp.dma(vj[:, :, 0:64], Vd[b * S:(b + 1) * S, j * 64:(j + 1) * 64].rearrange("(c p) d -> p c d", p=128), writes=[vjB])
                for i in range(34):
                    if i < 32:
                        chunks = [c for c in (i - 1, i, i + 1) if 0 <= c < 32] + [32, 33]
                    else:
                        chunks = [32, 33]
                    po, poB, pidx = kb.pin()
                    pts = []
                    for c in chunks:
                        ps, psB = kb.ps()
                        f.pe.op(lambda: nc.tensor.matmul(ps[:, 0:512], lhsT=kj[:, c * 128:(c + 1) * 128], rhs=qg[:, :, i * 128:(i + 1) * 128],
                                                         start=True, stop=True), reads=[kjB, qgB], writes=[psB])
                        pT, pTB = pT_r[ip % 6]
                        ip += 1
                        f.act.op(lambda: nc.scalar.activation(out=pT, in_=ps[:, 0:512], func=AF.Exp, scale=scale), reads=[psB], writes=[pTB])
                        if i < 32 and c == i - 1:
                            f.pool.op(lambda: nc.gpsimd.tensor_tensor(out=pT, in0=pT, in1=mL.rearrange("p h q -> p (h q)"), op=ALU.mult),
                                      reads=[pTB, mLB], writes=[pTB])
                        if i < 31 and c == i + 1:
                            f.pool.op(lambda: nc.gpsimd.tensor_tensor(out=pT, in0=pT, in1=mR.rearrange("p h q -> p (h q)"), op=ALU.mult),
                                      reads=[pTB, mRB], writes=[pTB])
                        pts.append((pT, pTB, c))
                    for hh in range(4):
                        for ci, (pT, pTB, c) in enumerate(pts):
                            f.pe.op(lambda: nc.tensor.matmul(po[:, hh * 65:(hh + 1) * 65], lhsT=pT[:, hh * 128:(hh + 1) * 128], rhs=vj[:, c, :],
                                                             start=(ci == 0), stop=(ci == len(pts) - 1)), reads=[pTB, vjB], writes=[poB])
                    den, denB = den_r[ig % 2]
                    po3 = po[:, 0:260].rearrange("p (h c) -> p h c", c=65)
                    f.dve.op(lambda: nc.vector.tensor_tensor(out=den[:, 0:4], in0=po3[:, :, 64], in1=esk[:, 4 * j:4 * j + 4], op=ALU.add),
                             reads=[poB, eskB], writes=[denB])
                    f.dve.op(lambda: nc.vector.reciprocal(out=den[:, 4:8], in_=den[:, 0:4]), reads=[denB], writes=[denB])
                    yb, ybB = yb_r[ig % 2]
                    for hh in range(4):
                        f.dve.op(lambda: nc.vector.tensor_scalar(out=yb[:, hh * 64:(hh + 1) * 64], in0=po[:, hh * 65:hh * 65 + 64],
                                                                 scalar1=den[:, 4 + hh:5 + hh], scalar2=None, op0=ALU.mult),
                                 reads=[poB, denB], writes=[ybB])
                    kb.unpin(pidx)
                    grp0 = (i // 4) * 4 if i < 32 else 32
                    gsz = 4 if i < 32 else 2
                    ybT, ybTB = ybT_r[(it * 9 + (i // 4)) % 2]
                    pt, ptB = kb.pb()
                    for k in range(2):
                        f.pe.op(lambda: nc.tensor.transpose(pt[:, k * 128:(k + 1) * 128], yb[:, k * 128:(k + 1) * 128], ident),
                                reads=[ybB, identB], writes=[ptB])
                    off = (i - grp0) * 128
                    f.act.op(lambda: nc.scalar.copy(out=ybT[:, :, off:off + 128], in_=pt[:, 0:256].rearrange("p (k t) -> p k t", k=2)),
                             reads=[ptB], writes=[ybTB])
                    if i - grp0 == gsz - 1:
                        tq0 = b * S + grp0 * 128
                        f.q_sp.dma(YB[j * 256:(j + 1) * 256, tq0:tq0 + gsz * 128].rearrange("(k p) t -> p k t", p=128),
                                   ybT[:, :, 0:gsz * 128], reads=[ybTB], writes=[YBb])
                    ig += 1
    f.barrier()


def phase_mla(kb, l, I=None):
    nc, f = kb.nc, kb.f
    QM, QMb = kb.dram["QM"]
    KN, KNb = kb.dram["KN"]
    KR, KRb = kb.dram["KR"]
    VM, VMb = kb.dram["VM"]
    YD, YDb = kb.dram["YD"]
    onesf, onesfB = kb.c["onesf"]
    scale = 96 ** -0.5
    with ExitStack() as es:
        kh_r = kb.ring(es, "kh", [96, S], BF16, 2)
        qh_r = kb.ring(es, "qh", [96, S], BF16, 2)
        vh_r = kb.ring(es, "vh", [128, 34, 65], BF16, 2)
        pT_r = kb.ring(es, "mpT", [128, 512], BF16, 4)
        rd_r = kb.ring(es, "mrd", [65, 512], F32, 2)
        on_r = kb.ring(es, "mon", [64, 512], F32, 2)
        yd_r = kb.ring(es, "myd", [64, 512], BF16, 2)
        for vh, vhB in vh_r:
            f.pool.op(lambda: nc.gpsimd.memset(vh[:, :, 64:65], 1.0), writes=[vhB])
        if I is not None and "w_gu" in I:
            WBG, WBGb = kb.dram["WBG"]
            WBD, WBDb = kb.dram["WBD"]
            tg_r = kb.ring(es, "tg", [128, 8, 2048], BF16, 2)
            td_r = kb.ring(es, "td", [128, 8, 1024], BF16, 2)
            for e in range(NE):
                tg, tgB = tg_r[e % 2]
                td, tdB = td_r[e % 2]
                f.q_pool.dma(tg, I["w_gu"][l, e].rearrange("(p k) c -> p k c", k=8), writes=[tgB])
                f.q_pool.dma(td, I["w_down"][l, e].rearrange("(p k) c -> p k c", k=8), writes=[tdB])
                f.q_pool.dma(WBG[e * 128:(e + 1) * 128, :], tg.rearrange("p k c -> p (k c)"), reads=[tgB], writes=[WBGb])
                f.q_pool.dma(WBD[e * 128:(e + 1) * 128, :], td.rearrange("p k c -> p (k c)"), reads=[tdB], writes=[WBDb])
        ip = 0
        iq = 0
        for b in range(NB):
            for h in range(8):
                it = b * 8 + h
                kh, khB = kh_r[it % 2]
                qh, qhB = qh_r[it % 2]
                vh, vhB = vh_r[it % 2]
                f.q_sp.dma(kh[0:64, :], KN[h, :, b * S:(b + 1) * S], writes=[khB])
                f.q_sp.dma(kh[64:96, :], KR[:, b * S:(b + 1) * S], writes=[khB])
                f.q_sp.dma(qh, QM[h, :, b * S:(b + 1) * S], writes=[qhB])
                f.q_sp.dma(vh[:, :, 0:64], VM[b * S:(b + 1) * S, h * 64:(h + 1) * 64].rearrange("(c p) d -> p c d", p=128), writes=[vhB])
                for qi in range(9):
                    q0 = qi * 512
                    n = 512 if qi < 8 else 256
                    chunks = list(range(34)) if qi < 8 else [32, 33]
                    po, poB, pidx = kb.pin()
                    pend = []
                    LAG = 2
                    for ci in range(len(chunks)):
                        c = chunks[ci]
                        ps, psB = kb.ps()
                        f.pe.op(lambda: nc.tensor.matmul(ps[:, 0:n], lhsT=kh[:, c * 128:(c + 1) * 128], rhs=qh[:, q0:q0 + n], start=True, stop=True),
                                reads=[khB, qhB], writes=[psB])
                        pT, pTB = pT_r[ip % 4]
                        ip += 1
                        f.act.op(lambda: nc.scalar.activation(out=pT[:, 0:n], in_=ps[:, 0:n], func=AF.Exp, scale=scale), reads=[psB], writes=[pTB])
                        pend.append((pT, pTB, c, ci))
                        if len(pend) > LAG:
                            pT2, pT2B, c2, ci2 = pend.pop(0)
                            f.pe.op(lambda: nc.tensor.matmul(po[0:65, 0:n], lhsT=vh[:, c2, :], rhs=pT2[:, 0:n], start=(ci2 == 0), stop=(ci2 == len(chunks) - 1)),
                                    reads=[vhB, pT2B], writes=[poB])
                    while pend:
                        pT2, pT2B, c2, ci2 = pend.pop(0)
                        f.pe.op(lambda: nc.tensor.matmul(po[0:65, 0:n], lhsT=vh[:, c2, :], rhs=pT2[:, 0:n], start=(ci2 == 0), stop=(ci2 == len(chunks) - 1)),
                                reads=[vhB, pT2B], writes=[poB])
                    rd, rdB = rd_r[iq % 2]
                    on, onB = on_r[iq % 2]
                    yd, ydB = yd_r[iq % 2]
                    iq += 1
                    f.dve.op(lambda: nc.vector.reciprocal(out=rd[64:65, 0:n], in_=po[64:65, 0:n]), reads=[poB], writes=[rdB])
                    f.act.op(lambda: nc.scalar.copy(out=on[:, 0:n], in_=po[0:64, 0:n]), reads=[poB], writes=[onB])
                    kb.unpin(pidx)
                    pbc, pbcB = kb.ps()
                    f.pe.op(lambda: nc.tensor.matmul(pbc[0:64, 0:n], lhsT=onesf[64:65, 0:64], rhs=rd[64:65, 0:n], start=True, stop=True),
                            reads=[onesfB, rdB], writes=[pbcB])
                    f.dve.op(lambda: nc.vector.tensor_tensor(out=yd[:, 0:n], in0=on[:, 0:n], in1=pbc[0:64, 0:n], op=ALU.mult),
                             reads=[onB, pbcB], writes=[ydB])
                    f.q_sp.dma(YD[h * 64:(h + 1) * 64, b * S + q0:b * S + q0 + n], yd[:, 0:n], reads=[ydB], writes=[YDb])
    f.barrier()


def resid_ln(kb, es_bufs, src_y, src_yB, ysrc_is_psum_halves, xs, xsB, gt, gtB, lg, lgB, lb, lbB, dst_ap, dstB, idx):
    nc, f = kb.nc, kb.f
    t_r, st_r, o_r = es_bufs
    t, tB = t_r[idx % len(t_r)]
    st, stB = st_r[idx % len(st_r)]
    o, oB = o_r[idx % len(o_r)]
    for hf in range(2):
        ya, yaB = src_y[hf]
        f.dve.op(lambda: nc.vector.tensor_tensor(out=t[:, hf * 512:(hf + 1) * 512], in0=ya, in1=gt[:, hf * 512:(hf + 1) * 512], op=ALU.mult),
                 reads=[yaB, gtB], writes=[tB])
    f.dve.op(lambda: nc.vector.scalar_tensor_tensor(out=t, in0=xs, scalar=ALPHA, in1=t, op0=ALU.mult, op1=ALU.add),
             reads=[xsB, tB], writes=[tB])
    mean, rstd = ln_tile(kb, t, tB, st, stB)
    f.dve.op(lambda: nc.vector.tensor_scalar(out=o, in0=t, scalar1=mean, scalar2=rstd, op0=ALU.subtract, op1=ALU.mult),
             reads=[tB, stB], writes=[oB])
    f.pool.op(lambda: nc.gpsimd.tensor_tensor(out=o, in0=o, in1=lg, op=ALU.mult), reads=[oB, lgB], writes=[oB])
    f.pool.op(lambda: nc.gpsimd.tensor_tensor(out=o, in0=o, in1=lb, op=ALU.add), reads=[oB, lbB], writes=[oB])
    f.q_sp.dma(dst_ap, o, reads=[oB], writes=[dstB])


def phase_merge(kb, l, xsrc, I, ADA, XA, skip_ctx=False):
    nc, f = kb.nc, kb.f
    HT, HTb = kb.dram["HT"]
    XAb = kb.dram["XA"][1]
    with ExitStack() as es:
        wg, wgB = kb.sb(es, "wg", [128, 4, 8, D], BF16)
        for i in range(4):
            f.q_pool.dma(wg[:, i], I["w_gate"][l, i].rearrange("(k p) c -> p k c", p=128), writes=[wgB])
        wb, wbB = kb.sb(es, "wbr", [128, 4, 4, D], BF16)
        for i in range(4):
            f.q_pool.dma(wb[:, i], I["w_branch"][l, i].rearrange("(k p) c -> p k c", p=128), writes=[wbB])
        wo, woB = kb.sb(es, "wo", [128, 8, D], BF16)
        f.q_pool.dma(wo, I["w_out"][l].rearrange("(k p) c -> p k c", p=128), writes=[woB])
        bg, bgB = kb.sb(es, "bg", [128, 4, 8], F32)
        for i in range(4):
            f.q_sp.dma(bg[:, i, :], I["b_gate"][l, i].rearrange("(k p) -> p k", p=128), writes=[bgB])
        g1 = []
        for r in range(3):
            g, gB = kb.sb(es, f"g1_{r}", [128, D], F32)
            load_bcast(kb, g, gB, ADA[l, r:r + 1, 2 * D:3 * D])
            g1.append((g, gB))
        lg, lgB = kb.sb(es, "ln1g", [128, D], F32)
        lb, lbB = kb.sb(es, "ln1b", [128, D], F32)
        load_bcast(kb, lg, lgB, I["ln1_g"][l:l + 1, :])
        load_bcast(kb, lb, lbB, I["ln1_b"][l:l + 1, :])
        hT_r = kb.ring(es, "mhT", [128, 8, 512], BF16, 1)
        ys_r = kb.ring(es, "mys", [128, 16, 512], BF16, 1)
        mT_r = kb.ring(es, "mmT", [128, 8, 512], BF16, 1)
        sg_r = kb.ring(es, "msg", [128, 512], F32, 2)
        tmp_r = kb.ring(es, "mtmp", [128, 512], F32, 2)
        acc_r = kb.ring(es, "macc", [128, 512], F32, 2)
        xs_r = kb.ring(es, "mxs", [128, D], F32, 2)
        rl = (kb.ring(es, "mt", [128, D], F32, 1), kb.ring(es, "mst", [128, 16], F32, 2), kb.ring(es, "mo", [128, D], F32, 2))
        HT3 = HT.rearrange("(k p) t -> p k t", p=128)
        idx = 0
        for ti, (t0, n, b, isctx, pos) in enumerate(tiles_all()):
            if isctx and skip_ctx:
                continue
            r = 2 if isctx else b
            hT, hTB = hT_r[0]
            ys, ysB = ys_r[0]
            mT, mTB = mT_r[0]
            f.q_sp.dma(hT[:, :, 0:n], HT3[:, :, t0:t0 + n], writes=[hTB])
            for i, nm in enumerate(("YA", "YB", "YC", "YD")):
                f.q_sp.dma(ys[:, i * 4:(i + 1) * 4, 0:n], kb.dram[nm][0].rearrange("(k p) t -> p k t", p=128)[:, :, t0:t0 + n], writes=[ysB])
            for oc in range(8):
                acc, accB = acc_r[oc % 2]
                for i in range(4):
                    pg, pgB = kb.ps()
                    for k in range(8):
                        f.pe.op(lambda: nc.tensor.matmul(pg[:, 0:n], lhsT=wg[:, i, k, oc * 128:(oc + 1) * 128], rhs=hT[:, k, 0:n],
                                                         start=(k == 0), stop=(k == 7)), reads=[wgB, hTB], writes=[pgB])
                    sg, sgB = sg_r[i % 2]
                    f.act.op(lambda: nc.scalar.activation(out=sg[:, 0:n], in_=pg[:, 0:n], func=AF.Sigmoid, bias=bg[:, i, oc:oc + 1]),
                             reads=[pgB, bgB], writes=[sgB])
                    pbr, pbrB = kb.ps()
                    for k in range(4):
                        f.pe.op(lambda: nc.tensor.matmul(pbr[:, 0:n], lhsT=wb[:, i, k, oc * 128:(oc + 1) * 128], rhs=ys[:, i * 4 + k, 0:n],
                                                         start=(k == 0), stop=(k == 3)), reads=[wbB, ysB], writes=[pbrB])
                    if i == 0:
                        f.dve.op(lambda: nc.vector.tensor_tensor(out=acc[:, 0:n], in0=sg[:, 0:n], in1=pbr[:, 0:n], op=ALU.mult),
                                 reads=[sgB, pbrB], writes=[accB])
                    else:
                        tmp, tmpB = tmp_r[i % 2]
                        f.dve.op(lambda: nc.vector.tensor_tensor(out=tmp[:, 0:n], in0=sg[:, 0:n], in1=pbr[:, 0:n], op=ALU.mult),
                                 reads=[sgB, pbrB], writes=[tmpB])
                        if i < 3:
                            f.pool.op(lambda: nc.gpsimd.tensor_tensor(out=acc[:, 0:n], in0=acc[:, 0:n], in1=tmp[:, 0:n], op=ALU.add),
                                      reads=[accB, tmpB], writes=[accB])
                        else:
                            f.pool.op(lambda: nc.gpsimd.tensor_tensor(out=mT[:, oc, 0:n], in0=acc[:, 0:n], in1=tmp[:, 0:n], op=ALU.add),
                                      reads=[accB, tmpB], writes=[mTB])
            g, gB = g1[r]
            for j in range(n // 128):
                xs, xsB = xs_r[idx % 2]
                f.q_sp.dma(xs, xsrc(t0 + j * 128, 128), writes=[xsB])
                halves = []
                for hf in range(2):
                    po, poB = kb.ps()
                    for k in range(8):
                        f.pe.op(lambda: nc.tensor.matmul(po[:, 0:512], lhsT=mT[:, k, j * 128:(j + 1) * 128], rhs=wo[:, k, hf * 512:(hf + 1) * 512],
                                                         start=(k == 0), stop=(k == 7)), reads=[mTB, woB], writes=[poB])
                    halves.append((po[:, 0:512], poB))
                resid_ln(kb, rl, halves, None, True, xs, xsB, g, gB, lg, lgB, lb, lbB, XA[t0 + j * 128:t0 + (j + 1) * 128, :], XAb, idx)
                idx += 1
    f.barrier()


NT128 = T // 128
BIGIDX = 4.0e6


def phase_moe(kb, l, I, ADA, XA, XB, out, last):
    nc, f = kb.nc, kb.f
    dr = kb.dram
    H2, H2b = dr["H2"]
    XG, XGb = dr["XG"]
    YG, YGb = dr["YG"]
    XAb = dr["XA"][1]
    ident, identB = kb.c["ident"]
    onesb, onesbB = kb.c["onesb"]
    onesf, onesfB = kb.c["onesf"]
    with ExitStack() as es0:
        TK, TKB = kb.sb(es0, "TK", [128, NT128, 12], F32)
        WK, WKB = kb.sb(es0, "WK", [128, NT128, 4], F32)
        DSTI, DSTIB = kb.sb(es0, "DSTI", [128, NT128, 4], I32)
        carry, carryB = kb.sb(es0, "carry", [128, 32], F32)
        eio, eioB = kb.sb(es0, "eio", [128, 32], F32)
        eioi, eioiB = kb.sb(es0, "eioi", [128, 32], I32)
        f.pool.op(lambda: nc.gpsimd.iota(eioi, pattern=[[1, 32]], base=0, channel_multiplier=0), writes=[eioiB])
        f.dve.op(lambda: nc.vector.tensor_copy(out=eio, in_=eioi), reads=[eioiB], writes=[eioB])
        f.pool.op(lambda: nc.gpsimd.memset(carry, 0.0), writes=[carryB])
        IDXG, IDXGB = kb.sb(es0, "IDXG", [128, NBLK], I32)
        BEI, BEIB = kb.sb(es0, "BEI", [128, NBLK], I32)
        IDX2, IDX2B = kb.sb(es0, "IDX2", [128, 2, NBLK], I32)
        BE, BEB = kb.sb(es0, "BE", [128, NBLK], F32)
        with ExitStack() as es:
            U, UB = kb.sb(es, "U", [128, 128], BF16)
            f.pool.op(lambda: nc.gpsimd.memset(U, 1.0), writes=[UB])
            f.pool.op(lambda: nc.gpsimd.affine_select(out=U, in_=U, pattern=[[1, 128]], compare_op=ALU.is_gt, fill=0.0, base=0,
                                                      channel_multiplier=-1), reads=[UB], writes=[UB])
            wr, wrB = kb.sb(es, "wr", [128, 8, 32], BF16)
            f.q_pool.dma(wr, I["router_w"][l].rearrange("(k p) e -> p k e", p=128), writes=[wrB])
            rb, rbB = kb.sb(es, "rb", [1, 32], BF16)
            f.q_pool.dma(rb, I["router_b"][l:l + 1, :], writes=[rbB])
            mods = []
            for r in range(3):
                sc, scB = kb.sb(es, f"sc2_{r}", [128, D], F32)
                sh, shB = kb.sb(es, f"sh2_{r}", [128, D], F32)
                load_bcast(kb, sh, shB, ADA[l, r:r + 1, 3 * D:4 * D])
                load_bcast(kb, sc, scB, ADA[l, r:r + 1, 4 * D:5 * D])
                f.pool.op(lambda: nc.gpsimd.tensor_scalar(out=sc, in0=sc, scalar1=1.0, scalar2=None, op0=ALU.add), reads=[scB], writes=[scB])
                mods.append((sc, scB, sh, shB))
            xs_r = kb.ring(es, "rxs", [128, D], F32, 2)
            st_r = kb.ring(es, "rst", [128, 16], F32, 2)
            xn_r = kb.ring(es, "rxn", [128, D], F32, 2)
            hb_r = kb.ring(es, "rhb", [128, D], BF16, 2)
            hT_r = kb.ring(es, "rhT", [128, 8, 128], BF16, 2)
            lg_r = kb.ring(es, "rlg", [128, 32], F32, 2)
            t8_r = kb.ring(es, "rt8", [128, 16], F32, 2)
            M_r = kb.ring(es, "rM", [128, 32], BF16, 2)
            rk_r = kb.ring(es, "rrk", [128, 32], F32, 2)
            jk_r = kb.ring(es, "rjk", [128, 32], F32, 2)
            for it in range(NT128):
                t0 = it * 128
                b, o = divmod(t0, S)
                r = 2 if o >= L else b
                sc, scB, sh, shB = mods[r]
                xs, xsB = xs_r[it % 2]
                st, stB = st_r[it % 2]
                xn, xnB = xn_r[it % 2]
                hb, hbB = hb_r[it % 2]
                hT, hTB = hT_r[it % 2]
                f.q_sp.dma(xs, XA[t0:t0 + 128, :], writes=[xsB])
                mean, rstd = ln_tile(kb, xs, xsB, st, stB)
                f.dve.op(lambda: nc.vector.tensor_scalar(out=xn, in0=xs, scalar1=mean, scalar2=rstd, op0=ALU.subtract, op1=ALU.mult),
                         reads=[xsB, stB], writes=[xnB])
                f.pool.op(lambda: nc.gpsimd.tensor_tensor(out=xn, in0=xn, in1=sc, op=ALU.mult), reads=[xnB, scB], writes=[xnB])
                f.dve.op(lambda: nc.vector.tensor_tensor(out=hb, in0=xn, in1=sh, op=ALU.add), reads=[xnB, shB], writes=[hbB])
                f.q_sp.dma(H2[t0:t0 + 128, :], hb, reads=[hbB], writes=[H2b])
                pt, ptB = kb.pb()
                for k in range(8):
                    f.pe.op(lambda: nc.tensor.transpose(pt[:, k * 128:(k + 1) * 128], hb[:, k * 128:(k + 1) * 128], ident),
                            reads=[hbB, identB], writes=[ptB])
                f.act.op(lambda: nc.scalar.copy(out=hT, in_=pt.rearrange("p (k t) -> p k t", k=8)), reads=[ptB], writes=[hTB])
                pl, plB = kb.ps()
                for k in range(8):
                    f.pe.op(lambda: nc.tensor.matmul(pl[:, 0:32], lhsT=hT[:, k, :], rhs=wr[:, k, :], start=(k == 0), stop=False),
                            reads=[hTB, wrB], writes=[plB])
                f.pe.op(lambda: nc.tensor.matmul(pl[:, 0:32], lhsT=onesb[0:1, :], rhs=rb, start=False, stop=True),
                        reads=[onesbB, rbB], writes=[plB])
                lg, lgB = lg_r[it % 2]
                f.act.op(lambda: nc.scalar.copy(out=lg, in_=pl[:, 0:32]), reads=[plB], writes=[lgB])
                t8, t8B = t8_r[it % 2]
                f.dve.op(lambda: nc.vector.max(out=t8[:, 0:8], in_=lg), reads=[lgB], writes=[t8B])
                M, MB = M_r[it % 2]
                f.dve.op(lambda: nc.vector.tensor_scalar(out=M, in0=lg, scalar1=t8[:, 3:4], scalar2=None, op0=ALU.is_ge),
                         reads=[lgB, t8B], writes=[MB])
                pr, prB = kb.ps()
                f.pe.op(lambda: nc.tensor.matmul(pr[:, 0:32], lhsT=U, rhs=M, start=True, stop=True), reads=[UB, MB], writes=[prB])
                f.pe.op(lambda: nc.tensor.matmul(pr[:, 32:64], lhsT=onesb, rhs=M, start=True, stop=True), reads=[onesbB, MB], writes=[prB])
                rk, rkB = rk_r[it % 2]
                f.dve.op(lambda: nc.vector.tensor_tensor(out=rk, in0=pr[:, 0:32], in1=carry, op=ALU.add), reads=[prB, carryB], writes=[rkB])
                f.dve.op(lambda: nc.vector.tensor_tensor(out=carry, in0=pr[:, 32:64], in1=carry, op=ALU.add), reads=[prB, carryB], writes=[carryB])
                jk, jkB = jk_r[it % 2]
                for k in range(4):
                    f.dve.op(lambda: nc.vector.scalar_tensor_tensor(out=jk, in0=lg, scalar=t8[:, k:k + 1], in1=rk, op0=ALU.is_equal, op1=ALU.mult,
                                                                    accum_out=TK[:, it, 4 + k:5 + k]), reads=[lgB, t8B, rkB], writes=[jkB, TKB])
                    f.dve.op(lambda: nc.vector.scalar_tensor_tensor(out=jk, in0=lg, scalar=t8[:, k:k + 1], in1=eio, op0=ALU.is_equal, op1=ALU.mult,
                                                                    accum_out=TK[:, it, 8 + k:9 + k]), reads=[lgB, t8B, eioB], writes=[jkB, TKB])
                f.dve.op(lambda: nc.vector.tensor_scalar(out=t8[:, 8:9], in0=t8[:, 0:1], scalar1=-1.0, scalar2=None, op0=ALU.mult),
                         reads=[t8B], writes=[t8B])
                f.act.op(lambda: nc.scalar.activation(out=TK[:, it, 0:4], in_=t8[:, 0:4], func=AF.Exp, bias=t8[:, 8:9], accum_out=t8[:, 9:10]),
                         reads=[t8B], writes=[TKB, t8B])
                f.dve.op(lambda: nc.vector.reciprocal(out=t8[:, 10:11], in_=t8[:, 9:10]), reads=[t8B], writes=[t8B])
                f.dve.op(lambda: nc.vector.tensor_scalar(out=WK[:, it, :], in0=TK[:, it, 0:4], scalar1=t8[:, 10:11], scalar2=None, op0=ALU.mult),
                         reads=[TKB, t8B], writes=[WKB])
        f.barrier()
        with ExitStack() as es:
            MAXB = T // BLK + 1
            i256i, i256iB = kb.sb(es, "i256i", [128, NBLK], I32)
            i256, i256B = kb.sb(es, "i256", [128, NBLK], F32)
            f.pool.op(lambda: nc.gpsimd.iota(i256i, pattern=[[BLK, NBLK]], base=0, channel_multiplier=0), writes=[i256iB])
            f.dve.op(lambda: nc.vector.tensor_copy(out=i256, in_=i256i), reads=[i256iB], writes=[i256B])
            cmp, cmpB = kb.sb(es, "cmpA", [128, 32, MAXB], F32)
            f.dve.op(lambda: nc.vector.tensor_tensor(out=cmp, in0=carry.unsqueeze(2).to_broadcast([128, 32, MAXB]),
                                                     in1=i256[:, 0:MAXB].unsqueeze(1).to_broadcast([128, 32, MAXB]), op=ALU.is_gt),
                     reads=[carryB, i256B], writes=[cmpB])
            pe_a, pe_aB = kb.sb(es, "pend_a", [128, 32], F32)
            pe_b, pe_bB = kb.sb(es, "pend_b", [128, 32], F32)
            pst, pstB = kb.sb(es, "pstart", [128, 32], F32)
            pad, padB = kb.sb(es, "padded", [128, 32], F32)
            f.dve.op(lambda: nc.vector.reduce_sum(out=pad, in_=cmp, axis=AX.X), reads=[cmpB], writes=[padB])
            f.dve.op(lambda: nc.vector.tensor_scalar(out=pad, in0=pad, scalar1=float(BLK), scalar2=None, op0=ALU.mult), reads=[padB], writes=[padB])
            f.dve.op(lambda: nc.vector.tensor_copy(out=pe_a, in_=pad), reads=[padB], writes=[pe_aB])
            cur, curB, nxt, nxtB = pe_a, pe_aB, pe_b, pe_bB
            for s in (1, 2, 4, 8, 16):
                f.dve.op(lambda: nc.vector.tensor_copy(out=nxt[:, 0:s], in_=cur[:, 0:s]), reads=[curB], writes=[nxtB])
                f.dve.op(lambda: nc.vector.tensor_tensor(out=nxt[:, s:32], in0=cur[:, s:32], in1=cur[:, 0:32 - s], op=ALU.add),
                         reads=[curB], writes=[nxtB])
                cur, curB, nxt, nxtB = nxt, nxtB, cur, curB
            pend, pendB = cur, curB
            f.dve.op(lambda: nc.vector.tensor_tensor(out=pst, in0=pend, in1=pad, op=ALU.subtract), reads=[pendB, padB], writes=[pstB])
            cmp2, cmp2B = kb.sb(es, "cmpB", [128, NBLK, 32], F32)
            f.dve.op(lambda: nc.vector.tensor_tensor(out=cmp2, in0=pend.unsqueeze(1).to_broadcast([128, NBLK, 32]),
                                                     in1=i256.unsqueeze(2).to_broadcast([128, NBLK, 32]), op=ALU.is_le),
                     reads=[pendB, i256B], writes=[cmp2B])
            f.dve.op(lambda: nc.vector.reduce_sum(out=BE, in_=cmp2, axis=AX.X), reads=[cmp2B], writes=[BEB])
            f.dve.op(lambda: nc.vector.tensor_scalar(out=BE, in0=BE, scalar1=31.0, scalar2=None, op0=ALU.min), reads=[BEB], writes=[BEB])
            chg, chgB = kb.sb(es, "chg", [128, NBLK], F32)
            f.pool.op(lambda: nc.gpsimd.memset(chg[:, 0:1], 1.0), writes=[chgB])
            f.dve.op(lambda: nc.vector.tensor_tensor(out=chg[:, 1:NBLK], in0=BE[:, 1:NBLK], in1=BE[:, 0:NBLK - 1], op=ALU.not_equal),
                     reads=[BEB], writes=[chgB])
            base, baseB = kb.sb(es, "ibase", [128, NBLK], F32)
            f.dve.op(lambda: nc.vector.tensor_scalar(out=base, in0=BE, scalar1=128.0, scalar2=-BIGIDX, op0=ALU.mult, op1=ALU.add),
                     reads=[BEB], writes=[baseB])
            f.dve.op(lambda: nc.vector.tensor_tensor(out=base, in0=base, in1=chg, op=ALU.mult), reads=[baseB, chgB], writes=[baseB])
            pio_i, pio_iB = kb.sb(es, "pio_i", [128, 8], I32)
            pio, pioB = kb.sb(es, "pio", [128, 8], F32)
            f.pool.op(lambda: nc.gpsimd.iota(pio_i, pattern=[[128, 8]], base=0, channel_multiplier=1), writes=[pio_iB])
            f.dve.op(lambda: nc.vector.tensor_copy(out=pio, in_=pio_i), reads=[pio_iB], writes=[pioB])
            f.dve.op(lambda: nc.vector.tensor_scalar(out=base, in0=base, scalar1=BIGIDX, scalar2=None, op0=ALU.add), reads=[baseB], writes=[baseB])
            idxf, idxfB = kb.sb(es, "idxf", [128, NBLK], F32)
            f.dve.op(lambda: nc.vector.tensor_scalar(out=idxf, in0=base, scalar1=pio[:, 0:1], scalar2=None, op0=ALU.add),
                     reads=[baseB, pioB], writes=[idxfB])
            f.dve.op(lambda: nc.vector.tensor_copy(out=IDXG, in_=idxf), reads=[idxfB], writes=[IDXGB])
            idx2, idx2B = kb.sb(es, "idx2", [128, NBLK], F32)
            f.dve.op(lambda: nc.vector.tensor_scalar(out=idx2, in0=idxf, scalar1=2.0, scalar2=None, op0=ALU.mult), reads=[idxfB], writes=[idx2B])
            f.dve.op(lambda: nc.vector.tensor_copy(out=IDX2[:, 0, :], in_=idx2), reads=[idx2B], writes=[IDX2B])
            f.dve.op(lambda: nc.vector.tensor_scalar(out=idx2, in0=idx2, scalar1=1.0, scalar2=None, op0=ALU.add), reads=[idx2B], writes=[idx2B])
            f.dve.op(lambda: nc.vector.tensor_copy(out=IDX2[:, 1, :], in_=idx2), reads=[idx2B], writes=[IDX2B])
            f.dve.op(lambda: nc.vector.tensor_scalar(out=idxf, in0=base, scalar1=1.0 / 128.0, scalar2=float(l * NE), op0=ALU.mult, op1=ALU.add),
                     reads=[baseB], writes=[idxfB])
            f.dve.op(lambda: nc.vector.tensor_copy(out=BEI, in_=idxf), reads=[idxfB], writes=[BEIB])
            zt, ztB = kb.sb(es, "zt", [128, 8192], BF16)
            f.pool.op(lambda: nc.gpsimd.memset(zt, 0.0), writes=[ztB])
            XGf = XG.rearrange("(a p r) c -> a p (r c)", p=128, r=8)
            for a in range(NROWS // 1024):
                f.q_sp.dma(XGf[a], zt, reads=[ztB], writes=[XGb])
            dst, dstB = kb.sb(es, "dstf", [128, NT128, 4], F32)
            jk_r = kb.ring(es, "cjk", [128, 32], F32, 2)
            for it in range(NT128):
                jk, jkB = jk_r[it % 2]
                for k in range(4):
                    f.dve.op(lambda: nc.vector.scalar_tensor_tensor(out=jk, in0=eio, scalar=TK[:, it, 8 + k:9 + k], in1=pst, op0=ALU.is_equal,
                                                                    op1=ALU.mult, accum_out=dst[:, it, k:k + 1]),
                             reads=[eioB, TKB, pstB], writes=[jkB, dstB])
            f.dve.op(lambda: nc.vector.tensor_tensor(out=dst, in0=dst, in1=TK[:, :, 4:8], op=ALU.add), reads=[dstB, TKB], writes=[dstB])
            f.dve.op(lambda: nc.vector.tensor_copy(out=DSTI, in_=dst), reads=[dstB], writes=[DSTIB])
            f.barrier()
            hb_r = kb.ring(es, "shb", [128, D], BF16, 3)
            for it in range(NT128):
                hb, hbB = hb_r[it % 3]
                f.q_sp.dma(hb, H2[it * 128:(it + 1) * 128, :], writes=[hbB])
                for k in range(4):
                    f.q_pool.dma(None, None, reads=[hbB, DSTIB], writes=[XGb],
                                 fn=lambda g: g.indirect_dma_start(out=XG, out_offset=bass.IndirectOffsetOnAxis(ap=DSTI[:, it, k:k + 1], axis=0),
                                                                   in_=hb, in_offset=None, bounds_check=kb.breg(NROWS - 1), oob_is_err=False))
        f.barrier()
        with ExitStack() as es:
            wgu2, wguB = kb.sb(es, "wgu", [128, 8 * 2048], BF16)
            wdn2, wdnB = kb.sb(es, "wdn", [128, 8 * D], BF16)
            wgu = wgu2.rearrange("p (k c) -> p k c", k=8)
            wdn = wdn2.rearrange("p (k c) -> p k c", k=8)
            WBG = dr["WBG"][0]
            WBD = dr["WBD"][0]
            bgu, bguB = kb.sb(es, "bgu", [128, 2048], BF16)
            bdn, bdnB = kb.sb(es, "bdn", [128, D], BF16)
            WGU2 = I["w_gu"].rearrange("l e (q k) c -> (l e q) k c", k=4)
            WDN2 = I["w_down"].rearrange("l e (q k) c -> (l e q) k c", k=4)
            BGU2 = I["b_gu"].rearrange("l e c -> (l e) c")
            BDN2 = I["b_down"].rearrange("l e c -> (l e) c")
            xr_r = kb.ring(es, "bxr", [128, 2, D], BF16, 2)
            xT_r = kb.ring(es, "bxT", [128, 8, BLK], BF16, 2)
            aT_r = kb.ring(es, "baT", [128, 8, BLK], BF16, 2)
            g_r = kb.ring(es, "bg", [128, BLK], F32, 2)
            sg_r = kb.ring(es, "bsg", [128, BLK], F32, 2)
            u_r = kb.ring(es, "bu", [128, BLK], F32, 2)
            yo_r = kb.ring(es, "byo", [128, 2, D], F32, 2)
            for bi in range(NBLK):
                f.q_pool.dma(None, None, reads=[IDXGB], writes=[wguB],
                             fn=lambda g: g.indirect_dma_start(out=wgu2, out_offset=None, in_=WBG,
                                                               in_offset=bass.IndirectOffsetOnAxis(ap=IDXG[:, bi:bi + 1], axis=0),
                                                               bounds_check=kb.breg(NE * 128 - 1), oob_is_err=False))
                f.q_pool.dma(None, None, reads=[BEIB], writes=[bguB],
                             fn=lambda g: g.indirect_dma_start(out=bgu, out_offset=None, in_=BGU2,
                                                               in_offset=bass.IndirectOffsetOnAxis(ap=BEI[:, bi:bi + 1], axis=0),
                                                               bounds_check=kb.breg((l + 1) * NE - 1), oob_is_err=False))
                f.q_pool.dma(None, None, reads=[IDXGB], writes=[wdnB],
                             fn=lambda g: g.indirect_dma_start(out=wdn2, out_offset=None, in_=WBD,
                                                               in_offset=bass.IndirectOffsetOnAxis(ap=IDXG[:, bi:bi + 1], axis=0),
                                                               bounds_check=kb.breg(NE * 128 - 1), oob_is_err=False))
                f.q_pool.dma(None, None, reads=[BEIB], writes=[bdnB],
                             fn=lambda g: g.indirect_dma_start(out=bdn, out_offset=None, in_=BDN2,
                                                               in_offset=bass.IndirectOffsetOnAxis(ap=BEI[:, bi:bi + 1], axis=0),
                                                               bounds_check=kb.breg((l + 1) * NE - 1), oob_is_err=False))
                xr, xrB = xr_r[bi % 2]
                f.q_sp.dma(xr, XG[bi * BLK:(bi + 1) * BLK, :].rearrange("(j p) c -> p j c", p=128), writes=[xrB])
                xT, xTB = xT_r[bi % 2]
                for j in range(2):
                    pt, ptB = kb.pb()
                    for k in range(8):
                        f.pe.op(lambda: nc.tensor.transpose(pt[:, k * 128:(k + 1) * 128], xr[:, j, :].rearrange("r (p k) -> r k p", k=8)[:, k, :], ident),
                                reads=[xrB, identB], writes=[ptB])
                    f.act.op(lambda: nc.scalar.copy(out=xT[:, :, j * 128:(j + 1) * 128], in_=pt.rearrange("p (k t) -> p k t", k=8)),
                             reads=[ptB], writes=[xTB])
                aT, aTB = aT_r[bi % 2]
                for oc in range(8):
                    pz = []
                    for half in range(2):
                        p, pB = kb.ps()
                        for k in range(8):
                            wsl = wgu[:, k, half * 1024:(half + 1) * 1024].rearrange("p (m j) -> p j m", j=8)[:, oc, :]
                            f.pe.op(lambda: nc.tensor.matmul(p[:, 0:BLK], lhsT=wsl, rhs=xT[:, k, :], start=(k == 0), stop=False),
                                    reads=[wguB, xTB], writes=[pB])
                        bsl = bgu[0:1, half * 1024:(half + 1) * 1024].rearrange("p (m j) -> p j m", j=8)[:, oc, :]
                        f.pe.op(lambda: nc.tensor.matmul(p[:, 0:BLK], lhsT=bsl, rhs=kb.c["onesrow"][0][0:1, 0:BLK],
                                                         start=False, stop=True), reads=[bguB, kb.c["onesrow"][1]], writes=[pB])
                        pz.append((p, pB))
                    (pg, pgB), (pu, puB) = pz
                    g, gB = g_r[oc % 2]
                    sg, sgB = sg_r[oc % 2]
                    u, uB = u_r[oc % 2]
                    f.dve.op(lambda: nc.vector.tensor_scalar(out=g, in0=pg[:, 0:BLK], scalar1=7.0, scalar2=None, op0=ALU.min), reads=[pgB], writes=[gB])
                    f.act.op(lambda: nc.scalar.activation(out=sg, in_=g, func=AF.Sigmoid, scale=1.702), reads=[gB], writes=[sgB])
                    f.dve.op(lambda: nc.vector.tensor_scalar(out=u, in0=pu[:, 0:BLK], scalar1=-7.0, scalar2=7.0, op0=ALU.max, op1=ALU.min),
                             reads=[puB], writes=[uB])
                    f.dve.op(lambda: nc.vector.scalar_tensor_tensor(out=u, in0=u, scalar=1.0, in1=g, op0=ALU.add, op1=ALU.mult),
                             reads=[uB, gB], writes=[uB])
                    f.pool.op(lambda: nc.gpsimd.tensor_tensor(out=aT[:, oc, :], in0=u, in1=sg, op=ALU.mult), reads=[uB, sgB], writes=[aTB])
                yo, yoB = yo_r[bi % 2]
                for j in range(2):
                    for hf in range(2):
                        p, pB = kb.ps()
                        for k in range(8):
                            f.pe.op(lambda: nc.tensor.matmul(p[:, 0:512], lhsT=aT[:, k, j * 128:(j + 1) * 128], rhs=wdn[:, k, hf * 512:(hf + 1) * 512],
                                                             start=(k == 0), stop=False), reads=[aTB, wdnB], writes=[pB])
                        f.pe.op(lambda: nc.tensor.matmul(p[:, 0:512], lhsT=onesb[0:1, :], rhs=bdn[0:1, hf * 512:(hf + 1) * 512], start=False, stop=True),
                                reads=[onesbB, bdnB], writes=[pB])
                        f.act.op(lambda: nc.scalar.copy(out=yo[:, j, hf * 512:(hf + 1) * 512], in_=p[:, 0:512]), reads=[pB], writes=[yoB])
                f.q_sp.dma(YG[bi * BLK:(bi + 1) * BLK, :].rearrange("(j p) c -> p j c", p=128), yo, reads=[yoB], writes=[YGb])
        f.barrier()
        with ExitStack() as es:
            g2 = []
            for r in range(3):
                g, gB = kb.sb(es, f"g2_{r}", [128, D], F32)
                load_bcast(kb, g, gB, ADA[l, r:r + 1, 5 * D:6 * D])
                g2.append((g, gB))
            lg, lgB = kb.sb(es, "ln2g", [128, D], F32)
            lb, lbB = kb.sb(es, "ln2b", [128, D], F32)
            load_bcast(kb, lg, lgB, I["ln2_g"][l:l + 1, :])
            load_bcast(kb, lb, lbB, I["ln2_b"][l:l + 1, :])
            yk_r = kb.ring(es, "eyk", [128, D], F32, 8)
            m_r = kb.ring(es, "em", [128, D], F32, 2)
            xs_r = kb.ring(es, "exs", [128, D], F32, 2)
            rl = (kb.ring(es, "et", [128, D], F32, 2), kb.ring(es, "est", [128, 16], F32, 2), kb.ring(es, "eo", [128, D], F32, 2))
            outB = dr["out"][1]
            idx = 0
            for it in range(NT128):
                t0 = it * 128
                b, o = divmod(t0, S)
                isctx = o >= L
                if isctx and last:
                    continue
                r = 2 if isctx else b
                yks = []
                for k in range(4):
                    yk, ykB = yk_r[(idx * 4 + k) % 8]
                    f.q_pool.dma(None, None, reads=[DSTIB, YGb], writes=[ykB],
                                 fn=lambda g: g.indirect_dma_start(out=yk, out_offset=None, in_=YG,
                                                                   in_offset=bass.IndirectOffsetOnAxis(ap=DSTI[:, it, k:k + 1], axis=0),
                                                                   bounds_check=kb.breg(NROWS - 1), oob_is_err=False))
                    yks.append((yk, ykB))
                m, mB = m_r[idx % 2]
                f.dve.op(lambda: nc.vector.tensor_scalar(out=m, in0=yks[0][0], scalar1=WK[:, it, 0:1], scalar2=None, op0=ALU.mult),
                         reads=[yks[0][1], WKB], writes=[mB])
                for k in range(1, 4):
                    f.dve.op(lambda: nc.vector.scalar_tensor_tensor(out=m, in0=yks[k][0], scalar=WK[:, it, k:k + 1], in1=m, op0=ALU.mult, op1=ALU.add),
                             reads=[yks[k][1], WKB, mB], writes=[mB])
                xs, xsB = xs_r[idx % 2]
                f.q_sp.dma(xs, XA[t0:t0 + 128, :], writes=[xsB])
                if last:
                    dst_ap, dB = out[b * L + o:b * L + o + 128, :], outB
                else:
                    dst_ap, dB = XB[t0:t0 + 128, :], dr["XB"][1]
                g, gB = g2[r]
                resid_ln(kb, rl, [(m[:, 0:512], mB), (m[:, 512:1024], mB)], None, False, xs, xsB, g, gB, lg, lgB, lb, lbB, dst_ap, dB, idx)
                idx += 1
    f.barrier()
import numpy as np


def make_tables():
    t = np.arange(L)
    row = (t // 64).astype(np.float32)
    col = (t % 64).astype(np.float32)

    def tab(rot_dim):
        nf = rot_dim // 4
        inv = (10000.0 ** (-np.arange(nf, dtype=np.float32) / nf)).astype(np.float32)
        ang = np.concatenate([row[:, None] * inv, col[:, None] * inv], axis=-1).astype(np.float32)
        return np.cos(ang).astype(np.float32), np.sin(ang).astype(np.float32)
    c64, s64 = tab(64)
    c32, s32 = tab(32)
    out = {}
    idx = (np.arange(128) % 64) % 32
    out["c64"] = np.ascontiguousarray(c64[:, idx].T)
    out["s64"] = np.ascontiguousarray(s64[:, idx].T)
    idx = np.arange(32) % 16
    out["c32"] = np.ascontiguousarray(c32[:, idx].T)
    out["s32"] = np.ascontiguousarray(s32[:, idx].T)
    c96 = np.ones((96, L), np.float32)
    s96 = np.zeros((96, L), np.float32)
    c96[64:] = out["c32"]
    s96[64:] = out["s32"]
    out["c96"] = c96
    out["s96"] = s96
    rc = np.zeros((2, 4, 16), np.float32)
    for a, Ls in enumerate((L, C)):
        for g in range(4):
            w = 2 << g
            for e in range(8):
                tau = e
                rc[a, g, e] = 1.0 / (min(tau + w // 2, Ls) - max(tau - w // 2, 0))
                tau = Ls - 8 + e
                rc[a, g, 8 + e] = 1.0 / (min(tau + w // 2, Ls) - max(tau - w // 2, 0))
    out["rc"] = np.ascontiguousarray(np.broadcast_to(rc.reshape(1, -1), (128, 128)))
    return out


PARAMS = [("w_ada", (4, 1024, 6144)), ("b_ada", (4, 6144)), ("w_in", (4, 1024, 2720)), ("conv_w", (4, 31, 512)),
          ("conv_b", (4, 512)), ("conv_ln_g", (4, 512)), ("conv_ln_b", (4, 512)), ("swa_sink", (4, 8)),
          ("pool_w", (4, 4, 128, 128)), ("pool_scale", (4, 512)), ("mla_q_g", (4, 256)), ("mla_w_uq", (4, 256, 768)),
          ("mla_kv_g", (4, 128)), ("mla_w_uk", (4, 128, 8, 64)), ("mla_w_uv", (4, 128, 8, 64)),
          ("w_branch", (4, 4, 512, 1024)), ("w_gate", (4, 4, 1024, 1024)), ("b_gate", (4, 4, 1024)),
          ("w_out", (4, 1024, 1024)), ("ln1_g", (4, 1024)), ("ln1_b", (4, 1024)), ("router_w", (4, 1024, 32)),
          ("router_b", (4, 32)), ("w_gu", (4, 32, 1024, 2048)), ("b_gu", (4, 32, 2048)), ("w_down", (4, 32, 1024, 1024)),
          ("b_down", (4, 32, 1024)), ("ln2_g", (4, 1024)), ("ln2_b", (4, 1024))]


def build(nlayers=DEPTH, upto=99, dbg=()):
    nc = bass.Bass("TRN2", target_bir_lowering=False)
    kb = KB(nc, dbg)
    I = {}
    I["x"] = kb.din("x", (NB * L, D))
    I["c"] = kb.din("c", (NB, D))
    I["ctx"] = kb.din("ctx", (NB * C, D))
    I["c_ctx"] = kb.din("c_ctx", (D,))
    for name, shp in PARAMS:
        if upto < 7 and name in ("w_gu", "w_down"):
            continue
        I[name] = kb.din(name, shp)
    tabs = {}
    for k, rows in (("c64", 128), ("s64", 128), ("c32", 32), ("s32", 32), ("c96", 96), ("s96", 96)):
        tabs[k] = kb.din("tab_" + k, (rows, L))
    kb.din("tab_rc", (128, 2 * 4 * 16))
    out = nc.dram_tensor("out", [NB * L, D], F32, kind="ExternalOutput").ap()
    kb.dram["out"] = (out, Buf("out"))
    ADA = kb.dscr("ADA", (DEPTH, 3, 6 * D), F32)
    kb.dscr("HT", (D, T), BF16)
    kb.dscr("VT", (512, T), BF16)
    kb.dscr("QT", (512, T), BF16)
    kb.dscr("KT", (128, T), BF16)
    kb.dscr("V", (T, 128), BF16)
    kb.dscr("PT", (512, T), BF16)
    kb.dscr("QM", (8, 96, T), BF16)
    kb.dscr("KN", (8, 64, T), BF16)
    kb.dscr("KR", (32, T), BF16)
    kb.dscr("VM", (T, 512), BF16)
    for nm in ("YA", "YB", "YC", "YD"):
        kb.dscr(nm, (512, T), BF16)
    kb.dscr("H2", (T, D), BF16)
    kb.dscr("WBG", (NE * 128, 8 * 2048), BF16)
    kb.dscr("WBD", (NE * 128, 8 * D), BF16)
    kb.dscr("XG", (NROWS, D), BF16)
    kb.dscr("YG", (NROWS, D), F32)
    XA = kb.dscr("XA", (T, D), F32)
    XB = kb.dscr("XB", (T, D), F32)
    with ExitStack() as ces:
        ces.enter_context(nc.allow_non_contiguous_dma(reason="small strided parameter loads"))
        ces.enter_context(nc.allow_low_precision(reason="bf16 matmul operands"))
        kb.c = build_consts(kb, ces)
        phase_ada(kb, kb.c, I["c"], I["c_ctx"], I["w_ada"], I["b_ada"], ADA)
        for l in range(nlayers):
            if l == 0:
                def xsrc(t0, n):
                    b, o = divmod(t0, S)
                    if o < L:
                        return I["x"][b * L + o:b * L + o + n, :]
                    return I["ctx"][b * C + (o - L):b * C + (o - L) + n, :]
            else:
                def xsrc(t0, n):
                    return XB[t0:t0 + n, :]
            if upto >= 1:
                phase_proj(kb, l, xsrc, I["w_in"], I["mla_q_g"], I["mla_w_uq"], I["mla_kv_g"], I["mla_w_uk"], I["mla_w_uv"], tabs, ADA)
            if upto >= 2:
                phase_conv(kb, l, I["conv_w"], I["conv_b"], I["conv_ln_g"], I["conv_ln_b"])
            if upto >= 3:
                phase_pool(kb, l, I["pool_w"], I["pool_scale"])
            if upto >= 4:
                phase_swa(kb, l, I["swa_sink"])
            if upto >= 5:
                phase_mla(kb, l, I)
            if upto >= 6:
                phase_merge(kb, l, xsrc, I, ADA, XA, skip_ctx=(l == DEPTH - 1))
            if upto >= 7:
                phase_moe(kb, l, I, ADA, XA, XB, out, l == nlayers - 1)
        kb.f.barrier()
    return nc


def core_inputs(inputs, core):
    m = {}
    b0 = core * NB
    m["x"] = np.ascontiguousarray(inputs["x"][b0:b0 + NB].reshape(NB * L, D))
    m["c"] = np.ascontiguousarray(inputs["c"][b0:b0 + NB])
    m["ctx"] = np.ascontiguousarray(inputs["ctx"][b0:b0 + NB].reshape(NB * C, D))
    m["c_ctx"] = np.ascontiguousarray(inputs["c_ctx"])
    for name, shp in PARAMS:
        m[name] = np.ascontiguousarray(inputs[name], dtype=np.float32)
    return m


_NC_CACHE = {}


def kernel(**inputs):
    n = 8
    if "nc" not in _NC_CACHE:
        _NC_CACHE["nc"] = build(nlayers=DEPTH, upto=99, dbg=())
    nc = _NC_CACHE["nc"]
    tabs = make_tables()
    shared = {}
    for name, shp in PARAMS:
        shared[name] = np.ascontiguousarray(np.asarray(inputs[name], dtype=np.float32))
    for k, v in tabs.items():
        shared["tab_" + k] = v
    x = np.asarray(inputs["x"], dtype=np.float32)
    c = np.asarray(inputs["c"], dtype=np.float32)
    ctx = np.asarray(inputs["ctx"], dtype=np.float32)
    c_ctx = np.ascontiguousarray(np.asarray(inputs["c_ctx"], dtype=np.float32))
    in_maps = []
    for core in range(n):
        b0 = core * NB
        m = dict(shared)
        m["x"] = np.ascontiguousarray(x[b0:b0 + NB].reshape(NB * L, D))
        m["c"] = np.ascontiguousarray(c[b0:b0 + NB])
        m["ctx"] = np.ascontiguousarray(ctx[b0:b0 + NB].reshape(NB * C, D))
        m["c_ctx"] = c_ctx
        in_maps.append(m)
    res = run_bass_kernel_spmd(nc, in_maps, core_ids=list(range(n)))
    outs = [np.asarray(r["out"], dtype=np.float32).reshape(NB, L, D) for r in res.results]
    return np.concatenate(outs, axis=0)
```

```python
import numpy as np
import concourse.bass as bass
import concourse.mybir as mybir
from concourse.bass_utils import run_bass_kernel_spmd

F32 = mybir.dt.float32
BF16 = mybir.dt.bfloat16
I32 = mybir.dt.int32
U32 = mybir.dt.uint32
AF = mybir.ActivationFunctionType
ALU = mybir.AluOpType
AX = mybir.AxisListType


class Buf:
    __slots__ = ("name", "w", "rs")

    def __init__(self, name=""):
        self.name = name
        self.w = None
        self.rs = []


class Eng:
    def __init__(self, fw, eng, name):
        self.fw = fw
        self.e = eng
        self.name = name
        self.sem = fw.nc.alloc_semaphore("s_" + name)
        self.cnt = 0
        self.inorder = (name == "pe")
        self.waited = {}

    def wait_tok(self, tok):
        if tok is None:
            return
        sem, val = tok
        if self.inorder and sem is self.sem:
            return
        k = id(sem)
        if self.waited.get(k, 0) < val:
            self.e.wait_ge(sem, val)
            self.waited[k] = val

    def deps(self, reads, writes):
        for b in reads:
            self.wait_tok(b.w)
        for b in writes:
            self.wait_tok(b.w)
            for t in b.rs:
                self.wait_tok(t)

    def mark(self, tok, reads, writes):
        for b in reads:
            b.rs.append(tok)
        for b in writes:
            b.w = tok
            b.rs = []

    def op(self, ins, reads=(), writes=()):
        self.deps(reads, writes)
        i = ins()
        self.cnt += 1
        i.then_inc(self.sem, 1)
        tok = (self.sem, self.cnt)
        self.mark(tok, reads, writes)
        return tok


class DmaQ:
    def __init__(self, fw, engw, npool, name):
        self.fw = fw
        self.engw = engw
        self.pool = [[fw.nc.alloc_semaphore(f"d_{name}_{i}"), 0] for i in range(npool)]
        self.nxt = 0

    def dma(self, out, in_, reads=(), writes=(), fn=None, **kw):
        ew = self.engw
        ew.deps(reads, writes)
        slot = self.pool[self.nxt]
        self.nxt = (self.nxt + 1) % len(self.pool)
        sem, val = slot
        if val:
            ew.wait_tok((sem, val))
        if fn is None:
            i = ew.e.dma_start(out=out, in_=in_, **kw)
        else:
            i = fn(ew.e)
        slot[1] = val + 16
        i.then_inc(sem, 16)
        tok = (sem, val + 16)
        ew.mark(tok, reads, writes)
        return tok


class FW:
    def __init__(self, nc):
        self.nc = nc
        self.pe = Eng(self, nc.tensor, "pe")
        self.act = Eng(self, nc.scalar, "act")
        self.dve = Eng(self, nc.vector, "dve")
        self.pool = Eng(self, nc.gpsimd, "pool")
        self.sp = Eng(self, nc.sync, "sp")
        self.engs = [self.pe, self.act, self.dve, self.pool, self.sp]
        self.q_sp = DmaQ(self, self.sp, 24, "sp")
        self.q_pool = DmaQ(self, self.pool, 16, "pool")
        self.q_act = DmaQ(self, self.act, 8, "act")
        self.qs = [self.q_sp, self.q_pool, self.q_act]

    def barrier(self, engs=None):
        toks = []
        for e in self.engs:
            if e.cnt:
                toks.append((e.sem, e.cnt))
        for q in self.qs:
            for sem, val in q.pool:
                if val:
                    toks.append((sem, val))
        for e in (engs or self.engs):
            for t in toks:
                e.wait_tok(t)
from contextlib import ExitStack

D = 1024
NB = 2
L = 4096
C = 256
S = L + C
T = NB * S
DEPTH = 4
INW = 2720
EXTW = INW + 512 + 128 + 32
Q0, K0, V0, P0_, CQ0, CKV0, KR0 = 1024, 1536, 1664, 1792, 2304, 2560, 2688
QR0, KRT0, KRR0 = 2720, 3232, 3360
EPS = 1e-6
ALPHA = 8 ** 0.25
NE = 32
BLK = 256
NBLK = -(-(T * 4 + NE * (BLK - 1)) // BLK)
NROWS = NBLK * BLK


def tiles_all():
    out = []
    for b in range(NB):
        for i in range(L // 512):
            out.append((b * S + i * 512, 512, b, False, i * 512))
        out.append((b * S + L, 256, b, True, 0))
    return out


class KB:
    def __init__(self, nc, dbg=()):
        self.nc = nc
        self.f = FW(nc)
        self.dbg = set(dbg)
        self.dram = {}
        self.psf = []
        self.psb = []
        for i in range(6):
            self.psf.append((nc.alloc_psum_tensor(f"psf{i}", [128, 512], F32).ap(), Buf(f"psf{i}")))
        for i in range(2):
            self.psb.append((nc.alloc_psum_tensor(f"psb{i}", [128, 1024], BF16).ap(), Buf(f"psb{i}")))
        self.psf_i = 0
        self.pinned = set()
        self.bregs = {}
        self.psb_i = 0

    def ps(self):
        while True:
            i = self.psf_i % len(self.psf)
            self.psf_i += 1
            if i not in self.pinned:
                return self.psf[i]

    def breg(self, v):
        if v not in self.bregs:
            self.bregs[v] = self.nc.gpsimd.to_reg(v)
        return self.bregs[v]

    def pin(self):
        while True:
            i = self.psf_i % len(self.psf)
            self.psf_i += 1
            if i not in self.pinned:
                self.pinned.add(i)
                return self.psf[i] + (i,)

    def unpin(self, i):
        self.pinned.discard(i)

    def pb(self):
        r = self.psb[self.psb_i % len(self.psb)]
        self.psb_i += 1
        return r

    def din(self, name, shape, dt=F32):
        t = self.nc.dram_tensor(name, list(shape), dt, kind="ExternalInput").ap()
        self.dram[name] = (t, Buf(name))
        return t

    def dscr(self, name, shape, dt):
        kind = "ExternalOutput" if name in self.dbg else "Internal"
        t = self.nc.dram_tensor(name, list(shape), dt, kind=kind).ap()
        self.dram[name] = (t, Buf(name))
        return t

    def sb(self, es, name, shape, dt):
        self.uid = getattr(self, "uid", 0) + 1
        h = es.enter_context(self.nc.sbuf_tensor(f"{name}_u{self.uid}", list(shape), dt))
        return h.ap(), Buf(name)

    def ring(self, es, name, shape, dt, n=2):
        return [self.sb(es, f"{name}{i}", shape, dt) for i in range(n)]


def build_consts(kb, es):
    nc, f = kb.nc, kb.f
    c = {}
    ident, identB = kb.sb(es, "ident", [128, 128], BF16)
    f.pool.op(lambda: nc.gpsimd.memset(ident, 1.0), writes=[identB])
    f.pool.op(lambda: nc.gpsimd.affine_select(out=ident, in_=ident, pattern=[[-1, 128]], compare_op=ALU.is_equal,
                                              fill=0.0, base=0, channel_multiplier=1), reads=[identB], writes=[identB])
    c["ident"] = (ident, identB)
    onesb, onesbB = kb.sb(es, "onesb", [128, 128], BF16)
    f.pool.op(lambda: nc.gpsimd.memset(onesb, 1.0), writes=[onesbB])
    c["onesb"] = (onesb, onesbB)
    onesf, onesfB = kb.sb(es, "onesf", [128, 128], F32)
    f.pool.op(lambda: nc.gpsimd.memset(onesf, 1.0), writes=[onesfB])
    c["onesf"] = (onesf, onesfB)
    nh, nhB = kb.sb(es, "neghalf", [128, 512], F32)
    f.pool.op(lambda: nc.gpsimd.memset(nh, -0.5), writes=[nhB])
    c["neghalf"] = (nh, nhB)
    orow, orowB = kb.sb(es, "onesrow", [1, 512], BF16)
    f.pool.op(lambda: nc.gpsimd.memset(orow, 1.0), writes=[orowB])
    c["onesrow"] = (orow, orowB)
    epsc, epscB = kb.sb(es, "epsc", [128, 1], F32)
    f.pool.op(lambda: nc.gpsimd.memset(epsc, EPS), writes=[epscB])
    c["epsc"] = (epsc, epscB)
    return c


def phase_ada(kb, cs, c_in, cctx_in, w_ada, b_ada, ADA):
    nc, f = kb.nc, kb.f
    ADAb = kb.dram["ADA"][1]
    with ExitStack() as es:
        crow, crowB = kb.sb(es, "crow", [3, D], F32)
        f.q_sp.dma(crow[0:2, :], c_in, writes=[crowB])
        f.q_sp.dma(crow[2:3, :], cctx_in.rearrange("(o d) -> o d", o=1), writes=[crowB])
        sg, sgB = kb.sb(es, "csg", [3, D], F32)
        f.act.op(lambda: nc.scalar.activation(out=sg, in_=crow, func=AF.Sigmoid), reads=[crowB], writes=[sgB])
        f.dve.op(lambda: nc.vector.tensor_tensor(out=sg, in0=sg, in1=crow, op=ALU.mult), reads=[sgB, crowB], writes=[sgB])
        id3, id3B = kb.sb(es, "id3", [3, 3], F32)
        f.pool.op(lambda: nc.gpsimd.memset(id3, 1.0), writes=[id3B])
        f.pool.op(lambda: nc.gpsimd.affine_select(out=id3, in_=id3, pattern=[[-1, 3]], compare_op=ALU.is_equal,
                                                  fill=0.0, base=0, channel_multiplier=1), reads=[id3B], writes=[id3B])
        sT, sTB = kb.sb(es, "sT", [128, 8, 3], F32)
        pt, ptB = kb.ps()
        for k in range(8):
            f.pe.op(lambda: nc.tensor.matmul(pt[:, k * 3:(k + 1) * 3], lhsT=sg[:, k * 128:(k + 1) * 128], rhs=id3,
                                             start=True, stop=True), reads=[sgB, id3B], writes=[ptB])
        f.dve.op(lambda: nc.vector.tensor_copy(out=sT.rearrange("p k r -> p (k r)"), in_=pt[:, 0:24]), reads=[ptB], writes=[sTB])
        ones3, ones3B = kb.sb(es, "ones3", [1, 3], F32)
        f.pool.op(lambda: nc.gpsimd.memset(ones3, 1.0), writes=[ones3B])
        wr = kb.ring(es, "wada", [128, 8, 512], F32, 2)
        br = kb.ring(es, "bada", [1, 512], F32, 2)
        orr = kb.ring(es, "oada", [3, 512], F32, 2)
        it = 0
        for l in range(DEPTH):
            wv = w_ada[l].rearrange("(k p) c -> p k c", p=128)
            for cc in range(12):
                w, wB = wr[it % 2]
                bb, bbB = br[it % 2]
                o, oB = orr[it % 2]
                f.q_sp.dma(w, wv[:, :, cc * 512:(cc + 1) * 512], writes=[wB])
                f.q_sp.dma(bb, b_ada[l:l + 1, cc * 512:(cc + 1) * 512], writes=[bbB])
                p, pB = kb.ps()
                for k in range(8):
                    f.pe.op(lambda: nc.tensor.matmul(p[0:3, :], lhsT=sT[:, k, :], rhs=w[:, k, :], start=(k == 0), stop=False),
                            reads=[sTB, wB], writes=[pB])
                f.pe.op(lambda: nc.tensor.matmul(p[0:3, :], lhsT=ones3, rhs=bb, start=False, stop=True),
                        reads=[ones3B, bbB], writes=[pB])
                f.act.op(lambda: nc.scalar.copy(out=o, in_=p[0:3, :]), reads=[pB], writes=[oB])
                f.q_sp.dma(ADA[l, :, cc * 512:(cc + 1) * 512], o, reads=[oB], writes=[ADAb])
                it += 1
    f.barrier()


def load_bcast(kb, dst, dstB, src_row_ap, n=128):
    kb.f.q_sp.dma(dst, src_row_ap.partition_broadcast(n), writes=[dstB])


def ln_tile(kb, xs, xsB, st, stB, eps_sqrt=True):
    nc, f = kb.nc, kb.f
    f.dve.op(lambda: nc.vector.bn_stats(out=st[:, 0:6], in_=xs[:, 0:512]), reads=[xsB], writes=[stB])
    f.dve.op(lambda: nc.vector.bn_stats(out=st[:, 6:12], in_=xs[:, 512:1024]), reads=[xsB], writes=[stB])
    f.dve.op(lambda: nc.vector.bn_aggr(out=st[:, 12:14], in_=st[:, 0:12]), reads=[stB], writes=[stB])
    f.dve.op(lambda: nc.vector.tensor_scalar(out=st[:, 14:15], in0=st[:, 13:14], scalar1=EPS, scalar2=None, op0=ALU.add),
             reads=[stB], writes=[stB])
    nh, nhB = kb.c["neghalf"]
    f.pool.op(lambda: nc.gpsimd.tensor_tensor(out=st[:, 15:16], in0=st[:, 14:15], in1=nh[:, 0:1], op=ALU.pow),
              reads=[stB, nhB], writes=[stB])
    return st[:, 12:13], st[:, 15:16]


def phase_proj(kb, l, xsrc, w_in, mla_q_g, mla_w_uq, mla_kv_g, mla_w_uk, mla_w_uv, tabs, ADA):
    nc, f = kb.nc, kb.f
    dr = kb.dram
    with ExitStack() as es:
        wext, wextB = kb.sb(es, "wext", [128, 8, EXTW], BF16)
        wv = w_in[l].rearrange("(k p) c -> p k c", p=128)
        f.q_pool.dma(wext[:, :, 0:2048], wv[:, :, 0:2048], writes=[wextB])
        f.q_pool.dma(wext[:, :, 2048:INW], wv[:, :, 2048:INW], writes=[wextB])

        def rot(dst0, src0, nheads, half, eng_neg, eng_cp, w=wext, wB=wextB, nk=8):
            for k in range(nk):
                s = w[:, k, src0:src0 + nheads * 2 * half].rearrange("p (h two d) -> p h two d", two=2, d=half)
                d = w[:, k, dst0:dst0 + nheads * 2 * half].rearrange("p (h two d) -> p h two d", two=2, d=half)
                f.act.op(lambda: nc.scalar.mul(out=d[:, :, 0, :], in_=s[:, :, 1, :], mul=-1.0), reads=[wB], writes=[wB])
                f.dve.op(lambda: nc.vector.tensor_copy(out=d[:, :, 1, :], in_=s[:, :, 0, :]), reads=[wB], writes=[wB])
        rot(QR0, Q0, 8, 32, None, None)
        rot(KRT0, K0, 2, 32, None, None)
        rot(KRR0, KR0, 1, 16, None, None)
        wuq, wuqB = kb.sb(es, "wuq", [128, 2, 768], BF16)
        f.q_pool.dma(wuq, mla_w_uq[l].rearrange("(k p) c -> p k c", p=128), writes=[wuqB])
        wuqr, wuqrB = kb.sb(es, "wuqr", [128, 2, 768], BF16)
        f.pool.op(lambda: nc.gpsimd.memset(wuqr, 0.0), writes=[wuqrB])
        for k in range(2):
            s = wuq[:, k, :].rearrange("p (h c) -> p h c", c=96)[:, :, 64:96].rearrange("p h (two d) -> p h two d", two=2)
            d = wuqr[:, k, :].rearrange("p (h c) -> p h c", c=96)[:, :, 64:96].rearrange("p h (two d) -> p h two d", two=2)
            f.act.op(lambda: nc.scalar.mul(out=d[:, :, 0, :], in_=s[:, :, 1, :], mul=-1.0), reads=[wuqB, wuqrB], writes=[wuqrB])
            f.dve.op(lambda: nc.vector.tensor_copy(out=d[:, :, 1, :], in_=s[:, :, 0, :]), reads=[wuqB, wuqrB], writes=[wuqrB])
        wuk, wukB = kb.sb(es, "wuk", [128, 512], BF16)
        f.q_pool.dma(wuk, mla_w_uk[l].rearrange("r h n -> r (h n)"), writes=[wukB])
        wuv, wuvB = kb.sb(es, "wuv", [128, 512], BF16)
        f.q_pool.dma(wuv, mla_w_uv[l].rearrange("r h n -> r (h n)"), writes=[wuvB])
        gq, gqB = kb.sb(es, "gq", [128, 2], F32)
        f.q_sp.dma(gq, mla_q_g[l].rearrange("(k p) -> p k", p=128), writes=[gqB])
        gkv, gkvB = kb.sb(es, "gkv", [128, 1], F32)
        f.q_sp.dma(gkv, mla_kv_g[l].rearrange("(p o) -> p o", o=1), writes=[gkvB])
        mods = []
        for r in range(3):
            sc, scB = kb.sb(es, f"sc1_{r}", [128, D], F32)
            sh, shB = kb.sb(es, f"sh1_{r}", [128, D], F32)
            load_bcast(kb, sh, shB, ADA[l, r:r + 1, 0:D])
            load_bcast(kb, sc, scB, ADA[l, r:r + 1, D:2 * D])
            f.pool.op(lambda: nc.gpsimd.tensor_scalar(out=sc, in0=sc, scalar1=1.0, scalar2=None, op0=ALU.add),
                      reads=[scB], writes=[scB])
            mods.append((sc, scB, sh, shB))
        ident, identB = kb.c["ident"]
        onesb, onesbB = kb.c["onesb"]
        nh, nhB = kb.c["neghalf"]
        xs_r = kb.ring(es, "xs", [128, D], F32, 2)
        st_r = kb.ring(es, "st", [128, 16], F32, 2)
        xn_r = kb.ring(es, "xn", [128, D], F32, 2)
        hb_r = kb.ring(es, "hb", [128, D], BF16, 2)
        hT_r = kb.ring(es, "hT", [128, 8, 512], BF16, 2)
        tab_r = {k: kb.ring(es, "tab_" + k, [tabs[k].shape[0], 512], F32, 1) for k in tabs}
        sg_r = kb.ring(es, "sgt", [128, 512], F32, 2)
        vT_r = kb.ring(es, "vTt", [128, 4, 512], BF16, 1)
        qT_r = kb.ring(es, "qTt", [128, 4, 512], BF16, 1)
        kT_r = kb.ring(es, "kTt", [128, 512], BF16, 2)
        pT_r = kb.ring(es, "pTt", [128, 4, 512], BF16, 1)
        t1_r = kb.ring(es, "t1", [128, 512], F32, 3)
        t2_r = kb.ring(es, "t2", [128, 512], F32, 3)
        vtm_r = kb.ring(es, "vtm", [128, 4, 128], BF16, 2)
        sq_r = kb.ring(es, "sq", [128, 3, 512], BF16, 1)
        rq_r = kb.ring(es, "rq", [128, 2, 512], F32, 1)
        cqn_r = kb.ring(es, "cqn", [128, 2, 512], BF16, 2)
        ckvn_r = kb.ring(es, "ckvn", [128, 512], BF16, 2)
        krT_r = kb.ring(es, "krT", [32, 512], BF16, 2)
        qm_r = kb.ring(es, "qm", [96, 8, 512], BF16, 1)
        kn_r = kb.ring(es, "kn", [64, 8, 512], BF16, 1)
        vm_r = kb.ring(es, "vmt", [128, 4, 512], BF16, 1)
        HT, HTb = dr["HT"]
        VT, VTb = dr["VT"]
        QT, QTb = dr["QT"]
        KT, KTb = dr["KT"]
        Vd, Vdb = dr["V"]
        PT, PTb = dr["PT"]
        QM, QMb = dr["QM"]
        KN, KNb = dr["KN"]
        KR, KRb = dr["KR"]
        VM, VMb = dr["VM"]

        def proj(c0, m, hT, hTB, n, w=wext, wB=wextB, nk=8):
            p, pB = kb.ps()
            for k in range(nk):
                f.pe.op(lambda: nc.tensor.matmul(p[0:m, 0:n], lhsT=w[:, k, c0:c0 + m], rhs=hT[:, k, 0:n],
                                                 start=(k == 0), stop=(k == nk - 1)), reads=[wB, hTB], writes=[pB])
            return p, pB

        for ti, (t0, n, b, isctx, pos) in enumerate(tiles_all()):
            r = 2 if isctx else b
            sc, scB, sh, shB = mods[r]
            hT, hTB = hT_r[ti % 2]
            nsub = n // 128
            for j in range(nsub):
                it = ti * 4 + j
                xs, xsB = xs_r[it % len(xs_r)]
                st, stB = st_r[it % len(st_r)]
                xn, xnB = xn_r[it % 2]
                hb, hbB = hb_r[it % 2]
                f.q_sp.dma(xs, xsrc(t0 + j * 128, 128), writes=[xsB])
                mean, rstd = ln_tile(kb, xs, xsB, st, stB)
                f.dve.op(lambda: nc.vector.tensor_scalar(out=xn, in0=xs, scalar1=mean, scalar2=rstd, op0=ALU.subtract, op1=ALU.mult),
                         reads=[xsB, stB], writes=[xnB])
                f.pool.op(lambda: nc.gpsimd.tensor_tensor(out=xn, in0=xn, in1=sc, op=ALU.mult), reads=[xnB, scB], writes=[xnB])
                f.dve.op(lambda: nc.vector.tensor_tensor(out=hb, in0=xn, in1=sh, op=ALU.add), reads=[xnB, shB], writes=[hbB])
                pt, ptB = kb.pb()
                for k in range(8):
                    f.pe.op(lambda: nc.tensor.transpose(pt[:, k * 128:(k + 1) * 128], hb[:, k * 128:(k + 1) * 128], ident),
                            reads=[hbB, identB], writes=[ptB])
                f.act.op(lambda: nc.scalar.copy(out=hT[:, :, j * 128:(j + 1) * 128], in_=pt.rearrange("p (k t) -> p k t", k=8)),
                         reads=[ptB], writes=[hTB])
            f.q_sp.dma(HT.rearrange("(k p) t -> p k t", p=128)[:, :, t0:t0 + n], hT[:, :, 0:n], reads=[hTB], writes=[HTb])
            tb = {}
            if not isctx:
                for k in tabs:
                    ta, taB = tab_r[k][0]
                    f.q_sp.dma(ta[:, 0:n], tabs[k][:, pos:pos + n], writes=[taB])
                    tb[k] = (ta, taB)
            vT, vTB = vT_r[ti % len(vT_r)]
            for cc in range(4):
                pg, pgB = proj(512 + cc * 128, 128, hT, hTB, n)
                sg, sgB = sg_r[cc % 2]
                f.act.op(lambda: nc.scalar.activation(out=sg[:, 0:n], in_=pg[:, 0:n], func=AF.Sigmoid), reads=[pgB], writes=[sgB])
                pv, pvB = proj(cc * 128, 128, hT, hTB, n)
                f.dve.op(lambda: nc.vector.tensor_tensor(out=vT[:, cc, 0:n], in0=pv[:, 0:n], in1=sg[:, 0:n], op=ALU.mult),
                         reads=[pvB, sgB], writes=[vTB])
            f.q_sp.dma(VT.rearrange("(k p) t -> p k t", p=128)[:, :, t0:t0 + n], vT[:, :, 0:n], reads=[vTB], writes=[VTb])

            def roped(c0, cr0, m, dst, dstB, ck, sk, ii):
                p1, p1B = proj(c0, m, hT, hTB, n)
                if isctx:
                    f.act.op(lambda: nc.scalar.copy(out=dst, in_=p1[0:m, 0:n]), reads=[p1B], writes=[dstB])
                    return
                p2, p2B = proj(cr0, m, hT, hTB, n)
                t1, t1B = t1_r[ii % 3]
                t2, t2B = t2_r[ii % 3]
                co, coB = tb[ck]
                si, siB = tb[sk]
                f.dve.op(lambda: nc.vector.tensor_tensor(out=t1[0:m, 0:n], in0=p1[0:m, 0:n], in1=co[0:m, 0:n], op=ALU.mult),
                         reads=[p1B, coB], writes=[t1B])
                f.dve.op(lambda: nc.vector.tensor_tensor(out=t2[0:m, 0:n], in0=p2[0:m, 0:n], in1=si[0:m, 0:n], op=ALU.mult),
                         reads=[p2B, siB], writes=[t2B])
                f.pool.op(lambda: nc.gpsimd.tensor_tensor(out=dst, in0=t1[0:m, 0:n], in1=t2[0:m, 0:n], op=ALU.add),
                          reads=[t1B, t2B], writes=[dstB])
            qT, qTB = qT_r[ti % len(qT_r)]
            for cc in range(4):
                roped(Q0 + cc * 128, QR0 + cc * 128, 128, qT[:, cc, 0:n], qTB, "c64", "s64", cc)
            f.q_sp.dma(QT.rearrange("(k p) t -> p k t", p=128)[:, :, t0:t0 + n], qT[:, :, 0:n], reads=[qTB], writes=[QTb])
            kT, kTB = kT_r[ti % 2]
            roped(K0, KRT0, 128, kT[:, 0:n], kTB, "c64", "s64", 4)
            f.q_sp.dma(KT[:, t0:t0 + n], kT[:, 0:n], reads=[kTB], writes=[KTb])
            krT, krTB = krT_r[ti % 2]
            roped(KR0, KRR0, 32, krT[:, 0:n], krTB, "c32", "s32", 5)
            f.q_sp.dma(KR[:, t0:t0 + n], krT[:, 0:n], reads=[krTB], writes=[KRb])
            vtm, vtmB = vtm_r[ti % 2]
            for j in range(nsub):
                p, pB = kb.ps()
                for k in range(8):
                    f.pe.op(lambda: nc.tensor.matmul(p[:, 0:128], lhsT=hT[:, k, j * 128:(j + 1) * 128], rhs=wext[:, k, V0:V0 + 128],
                                                     start=(k == 0), stop=(k == 7)), reads=[hTB, wextB], writes=[pB])
                f.act.op(lambda: nc.scalar.copy(out=vtm[:, j, :], in_=p[:, 0:128]), reads=[pB], writes=[vtmB])
            f.q_sp.dma(Vd[t0:t0 + n, :].rearrange("(j p) c -> p j c", p=128), vtm[:, 0:nsub, :], reads=[vtmB], writes=[Vdb])
            pT, pTB = pT_r[ti % len(pT_r)]
            for cc in range(4):
                p, pB = proj(P0_ + cc * 128, 128, hT, hTB, n)
                f.act.op(lambda: nc.scalar.copy(out=pT[:, cc, 0:n], in_=p[:, 0:n]), reads=[pB], writes=[pTB])
            f.q_sp.dma(PT.rearrange("(k p) t -> p k t", p=128)[:, :, t0:t0 + n], pT[:, :, 0:n], reads=[pTB], writes=[PTb])
            pcs = [proj(CQ0, 128, hT, hTB, n), proj(CQ0 + 128, 128, hT, hTB, n), proj(CKV0, 128, hT, hTB, n)]
            sq, sqB = sq_r[ti % len(sq_r)]
            for i3 in range(3):
                f.act.op(lambda: nc.scalar.activation(out=sq[:, i3, 0:n], in_=pcs[i3][0][:, 0:n], func=AF.Square),
                         reads=[pcs[i3][1]], writes=[sqB])
            rq, rqB = rq_r[ti % len(rq_r)]
            for i2, (ks, div) in enumerate((((0, 1), 256.0), ((2,), 128.0))):
                pss, pssB = kb.ps()
                for ii, k in enumerate(ks):
                    f.pe.op(lambda: nc.tensor.matmul(pss[:, 0:n], lhsT=onesb, rhs=sq[:, k, 0:n], start=(ii == 0), stop=(ii == len(ks) - 1)),
                            reads=[onesbB, sqB], writes=[pssB])
                f.act.op(lambda: nc.scalar.activation(out=rq[:, i2, 0:n], in_=pss[:, 0:n], func=AF.Sqrt, scale=1.0 / div,
                                                      bias=kb.c["epsc"][0][:, 0:1]), reads=[pssB, kb.c["epsc"][1]], writes=[rqB])
                f.dve.op(lambda: nc.vector.reciprocal(out=rq[:, i2, 0:n], in_=rq[:, i2, 0:n]), reads=[rqB], writes=[rqB])
            cqn, cqnB = cqn_r[ti % 2]
            for k in range(2):
                f.dve.op(lambda: nc.vector.scalar_tensor_tensor(out=cqn[:, k, 0:n], in0=pcs[k][0][:, 0:n], scalar=gq[:, k:k + 1],
                                                                in1=rq[:, 0, 0:n], op0=ALU.mult, op1=ALU.mult),
                         reads=[pcs[k][1], gqB, rqB], writes=[cqnB])
            ckvn, ckvnB = ckvn_r[ti % 2]
            f.dve.op(lambda: nc.vector.scalar_tensor_tensor(out=ckvn[:, 0:n], in0=pcs[2][0][:, 0:n], scalar=gkv[:, 0:1],
                                                            in1=rq[:, 1, 0:n], op0=ALU.mult, op1=ALU.mult),
                     reads=[pcs[2][1], gkvB, rqB], writes=[ckvnB])
            qm, qmB = qm_r[ti % len(qm_r)]
            for h in range(8):
                p1, p1B = proj(h * 96, 96, cqn, cqnB, n, w=wuq, wB=wuqB, nk=2)
                if isctx:
                    f.act.op(lambda: nc.scalar.copy(out=qm[:, h, 0:n], in_=p1[0:96, 0:n]), reads=[p1B], writes=[qmB])
                else:
                    p2, p2B = proj(h * 96, 96, cqn, cqnB, n, w=wuqr, wB=wuqrB, nk=2)
                    t1, t1B = t1_r[h % 3]
                    t2, t2B = t2_r[h % 3]
                    co, coB = tb["c96"]
                    si, siB = tb["s96"]
                    f.dve.op(lambda: nc.vector.tensor_tensor(out=t1[0:96, 0:n], in0=p1[0:96, 0:n], in1=co[0:96, 0:n], op=ALU.mult),
                             reads=[p1B, coB], writes=[t1B])
                    f.dve.op(lambda: nc.vector.tensor_tensor(out=t2[0:96, 0:n], in0=p2[0:96, 0:n], in1=si[0:96, 0:n], op=ALU.mult),
                             reads=[p2B, siB], writes=[t2B])
                    f.pool.op(lambda: nc.gpsimd.tensor_tensor(out=qm[:, h, 0:n], in0=t1[0:96, 0:n], in1=t2[0:96, 0:n], op=ALU.add),
                              reads=[t1B, t2B], writes=[qmB])
            f.q_sp.dma(QM.rearrange("h r t -> r h t")[:, :, t0:t0 + n], qm[:, :, 0:n], reads=[qmB], writes=[QMb])
            kn, knB = kn_r[ti % len(kn_r)]
            for h in range(8):
                p, pB = kb.ps()
                f.pe.op(lambda: nc.tensor.matmul(p[0:64, 0:n], lhsT=wuk[:, h * 64:(h + 1) * 64], rhs=ckvn[:, 0:n], start=True, stop=True),
                        reads=[wukB, ckvnB], writes=[pB])
                f.act.op(lambda: nc.scalar.copy(out=kn[:, h, 0:n], in_=p[0:64, 0:n]), reads=[pB], writes=[knB])
            f.q_sp.dma(KN.rearrange("h r t -> r h t")[:, :, t0:t0 + n], kn[:, :, 0:n], reads=[knB], writes=[KNb])
            vm, vmB = vm_r[ti % len(vm_r)]
            for j in range(nsub):
                p, pB = kb.ps()
                f.pe.op(lambda: nc.tensor.matmul(p[:, 0:512], lhsT=ckvn[:, j * 128:(j + 1) * 128], rhs=wuv, start=True, stop=True),
                        reads=[ckvnB, wuvB], writes=[pB])
                f.act.op(lambda: nc.scalar.copy(out=vm[:, j, :], in_=p[:, 0:512]), reads=[pB], writes=[vmB])
            f.q_sp.dma(VM[t0:t0 + n, :].rearrange("(j p) c -> p j c", p=128), vm[:, 0:nsub, :], reads=[vmB], writes=[VMb])
    f.barrier()


def segs():
    out = []
    for b in range(NB):
        out.append((b * S, L, b, False))
        out.append((b * S + L, C, b, True))
    return out


def seg_tiles():
    out = []
    for (s0, sl, b, isctx) in segs():
        n = 512 if not isctx else 256
        for i in range(sl // n):
            out.append((s0 + i * n, n, s0, s0 + sl, b, isctx))
    return out


def load_halo(kb, dst, dstB, src3, t0, n, halo, lo_lim, hi_lim):
    nc, f = kb.nc, kb.f
    lo = max(t0 - halo, lo_lim)
    hi = min(t0 + n + halo, hi_lim)
    off = lo - (t0 - halo)
    if off > 0:
        f.pool.op(lambda: nc.gpsimd.memset(dst[:, :, 0:off], 0.0), writes=[dstB])
    if off + (hi - lo) < n + 2 * halo:
        f.pool.op(lambda: nc.gpsimd.memset(dst[:, :, off + (hi - lo):n + 2 * halo], 0.0), writes=[dstB])
    f.q_sp.dma(dst[:, :, off:off + (hi - lo)], src3[:, :, lo:hi], writes=[dstB])


def phase_conv(kb, l, conv_w, conv_b, ln_g, ln_b):
    nc, f = kb.nc, kb.f
    VT, VTb = kb.dram["VT"]
    YA, YAb = kb.dram["YA"]
    onesf, onesfB = kb.c["onesf"]
    nh, nhB = kb.c["neghalf"]
    with ExitStack() as es:
        cw, cwB = kb.sb(es, "cw", [128, 4, 31], F32)
        for k in range(4):
            f.q_sp.dma(cw[:, k, :], conv_w[l][:, k * 128:(k + 1) * 128].rearrange("j p -> p j"), writes=[cwB])
        prm, prmB = kb.sb(es, "cprm", [128, 3, 4], F32)
        for i, a in enumerate((conv_b, ln_g, ln_b)):
            f.q_sp.dma(prm[:, i, :], a[l].rearrange("(k p) -> p k", p=128), writes=[prmB])
        ve_r = kb.ring(es, "vext", [128, 4, 512 + 30], BF16, 2)
        y_r = kb.ring(es, "cy", [128, 4, 512], F32, 2)
        sq_r = kb.ring(es, "csq", [128, 4, 512], F32, 1)
        mean_r = kb.ring(es, "cmean", [128, 512], F32, 2)
        rstd_r = kb.ring(es, "crstd", [128, 512], F32, 2)
        z_r = kb.ring(es, "cz", [128, 512], F32, 2)
        ya_r = kb.ring(es, "cya", [128, 4, 512], BF16, 2)
        VT3 = VT.rearrange("(k p) t -> p k t", p=128)
        YA3 = YA.rearrange("(k p) t -> p k t", p=128)
        for ti, (t0, n, slo, shi, b, isctx) in enumerate(seg_tiles()):
            ve, veB = ve_r[ti % 2]
            load_halo(kb, ve, veB, VT3, t0, n, 15, slo, shi)
            y, yB = y_r[ti % 2]
            for k in range(4):
                f.dve.op(lambda: nc.vector.tensor_scalar(out=y[:, k, 0:n], in0=ve[:, k, 0:n], scalar1=cw[:, k, 0:1], scalar2=prm[:, 0, k:k + 1],
                                                         op0=ALU.mult, op1=ALU.add), reads=[veB, cwB, prmB], writes=[yB])
                for j in range(1, 31):
                    f.dve.op(lambda: nc.vector.scalar_tensor_tensor(out=y[:, k, 0:n], in0=ve[:, k, j:j + n], scalar=cw[:, k, j:j + 1],
                                                                    in1=y[:, k, 0:n], op0=ALU.mult, op1=ALU.add),
                             reads=[veB, cwB, yB], writes=[yB])
            sq, sqB = sq_r[0]
            f.act.op(lambda: nc.scalar.activation(out=sq[:, :, 0:n], in_=y[:, :, 0:n], func=AF.Square), reads=[yB], writes=[sqB])
            p1, p1B = kb.ps()
            p2, p2B = kb.ps()
            for k in range(4):
                f.pe.op(lambda: nc.tensor.matmul(p1[:, 0:n], lhsT=onesf, rhs=y[:, k, 0:n], start=(k == 0), stop=(k == 3)),
                        reads=[onesfB, yB], writes=[p1B])
            for k in range(4):
                f.pe.op(lambda: nc.tensor.matmul(p2[:, 0:n], lhsT=onesf, rhs=sq[:, k, 0:n], start=(k == 0), stop=(k == 3)),
                        reads=[onesfB, sqB], writes=[p2B])
            mean, meanB = mean_r[ti % 2]
            rstd, rstdB = rstd_r[ti % 2]
            f.act.op(lambda: nc.scalar.mul(out=mean[:, 0:n], in_=p1[:, 0:n], mul=1.0 / 512), reads=[p1B], writes=[meanB])
            f.dve.op(lambda: nc.vector.tensor_tensor(out=rstd[:, 0:n], in0=mean[:, 0:n], in1=mean[:, 0:n], op=ALU.mult),
                     reads=[meanB], writes=[rstdB])
            f.dve.op(lambda: nc.vector.scalar_tensor_tensor(out=rstd[:, 0:n], in0=p2[:, 0:n], scalar=1.0 / 512, in1=rstd[:, 0:n],
                                                            op0=ALU.mult, op1=ALU.subtract), reads=[p2B, rstdB], writes=[rstdB])
            f.act.op(lambda: nc.scalar.activation(out=rstd[:, 0:n], in_=rstd[:, 0:n], func=AF.Sqrt, bias=kb.c["epsc"][0][:, 0:1]),
                     reads=[rstdB, kb.c["epsc"][1]], writes=[rstdB])
            f.dve.op(lambda: nc.vector.reciprocal(out=rstd[:, 0:n], in_=rstd[:, 0:n]), reads=[rstdB], writes=[rstdB])
            ya, yaB = ya_r[ti % 2]
            for k in range(4):
                z, zB = z_r[k % 2]
                f.dve.op(lambda: nc.vector.tensor_tensor(out=z[:, 0:n], in0=y[:, k, 0:n], in1=mean[:, 0:n], op=ALU.subtract),
                         reads=[yB, meanB], writes=[zB])
                f.pool.op(lambda: nc.gpsimd.tensor_tensor(out=z[:, 0:n], in0=z[:, 0:n], in1=rstd[:, 0:n], op=ALU.mult),
                          reads=[zB, rstdB], writes=[zB])
                f.act.op(lambda: nc.scalar.activation(out=ya[:, k, 0:n], in_=z[:, 0:n], func=AF.Silu, scale=prm[:, 1, k:k + 1],
                                                      bias=prm[:, 2, k:k + 1]), reads=[zB, prmB], writes=[yaB])
            f.q_sp.dma(YA3[:, :, t0:t0 + n], ya[:, :, 0:n], reads=[yaB], writes=[YAb])
    f.barrier()


def phase_pool(kb, l, pool_w, pool_scale):
    nc, f = kb.nc, kb.f
    PT, PTb = kb.dram["PT"]
    YC, YCb = kb.dram["YC"]
    rc_d = kb.dram["tab_rc"][0]
    with ExitStack() as es:
        pw, pwB = kb.sb(es, "pw", [128, 4, 128], BF16)
        f.q_pool.dma(pw, pool_w[l].rearrange("g c d -> c g d"), writes=[pwB])
        psc, pscB = kb.sb(es, "psc", [128, 4], F32)
        f.q_sp.dma(psc, pool_scale[l].rearrange("(k p) -> p k", p=128), writes=[pscB])
        rc, rcB = kb.sb(es, "rc", [128, 2, 4, 16], F32)
        f.q_sp.dma(rc.rearrange("p a g e -> p (a g e)"), rc_d, writes=[rcB])
        ue_r = kb.ring(es, "uext", [128, 4, 512 + 16], BF16, 2)
        A_r = kb.ring(es, "pA", [128, 512 + 16], F32, 2)
        B_r = kb.ring(es, "pB", [128, 512 + 16], F32, 2)
        pm_r = kb.ring(es, "ppm", [128, 512], BF16, 2)
        pmf_r = kb.ring(es, "ppmf", [128, 16], F32, 2)
        yc_r = kb.ring(es, "pyc", [128, 4, 512], BF16, 2)
        PT3 = PT.rearrange("(k p) t -> p k t", p=128)
        YC3 = YC.rearrange("(k p) t -> p k t", p=128)
        it = 0
        for ti, (t0, n, slo, shi, b, isctx) in enumerate(seg_tiles()):
            ue, ueB = ue_r[ti % 2]
            load_halo(kb, ue, ueB, PT3, t0, n, 8, slo, shi)
            yc, ycB = yc_r[ti % 2]
            E = n + 16
            ai = 1 if isctx else 0
            for g in range(4):
                w = 2 << g
                A, AB = A_r[it % 2]
                Bt, BB = B_r[it % 2]
                it += 1
                f.dve.op(lambda: nc.vector.tensor_tensor(out=A[:, 1:E], in0=ue[:, g, 0:E - 1], in1=ue[:, g, 1:E], op=ALU.add),
                         reads=[ueB], writes=[AB])
                s, sB = A, AB
                if g >= 1:
                    f.pool.op(lambda: nc.gpsimd.tensor_tensor(out=Bt[:, 2:E - 1], in0=A[:, 1:E - 2], in1=A[:, 3:E], op=ALU.add),
                              reads=[AB], writes=[BB])
                    s, sB = Bt, BB
                if g >= 2:
                    f.dve.op(lambda: nc.vector.tensor_tensor(out=A[:, 4:E - 3], in0=Bt[:, 2:E - 5], in1=Bt[:, 6:E - 1], op=ALU.add),
                             reads=[BB], writes=[AB])
                    s, sB = A, AB
                if g >= 3:
                    f.pool.op(lambda: nc.gpsimd.tensor_tensor(out=Bt[:, 8:E - 7], in0=A[:, 4:E - 11], in1=A[:, 12:E - 3], op=ALU.add),
                              reads=[AB], writes=[BB])
                    s, sB = Bt, BB
                pm, pmB = pm_r[it % 2]
                f.dve.op(lambda: nc.vector.scalar_tensor_tensor(out=pm[:, 0:n], in0=s[:, 8:8 + n], scalar=1.0 / w, in1=ue[:, g, 8:8 + n],
                                                                op0=ALU.mult, op1=ALU.subtract), reads=[sB, ueB], writes=[pmB])
                pmf, pmfB = pmf_r[it % 2]
                if t0 == slo:
                    f.dve.op(lambda: nc.vector.tensor_tensor(out=pmf[:, 0:8], in0=s[:, 8:16], in1=rc[:, ai, g, 0:8], op=ALU.mult),
                             reads=[sB, rcB], writes=[pmfB])
                    f.dve.op(lambda: nc.vector.tensor_tensor(out=pm[:, 0:8], in0=pmf[:, 0:8], in1=ue[:, g, 8:16], op=ALU.subtract),
                             reads=[pmfB, ueB], writes=[pmB])
                if t0 + n == shi:
                    f.dve.op(lambda: nc.vector.tensor_tensor(out=pmf[:, 8:16], in0=s[:, n:n + 8], in1=rc[:, ai, g, 8:16], op=ALU.mult),
                             reads=[sB, rcB], writes=[pmfB])
                    f.dve.op(lambda: nc.vector.tensor_tensor(out=pm[:, n - 8:n], in0=pmf[:, 8:16], in1=ue[:, g, n:n + 8], op=ALU.subtract),
                             reads=[pmfB, ueB], writes=[pmB])
                p, pB = kb.ps()
                f.pe.op(lambda: nc.tensor.matmul(p[:, 0:n], lhsT=pw[:, g, :], rhs=pm[:, 0:n], start=True, stop=True),
                        reads=[pwB, pmB], writes=[pB])
                f.dve.op(lambda: nc.vector.tensor_scalar(out=yc[:, g, 0:n], in0=p[:, 0:n], scalar1=psc[:, g:g + 1], scalar2=None, op0=ALU.mult),
                         reads=[pB, pscB], writes=[ycB])
            f.q_sp.dma(YC3[:, :, t0:t0 + n], yc[:, :, 0:n], reads=[ycB], writes=[YCb])
    f.barrier()


def phase_swa(kb, l, swa_sink):
    nc, f = kb.nc, kb.f
    QT, QTb = kb.dram["QT"]
    KT, KTb = kb.dram["KT"]
    Vd, Vdb = kb.dram["V"]
    YB, YBb = kb.dram["YB"]
    ident, identB = kb.c["ident"]
    scale = 64 ** -0.5
    with ExitStack() as es:
        mL, mLB = kb.sb(es, "mL", [128, 4, 128], BF16)
        mR, mRB = kb.sb(es, "mR", [128, 4, 128], BF16)
        for m, mB, cm, st in ((mL, mLB, 1, -1), (mR, mRB, -1, 1)):
            f.pool.op(lambda: nc.gpsimd.memset(m, 1.0), writes=[mB])
            f.pool.op(lambda: nc.gpsimd.affine_select(out=m, in_=m, pattern=[[0, 4], [st, 128]], compare_op=ALU.is_ge, fill=0.0,
                                                      base=0, channel_multiplier=cm), reads=[mB], writes=[mB])
        esk, eskB = kb.sb(es, "esk", [128, 8], F32)
        f.q_sp.dma(esk, swa_sink[l:l + 1, :].partition_broadcast(128), writes=[eskB])
        f.act.op(lambda: nc.scalar.activation(out=esk, in_=esk, func=AF.Exp), reads=[eskB], writes=[eskB])
        qg_r = kb.ring(es, "qg", [64, 4, S], BF16, 2)
        kj_r = kb.ring(es, "kj", [64, S], BF16, 2)
        vj_r = kb.ring(es, "vj", [128, 34, 65], BF16, 2)
        pT_r = kb.ring(es, "spT", [128, 512], BF16, 6)
        den_r = kb.ring(es, "sden", [128, 8], F32, 2)
        yb_r = kb.ring(es, "syb", [128, 256], BF16, 2)
        ybT_r = kb.ring(es, "sybT", [128, 2, 512], BF16, 2)
        for vj, vjB in vj_r:
            f.pool.op(lambda: nc.gpsimd.memset(vj[:, :, 64:65], 1.0), writes=[vjB])
        ip = 0
        ig = 0
        for b in range(NB):
            for j in range(2):
                it = b * 2 + j
                qg, qgB = qg_r[it % 2]
                kj, kjB = kj_r[it % 2]
                vj, vjB = vj_r[it % 2]
                f.q_sp.dma(qg, QT[j * 256:(j + 1) * 256, b * S:(b + 1) * S].rearrange("(h d) t -> d h t", d=64), writes=[qgB])
                f.q_sp.dma(kj, KT[j * 64:(j + 1) * 64, b * S:(b + 1) * S], writes=[kjB])
                f.q_sp.dma(vj[:, :, 0:64], Vd[b * S:(b + 1) * S, j * 64:(j + 1) * 64].rearrange("(c p) d -> p c d", p=128), writes=[vjB])
                for i in range(34):
                    if i < 32:
                        chunks = [c for c in (i - 1, i, i + 1) if 0 <= c < 32] + [32, 33]
                    else:
                        chunks = [32, 33]
                    po, poB, pidx = kb.pin()
                    pts = []
                    for c in chunks:
                        ps, psB = kb.ps()
                        f.pe.op(lambda: nc.tensor.matmul(ps[:, 0:512], lhsT=kj[:, c * 128:(c + 1) * 128], rhs=qg[:, :, i * 128:(i + 1) * 128],
                                                         start=True, stop=True), reads=[kjB, qgB], writes=[psB])
                        pT, pTB = pT_r[ip % 6]
                        ip += 1
                        f.act.op(lambda: nc.scalar.activation(out=pT, in_=ps[:, 0:512], func=AF.Exp, scale=scale), reads=[psB], writes=[pTB])
                        if i < 32 and c == i - 1:
                            f.pool.op(lambda: nc.gpsimd.tensor_tensor(out=pT, in0=pT, in1=mL.rearrange("p h q -> p (h q)"), op=ALU.mult),
                                      reads=[pTB, mLB], writes=[pTB])
                        if i < 31 and c == i + 1:
                            f.pool.op(lambda: nc.gpsimd.tensor_tensor(out=pT, in0=pT, in1=mR.rearrange("p h q -> p (h q)"), op=ALU.mult),
                                      reads=[pTB, mRB], writes=[pTB])
                        pts.append((pT, pTB, c))
                    for hh in range(4):
                        for ci, (pT, pTB, c) in enumerate(pts):
                            f.pe.op(lambda: nc.tensor.matmul(po[:, hh * 65:(hh + 1) * 65], lhsT=pT[:, hh * 128:(hh + 1) * 128], rhs=vj[:, c, :],
                                                             start=(ci == 0), stop=(ci == len(pts) - 1)), reads=[pTB, vjB], writes=[poB])
                    den, denB = den_r[ig % 2]
                    po3 = po[:, 0:260].rearrange("p (h c) -> p h c", c=65)
                    f.dve.op(lambda: nc.vector.tensor_tensor(out=den[:, 0:4], in0=po3[:, :, 64], in1=esk[:, 4 * j:4 * j + 4], op=ALU.add),
                             reads=[poB, eskB], writes=[denB])
                    f.dve.op(lambda: nc.vector.reciprocal(out=den[:, 4:8], in_=den[:, 0:4]), reads=[denB], writes=[denB])
                    yb, ybB = yb_r[ig % 2]
                    for hh in range(4):
                        f.dve.op(lambda: nc.vector.tensor_scalar(out=yb[:, hh * 64:(hh + 1) * 64], in0=po[:, hh * 65:hh * 65 + 64],
                                                                 scalar1=den[:, 4 + hh:5 + hh], scalar2=None, op0=ALU.mult),
                                 reads=[poB, denB], writes=[ybB])
                    kb.unpin(pidx)
                    grp0 = (i // 4) * 4 if i < 32 else 32
                    gsz = 4 if i < 32 else 2
                    ybT, ybTB = ybT_r[(it * 9 + (i // 4)) % 2]
                    pt, ptB = kb.pb()
                    for k in range(2):
                        f.pe.op(lambda: nc.tensor.transpose(pt[:, k * 128:(k + 1) * 128], yb[:, k * 128:(k + 1) * 128], ident),
                                reads=[ybB, identB], writes=[ptB])
                    off = (i - grp0) * 128
                    f.act.op(lambda: nc.scalar.copy(out=ybT[:, :, off:off + 128], in_=pt[:, 0:256].rearrange("p (k t) -> p k t", k=2)),
                             reads=[ptB], writes=[ybTB])
                    if i - grp0 == gsz - 1:
                        tq0 = b * S + grp0 * 128
                        f.q_sp.dma(YB[j * 256:(j + 1) * 256, tq0:tq0 + gsz * 128].rearrange("(k p) t -> p k t", p=128),
                                   ybT[:, :, 0:gsz * 128], reads=[ybTB], writes=[YBb])
                    ig += 1
    f.barrier()


def phase_mla(kb, l, I=None):
    nc, f = kb.nc, kb.f
    QM, QMb = kb.dram["QM"]
    KN, KNb = kb.dram["KN"]
    KR, KRb = kb.dram["KR"]
    VM, VMb = kb.dram["VM"]
    YD, YDb = kb.dram["YD"]
    onesf, onesfB = kb.c["onesf"]
    scale = 96 ** -0.5
    with ExitStack() as es:
        kh_r = kb.ring(es, "kh", [96, S], BF16, 2)
        qh_r = kb.ring(es, "qh", [96, S], BF16, 2)
        vh_r = kb.ring(es, "vh", [128, 34, 65], BF16, 2)
        pT_r = kb.ring(es, "mpT", [128, 512], BF16, 4)
        rd_r = kb.ring(es, "mrd", [65, 512], F32, 2)
        on_r = kb.ring(es, "mon", [64, 512], F32, 2)
        yd_r = kb.ring(es, "myd", [64, 512], BF16, 2)
        for vh, vhB in vh_r:
            f.pool.op(lambda: nc.gpsimd.memset(vh[:, :, 64:65], 1.0), writes=[vhB])
        precast = I is not None and "w_gu" in I
        if precast:
            WBG, WBGb = kb.dram["WBG"]
            WBD, WBDb = kb.dram["WBD"]
            tg_r = kb.ring(es, "tg", [128, 8, 2048], BF16, 2)
            td_r = kb.ring(es, "td", [128, 8, 1024], BF16, 2)

        def loads(it):
            b, h = divmod(it, 8)
            kh, khB = kh_r[it % 2]
            qh, qhB = qh_r[it % 2]
            vh, vhB = vh_r[it % 2]
            f.q_sp.dma(kh[0:64, :], KN[h, :, b * S:(b + 1) * S], writes=[khB])
            f.q_sp.dma(kh[64:96, :], KR[:, b * S:(b + 1) * S], writes=[khB])
            f.q_sp.dma(qh, QM[h, :, b * S:(b + 1) * S], writes=[qhB])
            f.q_sp.dma(vh[:, :, 0:64], VM[b * S:(b + 1) * S, h * 64:(h + 1) * 64].rearrange("(c p) d -> p c d", p=128), writes=[vhB])
        ip = 0
        iq = 0
        loads(0)
        for b in range(NB):
            for h in range(8):
                it = b * 8 + h
                kh, khB = kh_r[it % 2]
                qh, qhB = qh_r[it % 2]
                vh, vhB = vh_r[it % 2]
                if it + 1 < NB * 8:
                    loads(it + 1)
                if precast:
                    for e in range(it * NE // (NB * 8), (it + 1) * NE // (NB * 8)):
                        tg, tgB = tg_r[e % 2]
                        td, tdB = td_r[e % 2]
                        f.q_pool.dma(tg, I["w_gu"][l, e].rearrange("(p k) c -> p k c", k=8), reads=[vhB], writes=[tgB])
                        f.q_pool.dma(td, I["w_down"][l, e].rearrange("(p k) c -> p k c", k=8), writes=[tdB])
                        f.q_pool.dma(WBG[e * 128:(e + 1) * 128, :], tg.rearrange("p k c -> p (k c)"), reads=[tgB], writes=[WBGb])
                        f.q_pool.dma(WBD[e * 128:(e + 1) * 128, :], td.rearrange("p k c -> p (k c)"), reads=[tdB], writes=[WBDb])
                for qi in range(9):
                    q0 = qi * 512
                    n = 512 if qi < 8 else 256
                    chunks = list(range(34)) if qi < 8 else [32, 33]
                    po, poB, pidx = kb.pin()
                    pend = []
                    LAG = 2
                    for ci in range(len(chunks)):
                        c = chunks[ci]
                        ps, psB = kb.ps()
                        f.pe.op(lambda: nc.tensor.matmul(ps[:, 0:n], lhsT=kh[:, c * 128:(c + 1) * 128], rhs=qh[:, q0:q0 + n], start=True, stop=True),
                                reads=[khB, qhB], writes=[psB])
                        pT, pTB = pT_r[ip % 4]
                        ip += 1
                        f.act.op(lambda: nc.scalar.activation(out=pT[:, 0:n], in_=ps[:, 0:n], func=AF.Exp, scale=scale), reads=[psB], writes=[pTB])
                        pend.append((pT, pTB, c, ci))
                        if len(pend) > LAG:
                            pT2, pT2B, c2, ci2 = pend.pop(0)
                            f.pe.op(lambda: nc.tensor.matmul(po[0:65, 0:n], lhsT=vh[:, c2, :], rhs=pT2[:, 0:n], start=(ci2 == 0), stop=(ci2 == len(chunks) - 1)),
                                    reads=[vhB, pT2B], writes=[poB])
                    while pend:
                        pT2, pT2B, c2, ci2 = pend.pop(0)
                        f.pe.op(lambda: nc.tensor.matmul(po[0:65, 0:n], lhsT=vh[:, c2, :], rhs=pT2[:, 0:n], start=(ci2 == 0), stop=(ci2 == len(chunks) - 1)),
                                reads=[vhB, pT2B], writes=[poB])
                    rd, rdB = rd_r[iq % 2]
                    on, onB = on_r[iq % 2]
                    yd, ydB = yd_r[iq % 2]
                    iq += 1
                    f.dve.op(lambda: nc.vector.reciprocal(out=rd[64:65, 0:n], in_=po[64:65, 0:n]), reads=[poB], writes=[rdB])
                    f.act.op(lambda: nc.scalar.copy(out=on[:, 0:n], in_=po[0:64, 0:n]), reads=[poB], writes=[onB])
                    kb.unpin(pidx)
                    pbc, pbcB = kb.ps()
                    f.pe.op(lambda: nc.tensor.matmul(pbc[0:64, 0:n], lhsT=onesf[64:65, 0:64], rhs=rd[64:65, 0:n], start=True, stop=True),
                            reads=[onesfB, rdB], writes=[pbcB])
                    f.dve.op(lambda: nc.vector.tensor_tensor(out=yd[:, 0:n], in0=on[:, 0:n], in1=pbc[0:64, 0:n], op=ALU.mult),
                             reads=[onB, pbcB], writes=[ydB])
                    f.q_sp.dma(YD[h * 64:(h + 1) * 64, b * S + q0:b * S + q0 + n], yd[:, 0:n], reads=[ydB], writes=[YDb])
    f.barrier()


def resid_ln(kb, es_bufs, src_y, src_yB, ysrc_is_psum_halves, xs, xsB, gt, gtB, lg, lgB, lb, lbB, dst_ap, dstB, idx):
    nc, f = kb.nc, kb.f
    t_r, st_r, o_r = es_bufs
    t, tB = t_r[idx % len(t_r)]
    st, stB = st_r[idx % len(st_r)]
    o, oB = o_r[idx % len(o_r)]
    for hf in range(2):
        ya, yaB = src_y[hf]
        f.dve.op(lambda: nc.vector.tensor_tensor(out=t[:, hf * 512:(hf + 1) * 512], in0=ya, in1=gt[:, hf * 512:(hf + 1) * 512], op=ALU.mult),
                 reads=[yaB, gtB], writes=[tB])
    f.dve.op(lambda: nc.vector.scalar_tensor_tensor(out=t, in0=xs, scalar=ALPHA, in1=t, op0=ALU.mult, op1=ALU.add),
             reads=[xsB, tB], writes=[tB])
    mean, rstd = ln_tile(kb, t, tB, st, stB)
    f.dve.op(lambda: nc.vector.tensor_scalar(out=o, in0=t, scalar1=mean, scalar2=rstd, op0=ALU.subtract, op1=ALU.mult),
             reads=[tB, stB], writes=[oB])
    f.pool.op(lambda: nc.gpsimd.tensor_tensor(out=o, in0=o, in1=lg, op=ALU.mult), reads=[oB, lgB], writes=[oB])
    f.pool.op(lambda: nc.gpsimd.tensor_tensor(out=o, in0=o, in1=lb, op=ALU.add), reads=[oB, lbB], writes=[oB])
    f.q_sp.dma(dst_ap, o, reads=[oB], writes=[dstB])


def phase_merge(kb, l, xsrc, I, ADA, XA, skip_ctx=False):
    nc, f = kb.nc, kb.f
    HT, HTb = kb.dram["HT"]
    XAb = kb.dram["XA"][1]
    with ExitStack() as es:
        wg, wgB = kb.sb(es, "wg", [128, 4, 8, D], BF16)
        for i in range(4):
            f.q_pool.dma(wg[:, i], I["w_gate"][l, i].rearrange("(k p) c -> p k c", p=128), writes=[wgB])
        wb, wbB = kb.sb(es, "wbr", [128, 4, 4, D], BF16)
        for i in range(4):
            f.q_pool.dma(wb[:, i], I["w_branch"][l, i].rearrange("(k p) c -> p k c", p=128), writes=[wbB])
        wo, woB = kb.sb(es, "wo", [128, 8, D], BF16)
        f.q_pool.dma(wo, I["w_out"][l].rearrange("(k p) c -> p k c", p=128), writes=[woB])
        bg, bgB = kb.sb(es, "bg", [128, 4, 8], F32)
        for i in range(4):
            f.q_sp.dma(bg[:, i, :], I["b_gate"][l, i].rearrange("(k p) -> p k", p=128), writes=[bgB])
        g1 = []
        for r in range(3):
            g, gB = kb.sb(es, f"g1_{r}", [128, D], F32)
            load_bcast(kb, g, gB, ADA[l, r:r + 1, 2 * D:3 * D])
            g1.append((g, gB))
        lg, lgB = kb.sb(es, "ln1g", [128, D], F32)
        lb, lbB = kb.sb(es, "ln1b", [128, D], F32)
        load_bcast(kb, lg, lgB, I["ln1_g"][l:l + 1, :])
        load_bcast(kb, lb, lbB, I["ln1_b"][l:l + 1, :])
        hT_r = kb.ring(es, "mhT", [128, 8, 512], BF16, 1)
        ys_r = kb.ring(es, "mys", [128, 16, 512], BF16, 1)
        mT_r = kb.ring(es, "mmT", [128, 8, 512], BF16, 1)
        sg_r = kb.ring(es, "msg", [128, 512], F32, 2)
        tmp_r = kb.ring(es, "mtmp", [128, 512], F32, 2)
        acc_r = kb.ring(es, "macc", [128, 512], F32, 2)
        xs_r = kb.ring(es, "mxs", [128, D], F32, 2)
        rl = (kb.ring(es, "mt", [128, D], F32, 1), kb.ring(es, "mst", [128, 16], F32, 2), kb.ring(es, "mo", [128, D], F32, 2))
        HT3 = HT.rearrange("(k p) t -> p k t", p=128)
        idx = 0
        for ti, (t0, n, b, isctx, pos) in enumerate(tiles_all()):
            if isctx and skip_ctx:
                continue
            r = 2 if isctx else b
            hT, hTB = hT_r[0]
            ys, ysB = ys_r[0]
            mT, mTB = mT_r[0]
            f.q_sp.dma(hT[:, :, 0:n], HT3[:, :, t0:t0 + n], writes=[hTB])
            for i, nm in enumerate(("YA", "YB", "YC", "YD")):
                f.q_sp.dma(ys[:, i * 4:(i + 1) * 4, 0:n], kb.dram[nm][0].rearrange("(k p) t -> p k t", p=128)[:, :, t0:t0 + n], writes=[ysB])
            for oc in range(8):
                acc, accB = acc_r[oc % 2]
                for i in range(4):
                    pg, pgB = kb.ps()
                    for k in range(8):
                        f.pe.op(lambda: nc.tensor.matmul(pg[:, 0:n], lhsT=wg[:, i, k, oc * 128:(oc + 1) * 128], rhs=hT[:, k, 0:n],
                                                         start=(k == 0), stop=(k == 7)), reads=[wgB, hTB], writes=[pgB])
                    sg, sgB = sg_r[i % 2]
                    f.act.op(lambda: nc.scalar.activation(out=sg[:, 0:n], in_=pg[:, 0:n], func=AF.Sigmoid, bias=bg[:, i, oc:oc + 1]),
                             reads=[pgB, bgB], writes=[sgB])
                    pbr, pbrB = kb.ps()
                    for k in range(4):
                        f.pe.op(lambda: nc.tensor.matmul(pbr[:, 0:n], lhsT=wb[:, i, k, oc * 128:(oc + 1) * 128], rhs=ys[:, i * 4 + k, 0:n],
                                                         start=(k == 0), stop=(k == 3)), reads=[wbB, ysB], writes=[pbrB])
                    if i == 0:
                        f.dve.op(lambda: nc.vector.tensor_tensor(out=acc[:, 0:n], in0=sg[:, 0:n], in1=pbr[:, 0:n], op=ALU.mult),
                                 reads=[sgB, pbrB], writes=[accB])
                    else:
                        tmp, tmpB = tmp_r[i % 2]
                        f.dve.op(lambda: nc.vector.tensor_tensor(out=tmp[:, 0:n], in0=sg[:, 0:n], in1=pbr[:, 0:n], op=ALU.mult),
                                 reads=[sgB, pbrB], writes=[tmpB])
                        if i < 3:
                            f.pool.op(lambda: nc.gpsimd.tensor_tensor(out=acc[:, 0:n], in0=acc[:, 0:n], in1=tmp[:, 0:n], op=ALU.add),
                                      reads=[accB, tmpB], writes=[accB])
                        else:
                            f.pool.op(lambda: nc.gpsimd.tensor_tensor(out=mT[:, oc, 0:n], in0=acc[:, 0:n], in1=tmp[:, 0:n], op=ALU.add),
                                      reads=[accB, tmpB], writes=[mTB])
            g, gB = g1[r]
            for j in range(n // 128):
                xs, xsB = xs_r[idx % 2]
                f.q_sp.dma(xs, xsrc(t0 + j * 128, 128), writes=[xsB])
                halves = []
                for hf in range(2):
                    po, poB = kb.ps()
                    for k in range(8):
                        f.pe.op(lambda: nc.tensor.matmul(po[:, 0:512], lhsT=mT[:, k, j * 128:(j + 1) * 128], rhs=wo[:, k, hf * 512:(hf + 1) * 512],
                                                         start=(k == 0), stop=(k == 7)), reads=[mTB, woB], writes=[poB])
                    halves.append((po[:, 0:512], poB))
                resid_ln(kb, rl, halves, None, True, xs, xsB, g, gB, lg, lgB, lb, lbB, XA[t0 + j * 128:t0 + (j + 1) * 128, :], XAb, idx)
                idx += 1
    f.barrier()


NT128 = T // 128
BIGIDX = 4.0e6


def phase_moe(kb, l, I, ADA, XA, XB, out, last):
    nc, f = kb.nc, kb.f
    dr = kb.dram
    H2, H2b = dr["H2"]
    XG, XGb = dr["XG"]
    YG, YGb = dr["YG"]
    XAb = dr["XA"][1]
    ident, identB = kb.c["ident"]
    onesb, onesbB = kb.c["onesb"]
    onesf, onesfB = kb.c["onesf"]
    with ExitStack() as es0:
        TK, TKB = kb.sb(es0, "TK", [128, NT128, 12], F32)
        WK, WKB = kb.sb(es0, "WK", [128, NT128, 4], F32)
        DSTI, DSTIB = kb.sb(es0, "DSTI", [128, NT128, 4], I32)
        carry, carryB = kb.sb(es0, "carry", [128, 32], F32)
        eio, eioB = kb.sb(es0, "eio", [128, 32], F32)
        eioi, eioiB = kb.sb(es0, "eioi", [128, 32], I32)
        f.pool.op(lambda: nc.gpsimd.iota(eioi, pattern=[[1, 32]], base=0, channel_multiplier=0), writes=[eioiB])
        f.dve.op(lambda: nc.vector.tensor_copy(out=eio, in_=eioi), reads=[eioiB], writes=[eioB])
        f.pool.op(lambda: nc.gpsimd.memset(carry, 0.0), writes=[carryB])
        IDXG, IDXGB = kb.sb(es0, "IDXG", [128, NBLK], I32)
        BEI, BEIB = kb.sb(es0, "BEI", [128, NBLK], I32)
        IDX2, IDX2B = kb.sb(es0, "IDX2", [128, 2, NBLK], I32)
        BE, BEB = kb.sb(es0, "BE", [128, NBLK], F32)
        with ExitStack() as es:
            U, UB = kb.sb(es, "U", [128, 128], BF16)
            f.pool.op(lambda: nc.gpsimd.memset(U, 1.0), writes=[UB])
            f.pool.op(lambda: nc.gpsimd.affine_select(out=U, in_=U, pattern=[[1, 128]], compare_op=ALU.is_gt, fill=0.0, base=0,
                                                      channel_multiplier=-1), reads=[UB], writes=[UB])
            wr, wrB = kb.sb(es, "wr", [128, 8, 32], BF16)
            f.q_pool.dma(wr, I["router_w"][l].rearrange("(k p) e -> p k e", p=128), writes=[wrB])
            rb, rbB = kb.sb(es, "rb", [1, 32], BF16)
            f.q_pool.dma(rb, I["router_b"][l:l + 1, :], writes=[rbB])
            mods = []
            for r in range(3):
                sc, scB = kb.sb(es, f"sc2_{r}", [128, D], F32)
                sh, shB = kb.sb(es, f"sh2_{r}", [128, D], F32)
                load_bcast(kb, sh, shB, ADA[l, r:r + 1, 3 * D:4 * D])
                load_bcast(kb, sc, scB, ADA[l, r:r + 1, 4 * D:5 * D])
                f.pool.op(lambda: nc.gpsimd.tensor_scalar(out=sc, in0=sc, scalar1=1.0, scalar2=None, op0=ALU.add), reads=[scB], writes=[scB])
                mods.append((sc, scB, sh, shB))
            xs_r = kb.ring(es, "rxs", [128, D], F32, 2)
            st_r = kb.ring(es, "rst", [128, 16], F32, 2)
            xn_r = kb.ring(es, "rxn", [128, D], F32, 2)
            hb_r = kb.ring(es, "rhb", [128, D], BF16, 2)
            hT_r = kb.ring(es, "rhT", [128, 8, 128], BF16, 2)
            lg_r = kb.ring(es, "rlg", [128, 32], F32, 2)
            t8_r = kb.ring(es, "rt8", [128, 16], F32, 2)
            M_r = kb.ring(es, "rM", [128, 32], BF16, 2)
            rk_r = kb.ring(es, "rrk", [128, 32], F32, 2)
            jk_r = kb.ring(es, "rjk", [128, 32], F32, 2)
            for it in range(NT128):
                t0 = it * 128
                b, o = divmod(t0, S)
                r = 2 if o >= L else b
                sc, scB, sh, shB = mods[r]
                xs, xsB = xs_r[it % 2]
                st, stB = st_r[it % 2]
                xn, xnB = xn_r[it % 2]
                hb, hbB = hb_r[it % 2]
                hT, hTB = hT_r[it % 2]
                f.q_sp.dma(xs, XA[t0:t0 + 128, :], writes=[xsB])
                mean, rstd = ln_tile(kb, xs, xsB, st, stB)
                f.dve.op(lambda: nc.vector.tensor_scalar(out=xn, in0=xs, scalar1=mean, scalar2=rstd, op0=ALU.subtract, op1=ALU.mult),
                         reads=[xsB, stB], writes=[xnB])
                f.pool.op(lambda: nc.gpsimd.tensor_tensor(out=xn, in0=xn, in1=sc, op=ALU.mult), reads=[xnB, scB], writes=[xnB])
                f.dve.op(lambda: nc.vector.tensor_tensor(out=hb, in0=xn, in1=sh, op=ALU.add), reads=[xnB, shB], writes=[hbB])
                f.q_sp.dma(H2[t0:t0 + 128, :], hb, reads=[hbB], writes=[H2b])
                pt, ptB = kb.pb()
                for k in range(8):
                    f.pe.op(lambda: nc.tensor.transpose(pt[:, k * 128:(k + 1) * 128], hb[:, k * 128:(k + 1) * 128], ident),
                            reads=[hbB, identB], writes=[ptB])
                f.act.op(lambda: nc.scalar.copy(out=hT, in_=pt.rearrange("p (k t) -> p k t", k=8)), reads=[ptB], writes=[hTB])
                pl, plB = kb.ps()
                for k in range(8):
                    f.pe.op(lambda: nc.tensor.matmul(pl[:, 0:32], lhsT=hT[:, k, :], rhs=wr[:, k, :], start=(k == 0), stop=False),
                            reads=[hTB, wrB], writes=[plB])
                f.pe.op(lambda: nc.tensor.matmul(pl[:, 0:32], lhsT=onesb[0:1, :], rhs=rb, start=False, stop=True),
                        reads=[onesbB, rbB], writes=[plB])
                lg, lgB = lg_r[it % 2]
                f.act.op(lambda: nc.scalar.copy(out=lg, in_=pl[:, 0:32]), reads=[plB], writes=[lgB])
                t8, t8B = t8_r[it % 2]
                f.dve.op(lambda: nc.vector.max(out=t8[:, 0:8], in_=lg), reads=[lgB], writes=[t8B])
                M, MB = M_r[it % 2]
                f.dve.op(lambda: nc.vector.tensor_scalar(out=M, in0=lg, scalar1=t8[:, 3:4], scalar2=None, op0=ALU.is_ge),
                         reads=[lgB, t8B], writes=[MB])
                pr, prB = kb.ps()
                f.pe.op(lambda: nc.tensor.matmul(pr[:, 0:32], lhsT=U, rhs=M, start=True, stop=True), reads=[UB, MB], writes=[prB])
                f.pe.op(lambda: nc.tensor.matmul(pr[:, 32:64], lhsT=onesb, rhs=M, start=True, stop=True), reads=[onesbB, MB], writes=[prB])
                rk, rkB = rk_r[it % 2]
                f.dve.op(lambda: nc.vector.tensor_tensor(out=rk, in0=pr[:, 0:32], in1=carry, op=ALU.add), reads=[prB, carryB], writes=[rkB])
                f.dve.op(lambda: nc.vector.tensor_tensor(out=carry, in0=pr[:, 32:64], in1=carry, op=ALU.add), reads=[prB, carryB], writes=[carryB])
                jk, jkB = jk_r[it % 2]
                for k in range(4):
                    f.dve.op(lambda: nc.vector.scalar_tensor_tensor(out=jk, in0=lg, scalar=t8[:, k:k + 1], in1=rk, op0=ALU.is_equal, op1=ALU.mult,
                                                                    accum_out=TK[:, it, 4 + k:5 + k]), reads=[lgB, t8B, rkB], writes=[jkB, TKB])
                    f.dve.op(lambda: nc.vector.scalar_tensor_tensor(out=jk, in0=lg, scalar=t8[:, k:k + 1], in1=eio, op0=ALU.is_equal, op1=ALU.mult,
                                                                    accum_out=TK[:, it, 8 + k:9 + k]), reads=[lgB, t8B, eioB], writes=[jkB, TKB])
                f.dve.op(lambda: nc.vector.tensor_scalar(out=t8[:, 8:9], in0=t8[:, 0:1], scalar1=-1.0, scalar2=None, op0=ALU.mult),
                         reads=[t8B], writes=[t8B])
                f.act.op(lambda: nc.scalar.activation(out=TK[:, it, 0:4], in_=t8[:, 0:4], func=AF.Exp, bias=t8[:, 8:9], accum_out=t8[:, 9:10]),
                         reads=[t8B], writes=[TKB, t8B])
                f.dve.op(lambda: nc.vector.reciprocal(out=t8[:, 10:11], in_=t8[:, 9:10]), reads=[t8B], writes=[t8B])
                f.dve.op(lambda: nc.vector.tensor_scalar(out=WK[:, it, :], in0=TK[:, it, 0:4], scalar1=t8[:, 10:11], scalar2=None, op0=ALU.mult),
                         reads=[TKB, t8B], writes=[WKB])
        f.barrier()
        with ExitStack() as es:
            MAXB = T // BLK + 1
            i256i, i256iB = kb.sb(es, "i256i", [128, NBLK], I32)
            i256, i256B = kb.sb(es, "i256", [128, NBLK], F32)
            f.pool.op(lambda: nc.gpsimd.iota(i256i, pattern=[[BLK, NBLK]], base=0, channel_multiplier=0), writes=[i256iB])
            f.dve.op(lambda: nc.vector.tensor_copy(out=i256, in_=i256i), reads=[i256iB], writes=[i256B])
            cmp, cmpB = kb.sb(es, "cmpA", [128, 32, MAXB], F32)
            f.dve.op(lambda: nc.vector.tensor_tensor(out=cmp, in0=carry.unsqueeze(2).to_broadcast([128, 32, MAXB]),
                                                     in1=i256[:, 0:MAXB].unsqueeze(1).to_broadcast([128, 32, MAXB]), op=ALU.is_gt),
                     reads=[carryB, i256B], writes=[cmpB])
            pe_a, pe_aB = kb.sb(es, "pend_a", [128, 32], F32)
            pe_b, pe_bB = kb.sb(es, "pend_b", [128, 32], F32)
            pst, pstB = kb.sb(es, "pstart", [128, 32], F32)
            pad, padB = kb.sb(es, "padded", [128, 32], F32)
            f.dve.op(lambda: nc.vector.reduce_sum(out=pad, in_=cmp, axis=AX.X), reads=[cmpB], writes=[padB])
            f.dve.op(lambda: nc.vector.tensor_scalar(out=pad, in0=pad, scalar1=float(BLK), scalar2=None, op0=ALU.mult), reads=[padB], writes=[padB])
            f.dve.op(lambda: nc.vector.tensor_copy(out=pe_a, in_=pad), reads=[padB], writes=[pe_aB])
            cur, curB, nxt, nxtB = pe_a, pe_aB, pe_b, pe_bB
            for s in (1, 2, 4, 8, 16):
                f.dve.op(lambda: nc.vector.tensor_copy(out=nxt[:, 0:s], in_=cur[:, 0:s]), reads=[curB], writes=[nxtB])
                f.dve.op(lambda: nc.vector.tensor_tensor(out=nxt[:, s:32], in0=cur[:, s:32], in1=cur[:, 0:32 - s], op=ALU.add),
                         reads=[curB], writes=[nxtB])
                cur, curB, nxt, nxtB = nxt, nxtB, cur, curB
            pend, pendB = cur, curB
            f.dve.op(lambda: nc.vector.tensor_tensor(out=pst, in0=pend, in1=pad, op=ALU.subtract), reads=[pendB, padB], writes=[pstB])
            cmp2, cmp2B = kb.sb(es, "cmpB", [128, NBLK, 32], F32)
            f.dve.op(lambda: nc.vector.tensor_tensor(out=cmp2, in0=pend.unsqueeze(1).to_broadcast([128, NBLK, 32]),
                                                     in1=i256.unsqueeze(2).to_broadcast([128, NBLK, 32]), op=ALU.is_le),
                     reads=[pendB, i256B], writes=[cmp2B])
            f.dve.op(lambda: nc.vector.reduce_sum(out=BE, in_=cmp2, axis=AX.X), reads=[cmp2B], writes=[BEB])
            f.dve.op(lambda: nc.vector.tensor_scalar(out=BE, in0=BE, scalar1=31.0, scalar2=None, op0=ALU.min), reads=[BEB], writes=[BEB])
            chg, chgB = kb.sb(es, "chg", [128, NBLK], F32)
            f.pool.op(lambda: nc.gpsimd.memset(chg[:, 0:1], 1.0), writes=[chgB])
            f.dve.op(lambda: nc.vector.tensor_tensor(out=chg[:, 1:NBLK], in0=BE[:, 1:NBLK], in1=BE[:, 0:NBLK - 1], op=ALU.not_equal),
                     reads=[BEB], writes=[chgB])
            base, baseB = kb.sb(es, "ibase", [128, NBLK], F32)
            f.dve.op(lambda: nc.vector.tensor_scalar(out=base, in0=BE, scalar1=128.0, scalar2=-BIGIDX, op0=ALU.mult, op1=ALU.add),
                     reads=[BEB], writes=[baseB])
            f.dve.op(lambda: nc.vector.tensor_tensor(out=base, in0=base, in1=chg, op=ALU.mult), reads=[baseB, chgB], writes=[baseB])
            pio_i, pio_iB = kb.sb(es, "pio_i", [128, 8], I32)
            pio, pioB = kb.sb(es, "pio", [128, 8], F32)
            f.pool.op(lambda: nc.gpsimd.iota(pio_i, pattern=[[128, 8]], base=0, channel_multiplier=1), writes=[pio_iB])
            f.dve.op(lambda: nc.vector.tensor_copy(out=pio, in_=pio_i), reads=[pio_iB], writes=[pioB])
            f.dve.op(lambda: nc.vector.tensor_scalar(out=base, in0=base, scalar1=BIGIDX, scalar2=None, op0=ALU.add), reads=[baseB], writes=[baseB])
            idxf, idxfB = kb.sb(es, "idxf", [128, NBLK], F32)
            f.dve.op(lambda: nc.vector.tensor_scalar(out=idxf, in0=base, scalar1=pio[:, 0:1], scalar2=None, op0=ALU.add),
                     reads=[baseB, pioB], writes=[idxfB])
            f.dve.op(lambda: nc.vector.tensor_copy(out=IDXG, in_=idxf), reads=[idxfB], writes=[IDXGB])
            idx2, idx2B = kb.sb(es, "idx2", [128, NBLK], F32)
            f.dve.op(lambda: nc.vector.tensor_scalar(out=idx2, in0=idxf, scalar1=2.0, scalar2=None, op0=ALU.mult), reads=[idxfB], writes=[idx2B])
            f.dve.op(lambda: nc.vector.tensor_copy(out=IDX2[:, 0, :], in_=idx2), reads=[idx2B], writes=[IDX2B])
            f.dve.op(lambda: nc.vector.tensor_scalar(out=idx2, in0=idx2, scalar1=1.0, scalar2=None, op0=ALU.add), reads=[idx2B], writes=[idx2B])
            f.dve.op(lambda: nc.vector.tensor_copy(out=IDX2[:, 1, :], in_=idx2), reads=[idx2B], writes=[IDX2B])
            f.dve.op(lambda: nc.vector.tensor_scalar(out=idxf, in0=base, scalar1=1.0 / 128.0, scalar2=float(l * NE), op0=ALU.mult, op1=ALU.add),
                     reads=[baseB], writes=[idxfB])
            f.dve.op(lambda: nc.vector.tensor_copy(out=BEI, in_=idxf), reads=[idxfB], writes=[BEIB])
            zt, ztB = kb.sb(es, "zt", [128, 8192], BF16)
            f.pool.op(lambda: nc.gpsimd.memset(zt, 0.0), writes=[ztB])
            XGf = XG.rearrange("(a p r) c -> a p (r c)", p=128, r=8)
            for a in range(NROWS // 1024):
                f.q_sp.dma(XGf[a], zt, reads=[ztB], writes=[XGb])
            dst, dstB = kb.sb(es, "dstf", [128, NT128, 4], F32)
            jk_r = kb.ring(es, "cjk", [128, 32], F32, 2)
            for it in range(NT128):
                jk, jkB = jk_r[it % 2]
                for k in range(4):
                    f.dve.op(lambda: nc.vector.scalar_tensor_tensor(out=jk, in0=eio, scalar=TK[:, it, 8 + k:9 + k], in1=pst, op0=ALU.is_equal,
                                                                    op1=ALU.mult, accum_out=dst[:, it, k:k + 1]),
                             reads=[eioB, TKB, pstB], writes=[jkB, dstB])
            f.dve.op(lambda: nc.vector.tensor_tensor(out=dst, in0=dst, in1=TK[:, :, 4:8], op=ALU.add), reads=[dstB, TKB], writes=[dstB])
            f.dve.op(lambda: nc.vector.tensor_copy(out=DSTI, in_=dst), reads=[dstB], writes=[DSTIB])
            f.barrier()
            hb_r = kb.ring(es, "shb", [128, D], BF16, 3)
            for it in range(NT128):
                hb, hbB = hb_r[it % 3]
                f.q_sp.dma(hb, H2[it * 128:(it + 1) * 128, :], writes=[hbB])
                for k in range(4):
                    f.q_pool.dma(None, None, reads=[hbB, DSTIB], writes=[],
                                 fn=lambda g: g.indirect_dma_start(out=XG, out_offset=bass.IndirectOffsetOnAxis(ap=DSTI[:, it, k:k + 1], axis=0),
                                                                   in_=hb, in_offset=None, bounds_check=kb.breg(NROWS - 1), oob_is_err=False))
        f.barrier()
        with ExitStack() as es:
            wgu2, wguB = kb.sb(es, "wgu", [128, 8 * 2048], BF16)
            wdn2, wdnB = kb.sb(es, "wdn", [128, 8 * D], BF16)
            wgu = wgu2.rearrange("p (k c) -> p k c", k=8)
            wdn = wdn2.rearrange("p (k c) -> p k c", k=8)
            WBG = dr["WBG"][0]
            WBD = dr["WBD"][0]
            bgu, bguB = kb.sb(es, "bgu", [128, 2048], BF16)
            bdn, bdnB = kb.sb(es, "bdn", [128, D], BF16)
            WGU2 = I["w_gu"].rearrange("l e (q k) c -> (l e q) k c", k=4)
            WDN2 = I["w_down"].rearrange("l e (q k) c -> (l e q) k c", k=4)
            BGU2 = I["b_gu"].rearrange("l e c -> (l e) c")
            BDN2 = I["b_down"].rearrange("l e c -> (l e) c")
            xr_r = kb.ring(es, "bxr", [128, 2, D], BF16, 2)
            xT_r = kb.ring(es, "bxT", [128, 8, BLK], BF16, 2)
            aT_r = kb.ring(es, "baT", [128, 8, BLK], BF16, 2)
            g_r = kb.ring(es, "bg", [128, BLK], F32, 2)
            sg_r = kb.ring(es, "bsg", [128, BLK], F32, 2)
            u_r = kb.ring(es, "bu", [128, BLK], F32, 2)
            yo_r = kb.ring(es, "byo", [128, 2, D], F32, 2)
            for bi in range(NBLK):
                f.q_pool.dma(None, None, reads=[IDXGB], writes=[wguB],
                             fn=lambda g: g.indirect_dma_start(out=wgu2, out_offset=None, in_=WBG,
                                                               in_offset=bass.IndirectOffsetOnAxis(ap=IDXG[:, bi:bi + 1], axis=0),
                                                               bounds_check=kb.breg(NE * 128 - 1), oob_is_err=False))
                f.q_pool.dma(None, None, reads=[BEIB], writes=[bguB],
                             fn=lambda g: g.indirect_dma_start(out=bgu, out_offset=None, in_=BGU2,
                                                               in_offset=bass.IndirectOffsetOnAxis(ap=BEI[:, bi:bi + 1], axis=0),
                                                               bounds_check=kb.breg((l + 1) * NE - 1), oob_is_err=False))
                f.q_pool.dma(None, None, reads=[IDXGB], writes=[wdnB],
                             fn=lambda g: g.indirect_dma_start(out=wdn2, out_offset=None, in_=WBD,
                                                               in_offset=bass.IndirectOffsetOnAxis(ap=IDXG[:, bi:bi + 1], axis=0),
                                                               bounds_check=kb.breg(NE * 128 - 1), oob_is_err=False))
                f.q_pool.dma(None, None, reads=[BEIB], writes=[bdnB],
                             fn=lambda g: g.indirect_dma_start(out=bdn, out_offset=None, in_=BDN2,
                                                               in_offset=bass.IndirectOffsetOnAxis(ap=BEI[:, bi:bi + 1], axis=0),
                                                               bounds_check=kb.breg((l + 1) * NE - 1), oob_is_err=False))
                xr, xrB = xr_r[bi % 2]
                f.q_sp.dma(xr, XG[bi * BLK:(bi + 1) * BLK, :].rearrange("(j p) c -> p j c", p=128), writes=[xrB])
                xT, xTB = xT_r[bi % 2]
                for j in range(2):
                    pt, ptB = kb.pb()
                    for k in range(8):
                        f.pe.op(lambda: nc.tensor.transpose(pt[:, k * 128:(k + 1) * 128], xr[:, j, :].rearrange("r (p k) -> r k p", k=8)[:, k, :], ident),
                                reads=[xrB, identB], writes=[ptB])
                    f.act.op(lambda: nc.scalar.copy(out=xT[:, :, j * 128:(j + 1) * 128], in_=pt.rearrange("p (k t) -> p k t", k=8)),
                             reads=[ptB], writes=[xTB])
                aT, aTB = aT_r[bi % 2]
                for oc in range(8):
                    pz = []
                    for half in range(2):
                        p, pB = kb.ps()
                        for k in range(8):
                            wsl = wgu[:, k, half * 1024:(half + 1) * 1024].rearrange("p (m j) -> p j m", j=8)[:, oc, :]
                            f.pe.op(lambda: nc.tensor.matmul(p[:, 0:BLK], lhsT=wsl, rhs=xT[:, k, :], start=(k == 0), stop=False),
                                    reads=[wguB, xTB], writes=[pB])
                        bsl = bgu[0:1, half * 1024:(half + 1) * 1024].rearrange("p (m j) -> p j m", j=8)[:, oc, :]
                        f.pe.op(lambda: nc.tensor.matmul(p[:, 0:BLK], lhsT=bsl, rhs=kb.c["onesrow"][0][0:1, 0:BLK],
                                                         start=False, stop=True), reads=[bguB, kb.c["onesrow"][1]], writes=[pB])
                        pz.append((p, pB))
                    (pg, pgB), (pu, puB) = pz
                    g, gB = g_r[oc % 2]
                    sg, sgB = sg_r[oc % 2]
                    u, uB = u_r[oc % 2]
                    f.dve.op(lambda: nc.vector.tensor_scalar(out=g, in0=pg[:, 0:BLK], scalar1=7.0, scalar2=None, op0=ALU.min), reads=[pgB], writes=[gB])
                    f.act.op(lambda: nc.scalar.activation(out=sg, in_=g, func=AF.Sigmoid, scale=1.702), reads=[gB], writes=[sgB])
                    f.dve.op(lambda: nc.vector.tensor_scalar(out=u, in0=pu[:, 0:BLK], scalar1=-7.0, scalar2=7.0, op0=ALU.max, op1=ALU.min),
                             reads=[puB], writes=[uB])
                    f.dve.op(lambda: nc.vector.scalar_tensor_tensor(out=u, in0=u, scalar=1.0, in1=g, op0=ALU.add, op1=ALU.mult),
                             reads=[uB, gB], writes=[uB])
                    f.pool.op(lambda: nc.gpsimd.tensor_tensor(out=aT[:, oc, :], in0=u, in1=sg, op=ALU.mult), reads=[uB, sgB], writes=[aTB])
                yo, yoB = yo_r[bi % 2]
                for j in range(2):
                    for hf in range(2):
                        p, pB = kb.ps()
                        for k in range(8):
                            f.pe.op(lambda: nc.tensor.matmul(p[:, 0:512], lhsT=aT[:, k, j * 128:(j + 1) * 128], rhs=wdn[:, k, hf * 512:(hf + 1) * 512],
                                                             start=(k == 0), stop=False), reads=[aTB, wdnB], writes=[pB])
                        f.pe.op(lambda: nc.tensor.matmul(p[:, 0:512], lhsT=onesb[0:1, :], rhs=bdn[0:1, hf * 512:(hf + 1) * 512], start=False, stop=True),
                                reads=[onesbB, bdnB], writes=[pB])
                        f.act.op(lambda: nc.scalar.copy(out=yo[:, j, hf * 512:(hf + 1) * 512], in_=p[:, 0:512]), reads=[pB], writes=[yoB])
                f.q_sp.dma(YG[bi * BLK:(bi + 1) * BLK, :].rearrange("(j p) c -> p j c", p=128), yo, reads=[yoB], writes=[YGb])
        f.barrier()
        with ExitStack() as es:
            g2 = []
            for r in range(3):
                g, gB = kb.sb(es, f"g2_{r}", [128, D], F32)
                load_bcast(kb, g, gB, ADA[l, r:r + 1, 5 * D:6 * D])
                g2.append((g, gB))
            lg, lgB = kb.sb(es, "ln2g", [128, D], F32)
            lb, lbB = kb.sb(es, "ln2b", [128, D], F32)
            load_bcast(kb, lg, lgB, I["ln2_g"][l:l + 1, :])
            load_bcast(kb, lb, lbB, I["ln2_b"][l:l + 1, :])
            yk_r = kb.ring(es, "eyk", [128, D], F32, 8)
            m_r = kb.ring(es, "em", [128, D], F32, 2)
            xs_r = kb.ring(es, "exs", [128, D], F32, 2)
            rl = (kb.ring(es, "et", [128, D], F32, 2), kb.ring(es, "est", [128, 16], F32, 2), kb.ring(es, "eo", [128, D], F32, 2))
            outB = dr["out"][1]
            idx = 0
            tl = [it for it in range(NT128) if not (last and (it * 128) % S >= L)]

            def gath(n):
                it = tl[n]
                res = []
                for k in range(4):
                    yk, ykB = yk_r[(n * 4 + k) % 8]
                    f.q_pool.dma(None, None, reads=[DSTIB, YGb], writes=[ykB],
                                 fn=lambda g: g.indirect_dma_start(out=yk, out_offset=None, in_=YG,
                                                                   in_offset=bass.IndirectOffsetOnAxis(ap=DSTI[:, it, k:k + 1], axis=0),
                                                                   bounds_check=kb.breg(NROWS - 1), oob_is_err=False))
                    res.append((yk, ykB))
                return res
            g_next = gath(0)
            for n_, it in enumerate(tl):
                t0 = it * 128
                b, o = divmod(t0, S)
                isctx = o >= L
                r = 2 if isctx else b
                yks = g_next
                if n_ + 1 < len(tl):
                    g_next = gath(n_ + 1)
                m, mB = m_r[idx % 2]
                f.dve.op(lambda: nc.vector.tensor_scalar(out=m, in0=yks[0][0], scalar1=WK[:, it, 0:1], scalar2=None, op0=ALU.mult),
                         reads=[yks[0][1], WKB], writes=[mB])
                for k in range(1, 4):
                    f.dve.op(lambda: nc.vector.scalar_tensor_tensor(out=m, in0=yks[k][0], scalar=WK[:, it, k:k + 1], in1=m, op0=ALU.mult, op1=ALU.add),
                             reads=[yks[k][1], WKB, mB], writes=[mB])
                xs, xsB = xs_r[idx % 2]
                f.q_sp.dma(xs, XA[t0:t0 + 128, :], writes=[xsB])
                if last:
                    dst_ap, dB = out[b * L + o:b * L + o + 128, :], outB
                else:
                    dst_ap, dB = XB[t0:t0 + 128, :], dr["XB"][1]
                g, gB = g2[r]
                resid_ln(kb, rl, [(m[:, 0:512], mB), (m[:, 512:1024], mB)], None, False, xs, xsB, g, gB, lg, lgB, lb, lbB, dst_ap, dB, idx)
                idx += 1
    f.barrier()
import numpy as np


def make_tables():
    t = np.arange(L)
    row = (t // 64).astype(np.float32)
    col = (t % 64).astype(np.float32)

    def tab(rot_dim):
        nf = rot_dim // 4
        inv = (10000.0 ** (-np.arange(nf, dtype=np.float32) / nf)).astype(np.float32)
        ang = np.concatenate([row[:, None] * inv, col[:, None] * inv], axis=-1).astype(np.float32)
        return np.cos(ang).astype(np.float32), np.sin(ang).astype(np.float32)
    c64, s64 = tab(64)
    c32, s32 = tab(32)
    out = {}
    idx = (np.arange(128) % 64) % 32
    out["c64"] = np.ascontiguousarray(c64[:, idx].T)
    out["s64"] = np.ascontiguousarray(s64[:, idx].T)
    idx = np.arange(32) % 16
    out["c32"] = np.ascontiguousarray(c32[:, idx].T)
    out["s32"] = np.ascontiguousarray(s32[:, idx].T)
    c96 = np.ones((96, L), np.float32)
    s96 = np.zeros((96, L), np.float32)
    c96[64:] = out["c32"]
    s96[64:] = out["s32"]
    out["c96"] = c96
    out["s96"] = s96
    rc = np.zeros((2, 4, 16), np.float32)
    for a, Ls in enumerate((L, C)):
        for g in range(4):
            w = 2 << g
            for e in range(8):
                tau = e
                rc[a, g, e] = 1.0 / (min(tau + w // 2, Ls) - max(tau - w // 2, 0))
                tau = Ls - 8 + e
                rc[a, g, 8 + e] = 1.0 / (min(tau + w // 2, Ls) - max(tau - w // 2, 0))
    out["rc"] = np.ascontiguousarray(np.broadcast_to(rc.reshape(1, -1), (128, 128)))
    return out


PARAMS = [("w_ada", (4, 1024, 6144)), ("b_ada", (4, 6144)), ("w_in", (4, 1024, 2720)), ("conv_w", (4, 31, 512)),
          ("conv_b", (4, 512)), ("conv_ln_g", (4, 512)), ("conv_ln_b", (4, 512)), ("swa_sink", (4, 8)),
          ("pool_w", (4, 4, 128, 128)), ("pool_scale", (4, 512)), ("mla_q_g", (4, 256)), ("mla_w_uq", (4, 256, 768)),
          ("mla_kv_g", (4, 128)), ("mla_w_uk", (4, 128, 8, 64)), ("mla_w_uv", (4, 128, 8, 64)),
          ("w_branch", (4, 4, 512, 1024)), ("w_gate", (4, 4, 1024, 1024)), ("b_gate", (4, 4, 1024)),
          ("w_out", (4, 1024, 1024)), ("ln1_g", (4, 1024)), ("ln1_b", (4, 1024)), ("router_w", (4, 1024, 32)),
          ("router_b", (4, 32)), ("w_gu", (4, 32, 1024, 2048)), ("b_gu", (4, 32, 2048)), ("w_down", (4, 32, 1024, 1024)),
          ("b_down", (4, 32, 1024)), ("ln2_g", (4, 1024)), ("ln2_b", (4, 1024))]


def build(nlayers=DEPTH, upto=99, dbg=()):
    nc = bass.Bass("TRN2", target_bir_lowering=False)
    kb = KB(nc, dbg)
    I = {}
    I["x"] = kb.din("x", (NB * L, D))
    I["c"] = kb.din("c", (NB, D))
    I["ctx"] = kb.din("ctx", (NB * C, D))
    I["c_ctx"] = kb.din("c_ctx", (D,))
    for name, shp in PARAMS:
        if upto < 7 and name in ("w_gu", "w_down"):
            continue
        I[name] = kb.din(name, shp)
    tabs = {}
    for k, rows in (("c64", 128), ("s64", 128), ("c32", 32), ("s32", 32), ("c96", 96), ("s96", 96)):
        tabs[k] = kb.din("tab_" + k, (rows, L))
    kb.din("tab_rc", (128, 2 * 4 * 16))
    out = nc.dram_tensor("out", [NB * L, D], F32, kind="ExternalOutput").ap()
    kb.dram["out"] = (out, Buf("out"))
    ADA = kb.dscr("ADA", (DEPTH, 3, 6 * D), F32)
    kb.dscr("HT", (D, T), BF16)
    kb.dscr("VT", (512, T), BF16)
    kb.dscr("QT", (512, T), BF16)
    kb.dscr("KT", (128, T), BF16)
    kb.dscr("V", (T, 128), BF16)
    kb.dscr("PT", (512, T), BF16)
    kb.dscr("QM", (8, 96, T), BF16)
    kb.dscr("KN", (8, 64, T), BF16)
    kb.dscr("KR", (32, T), BF16)
    kb.dscr("VM", (T, 512), BF16)
    for nm in ("YA", "YB", "YC", "YD"):
        kb.dscr(nm, (512, T), BF16)
    kb.dscr("H2", (T, D), BF16)
    kb.dscr("WBG", (NE * 128, 8 * 2048), BF16)
    kb.dscr("WBD", (NE * 128, 8 * D), BF16)
    kb.dscr("XG", (NROWS, D), BF16)
    kb.dscr("YG", (NROWS, D), F32)
    XA = kb.dscr("XA", (T, D), F32)
    XB = kb.dscr("XB", (T, D), F32)
    with ExitStack() as ces:
        ces.enter_context(nc.allow_non_contiguous_dma(reason="small strided parameter loads"))
        ces.enter_context(nc.allow_low_precision(reason="bf16 matmul operands"))
        kb.c = build_consts(kb, ces)
        phase_ada(kb, kb.c, I["c"], I["c_ctx"], I["w_ada"], I["b_ada"], ADA)
        for l in range(nlayers):
            if l == 0:
                def xsrc(t0, n):
                    b, o = divmod(t0, S)
                    if o < L:
                        return I["x"][b * L + o:b * L + o + n, :]
                    return I["ctx"][b * C + (o - L):b * C + (o - L) + n, :]
            else:
                def xsrc(t0, n):
                    return XB[t0:t0 + n, :]
            if upto >= 1:
                phase_proj(kb, l, xsrc, I["w_in"], I["mla_q_g"], I["mla_w_uq"], I["mla_kv_g"], I["mla_w_uk"], I["mla_w_uv"], tabs, ADA)
            if upto >= 2:
                phase_conv(kb, l, I["conv_w"], I["conv_b"], I["conv_ln_g"], I["conv_ln_b"])
            if upto >= 3:
                phase_pool(kb, l, I["pool_w"], I["pool_scale"])
            if upto >= 4:
                phase_swa(kb, l, I["swa_sink"])
            if upto >= 5:
                phase_mla(kb, l, I)
            if upto >= 6:
                phase_merge(kb, l, xsrc, I, ADA, XA, skip_ctx=(l == DEPTH - 1))
            if upto >= 7:
                phase_moe(kb, l, I, ADA, XA, XB, out, l == nlayers - 1)
        kb.f.barrier()
    return nc


def core_inputs(inputs, core):
    m = {}
    b0 = core * NB
    m["x"] = np.ascontiguousarray(inputs["x"][b0:b0 + NB].reshape(NB * L, D))
    m["c"] = np.ascontiguousarray(inputs["c"][b0:b0 + NB])
    m["ctx"] = np.ascontiguousarray(inputs["ctx"][b0:b0 + NB].reshape(NB * C, D))
    m["c_ctx"] = np.ascontiguousarray(inputs["c_ctx"])
    for name, shp in PARAMS:
        m[name] = np.ascontiguousarray(inputs[name], dtype=np.float32)
    return m


_NC_CACHE = {}


def kernel(**inputs):
    n = 8
    if "nc" not in _NC_CACHE:
        _NC_CACHE["nc"] = build(nlayers=DEPTH, upto=99, dbg=())
    nc = _NC_CACHE["nc"]
    tabs = make_tables()
    shared = {}
    for name, shp in PARAMS:
        shared[name] = np.ascontiguousarray(np.asarray(inputs[name], dtype=np.float32))
    for k, v in tabs.items():
        shared["tab_" + k] = v
    x = np.asarray(inputs["x"], dtype=np.float32)
    c = np.asarray(inputs["c"], dtype=np.float32)
    ctx = np.asarray(inputs["ctx"], dtype=np.float32)
    c_ctx = np.ascontiguousarray(np.asarray(inputs["c_ctx"], dtype=np.float32))
    in_maps = []
    for core in range(n):
        b0 = core * NB
        m = dict(shared)
        m["x"] = np.ascontiguousarray(x[b0:b0 + NB].reshape(NB * L, D))
        m["c"] = np.ascontiguousarray(c[b0:b0 + NB])
        m["ctx"] = np.ascontiguousarray(ctx[b0:b0 + NB].reshape(NB * C, D))
        m["c_ctx"] = c_ctx
        in_maps.append(m)
    res = run_bass_kernel_spmd(nc, in_maps, core_ids=list(range(n)))
    outs = [np.asarray(r["out"], dtype=np.float32).reshape(NB, L, D) for r in res.results]
    return np.concatenate(outs, axis=0)
```
